# Optimizing a Trainium2 kernel written in Bass

```python
import math
import jax, jax.numpy as jnp
from jax import lax
import numpy as np

D_MODEL = 1024
BATCH = 2
SEQ = 16384
DEPTH = 2

GLA_HEADS = 4
GLA_DK = 256
GLA_DV = 512
GLA_HK = GLA_DK // GLA_HEADS
GLA_HV = GLA_DV // GLA_HEADS
GLA_LOWRANK = 16
GLA_TAU = 16.0
GLA_CHUNK = 64
MOBA_HEADS = 8
MOBA_HD = 64
MOBA_W = MOBA_HEADS * MOBA_HD
MOBA_BLOCK = 256
MOBA_TOPK = 3
MOBA_QCHUNK = 64
SC_W = 512
CONV_W = 3
N_BRANCH = 3
D_FF = 2816
EPS = 1e-6

IN_SPLITS = (GLA_DK, GLA_DK, GLA_DV, GLA_DV, GLA_LOWRANK,
             MOBA_W, MOBA_W, MOBA_W,
             SC_W, SC_W, SC_W,
             N_BRANCH * D_MODEL)
IN_COLS = sum(IN_SPLITS)

kernel_name = "hybrid_gla_moba_shortconv_convffn"


def rmsnorm(x, g):
    xf = x.astype(jnp.float32)
    y = xf * lax.rsqrt(jnp.mean(xf * xf, axis=-1, keepdims=True) + EPS)
    return (y * g.astype(jnp.float32)).astype(x.dtype)


def split_cols(z, sizes):
    out, start = [], 0
    for s in sizes:
        out.append(z[..., start:start + s])
        start += s
    return out


def alibi_slopes(n_heads):
    return 2.0 ** (-8.0 * (jnp.arange(n_heads, dtype=jnp.float32) + 1.0) / n_heads)


def causal_dwconv(x, w):
    c = x.shape[-1]
    return lax.conv_general_dilated(
        x, w[:, None, :].astype(x.dtype), window_strides=(1,), padding=[(CONV_W - 1, 0)],
        dimension_numbers=('NWC', 'WIO', 'NWC'), feature_group_count=c)


def gla_mixer(q, k, v, r, lr, w_lr2, b_lr, g_norm):
    bsz, t_len, _ = q.shape
    nc = t_len // GLA_CHUNK
    f32 = jnp.float32
    log_a = jax.nn.log_sigmoid((lr @ w_lr2 + b_lr).astype(f32)) / GLA_TAU

    def heads(t, hd):
        return t.astype(f32).reshape(bsz, nc, GLA_CHUNK, GLA_HEADS, hd).transpose(1, 0, 3, 2, 4)

    qc = heads(q, GLA_HK) * (GLA_HK ** -0.5)
    kc = heads(k, GLA_HK)
    vc = heads(v, GLA_HV)
    ac = heads(log_a, GLA_HK)
    causal = jnp.tril(jnp.ones((GLA_CHUNK, GLA_CHUNK), dtype=bool))

    def step(state, inp):
        qb, kb, vb, ab = inp
        bcum = jnp.cumsum(ab, axis=-2)
        diff = bcum[..., :, None, :] - bcum[..., None, :, :]
        decay = jnp.exp(jnp.where(causal[:, :, None], diff, -jnp.inf))
        att = jnp.einsum('bhid,bhjd,bhijd->bhij', qb, kb, decay)
        o = att @ vb + jnp.einsum('bhid,bhde->bhie', qb * jnp.exp(bcum), state)
        blast = bcum[..., -1:, :]
        state = (jnp.exp(blast[..., 0, :])[..., None] * state
                 + jnp.einsum('bhjd,bhje->bhde', kb * jnp.exp(blast - bcum), vb))
        return state, o

    s0 = jnp.zeros((bsz, GLA_HEADS, GLA_HK, GLA_HV), f32)
    _, o = lax.scan(step, s0, (qc, kc, vc, ac))
    o = o.transpose(1, 0, 3, 2, 4).reshape(bsz, t_len, GLA_HEADS, GLA_HV)
    o = rmsnorm(o, g_norm).reshape(bsz, t_len, GLA_DV) * jax.nn.silu(r.astype(f32))
    return o.astype(q.dtype)


def moba_mixer(q, k, v):
    bsz, t_len, _ = q.shape
    f32 = jnp.float32
    nb = -(-t_len // MOBA_BLOCK)
    tp = nb * MOBA_BLOCK
    nq = t_len // MOBA_QCHUNK
    ksel = min(MOBA_TOPK, nb)

    def heads(t):
        return t.astype(f32).reshape(bsz, t_len, MOBA_HEADS, MOBA_HD).transpose(0, 2, 1, 3)

    qh = heads(q) * (MOBA_HD ** -0.5)
    pad = ((0, 0), (0, 0), (0, tp - t_len), (0, 0))
    kp = jnp.pad(heads(k), pad)
    vp = jnp.pad(heads(v), pad)
    kblk = kp.reshape(bsz, MOBA_HEADS, nb, MOBA_BLOCK, MOBA_HD)
    vblk = vp.reshape(bsz, MOBA_HEADS, nb, MOBA_BLOCK, MOBA_HD)
    kmean = jnp.mean(kblk, axis=3)
    slopes = alibi_slopes(MOBA_HEADS)[None, :, None, None]
    bi = jnp.arange(bsz)[:, None, None, None]
    hi = jnp.arange(MOBA_HEADS)[None, :, None, None]
    offs = jnp.arange(MOBA_BLOCK)

    def one_chunk(c):
        t0 = c * MOBA_QCHUNK
        qc = lax.dynamic_slice_in_dim(qh, t0, MOBA_QCHUNK, axis=2)
        tpos = t0 + jnp.arange(MOBA_QCHUNK)
        n_own = t0 // MOBA_BLOCK
        gate = jnp.einsum('bhqd,bhnd->bhqn', qc, kmean)
        gate = jnp.where(jnp.arange(nb) < n_own, gate, -jnp.inf)
        _, sel = lax.top_k(gate, ksel)
        sel_valid = sel < n_own
        kg = kblk[bi, hi, sel]
        vg = vblk[bi, hi, sel]
        s_sel = jnp.einsum('bhqd,bhqksd->bhqks', qc, kg)
        spos = sel[..., None] * MOBA_BLOCK + offs
        s_sel = s_sel - slopes[..., None] * (tpos[:, None, None] - spos).astype(f32)
        s_sel = jnp.where(sel_valid[..., None], s_sel, -jnp.inf)
        s_sel = s_sel.reshape(bsz, MOBA_HEADS, MOBA_QCHUNK, ksel * MOBA_BLOCK)
        k_own = lax.dynamic_slice_in_dim(kp, n_own * MOBA_BLOCK, MOBA_BLOCK, axis=2)
        v_own = lax.dynamic_slice_in_dim(vp, n_own * MOBA_BLOCK, MOBA_BLOCK, axis=2)
        dist = (tpos[:, None] - (n_own * MOBA_BLOCK + offs)[None, :])
        s_own = jnp.einsum('bhqd,bhsd->bhqs', qc, k_own)
        s_own = jnp.where(dist >= 0, s_own - slopes * dist.astype(f32), -jnp.inf)
        p = jax.nn.softmax(jnp.concatenate([s_sel, s_own], axis=-1), axis=-1)
        p_sel = p[..., :ksel * MOBA_BLOCK].reshape(bsz, MOBA_HEADS, MOBA_QCHUNK, ksel, MOBA_BLOCK)
        p_own = p[..., ksel * MOBA_BLOCK:]
        return (jnp.einsum('bhqks,bhqksd->bhqd', p_sel, vg)
                + jnp.einsum('bhqs,bhsd->bhqd', p_own, v_own))

    o = lax.map(one_chunk, jnp.arange(nq))
    o = o.transpose(1, 2, 0, 3, 4).reshape(bsz, MOBA_HEADS, t_len, MOBA_HD)
    return o.transpose(0, 2, 1, 3).reshape(bsz, t_len, MOBA_W).astype(q.dtype)


def setup_inputs(seed: int = 0) -> dict:
    key = jax.random.key(seed)
    ks = jax.random.split(key, 20)
    L = DEPTH
    nrm = jax.random.normal

    def gain(k_):
        return 1.0 + 0.01 * nrm(k_, (L, D_MODEL), jnp.float32)

    return {
        "x": nrm(ks[0], (BATCH, SEQ, D_MODEL), jnp.float32),
        "g_mix_pre": gain(ks[1]),
        "w_in": nrm(ks[2], (L, D_MODEL, IN_COLS), jnp.float32) * D_MODEL ** -0.5,
        "gla_w_lr2": nrm(ks[3], (L, GLA_LOWRANK, GLA_DK), jnp.float32) * GLA_LOWRANK ** -0.5,
        "gla_b_lr": 0.1 * nrm(ks[4], (L, GLA_DK), jnp.float32),
        "gla_norm": 1.0 + 0.01 * nrm(ks[5], (L, GLA_HV), jnp.float32),
        "sc_conv_w": nrm(ks[6], (L, CONV_W, SC_W), jnp.float32) * CONV_W ** -0.5,
        "w_br_gla": nrm(ks[7], (L, GLA_DV, D_MODEL), jnp.float32) * GLA_DV ** -0.5,
        "w_br_moba": nrm(ks[8], (L, MOBA_W, D_MODEL), jnp.float32) * MOBA_W ** -0.5,
        "w_br_sc": nrm(ks[9], (L, SC_W, D_MODEL), jnp.float32) * SC_W ** -0.5,
        "w_out": nrm(ks[10], (L, D_MODEL, D_MODEL), jnp.float32) * D_MODEL ** -0.5,
        "g_mix_post": gain(ks[11]),
        "g_ffn_pre": gain(ks[12]),
        "ffn_w_up": nrm(ks[13], (L, D_MODEL, 2 * D_FF), jnp.float32) * D_MODEL ** -0.5,
        "ffn_conv_w": nrm(ks[14], (L, CONV_W, 2 * D_FF), jnp.float32) * CONV_W ** -0.5,
        "ffn_conv_b": 0.01 * nrm(ks[15], (L, 2 * D_FF), jnp.float32),
        "ffn_w_down": nrm(ks[16], (L, D_FF, D_MODEL), jnp.float32) * D_FF ** -0.5,
        "g_ffn_post": gain(ks[17]),
    }


def reference(x, g_mix_pre, w_in, gla_w_lr2, gla_b_lr, gla_norm, sc_conv_w, w_br_gla, w_br_moba,
              w_br_sc, w_out, g_mix_post, g_ffn_pre, ffn_w_up, ffn_conv_w, ffn_conv_b, ffn_w_down,
              g_ffn_post):
    bsz, t_len, _ = x.shape
    for l in range(DEPTH):
        h = rmsnorm(x, g_mix_pre[l])
        (gq, gk, gv, gr, glr, mq, mk, mv, sb, sc, sx, glog) = split_cols(h @ w_in[l], IN_SPLITS)
        br_a = gla_mixer(gq, gk, gv, gr, glr, gla_w_lr2[l], gla_b_lr[l], gla_norm[l])
        br_b = moba_mixer(mq, mk, mv)
        br_c = sb * causal_dwconv(sc * sx, sc_conv_w[l])
        gates = jax.nn.sigmoid(glog.astype(jnp.float32)).reshape(bsz, t_len, N_BRANCH, D_MODEL)
        merged = (gates[..., 0, :] * (br_a @ w_br_gla[l])
                  + gates[..., 1, :] * (br_b @ w_br_moba[l])
                  + gates[..., 2, :] * (br_c @ w_br_sc[l])).astype(x.dtype)
        x = x + rmsnorm(merged @ w_out[l], g_mix_post[l])
        h = rmsnorm(x, g_ffn_pre[l])
        u = causal_dwconv(h @ ffn_w_up[l], ffn_conv_w[l]) + ffn_conv_b[l]
        u_gate, u_up = u[..., :D_FF], u[..., D_FF:]
        y = (jax.nn.silu(u_gate) * u_up) @ ffn_w_down[l]
        x = x + rmsnorm(y, g_ffn_post[l])
    return x
```

```python
import concourse.bass as bass
import concourse.mybir as mybir

F32 = mybir.dt.float32
BF16 = mybir.dt.bfloat16
AF = mybir.ActivationFunctionType
ALU = mybir.AluOpType
AX = mybir.AxisListType

SEM_EPOCH = 40000


class Sched:
    def __init__(self, nc, es, same_engine_sync=True):
        self.nc = nc
        self.es = es
        self.same = same_engine_sync
        self.eng = {"pe": nc.tensor, "act": nc.scalar, "dve": nc.vector, "pool": nc.gpsimd, "sp": nc.sync}
        self.sem = {}
        self.cnt = {}
        self.nsem = 0
        for e in self.eng:
            self._new_sem(e)
        self.waited = {e: {} for e in self.eng}
        self.lastw = {}
        self.readers = {}
        self.dma_sems = {}
        self.ninstr = 0
        self.pending = {}

    def _new_sem(self, e):
        self.nsem += 1
        self.sem[e] = self.es.enter_context(self.nc.semaphore(f"s_{e}_{self.nsem}"))
        self.cnt[e] = 0

    def _wait(self, e, tok):
        if tok is None:
            return
        sem, val, src = tok
        if src == e and (not self.same or e == "pe"):
            return
        w = self.waited[e]
        k = id(sem)
        if w.get(k, 0) >= val:
            return
        self.eng[e].wait_ge(sem, val)
        w[k] = val

    def _deps(self, e, reads, writes):
        for r in reads:
            self._wait(e, self.lastw.get(r))
        for r in writes:
            self._wait(e, self.lastw.get(r))
            for t in list(self.readers.get(r, {}).values()):
                self._wait(e, t)

    def _commit(self, tok, reads, writes):
        for r in reads:
            self.readers.setdefault(r, {})[(tok[2], id(tok[0]))] = tok
        for r in writes:
            self.lastw[r] = tok
            self.readers[r] = {}

    def op(self, e, fn, reads=(), writes=(), sig=True):
        self._deps(e, reads, writes)
        if self.cnt[e] >= SEM_EPOCH and not self.pending.get(e):
            self._new_sem(e)
        ins = fn()
        if sig:
            self.cnt[e] += 1
            ins.then_inc(self.sem[e], 1)
            tok = (self.sem[e], self.cnt[e], e)
            self.pending[e] = False
        else:
            tok = (self.sem[e], self.cnt[e] + 1, e)
            self.pending[e] = True
        self._commit(tok, reads, writes)
        self.ninstr += 1
        return tok

    def dma(self, q, key, out, in_, reads=(), writes=()):
        self._deps(q, reads, writes)
        st = self.dma_sems.get(key)
        if st is None or st[1] >= SEM_EPOCH:
            self.nsem += 1
            st = [self.es.enter_context(self.nc.semaphore(f"d_{self.nsem}")), 0]
            self.dma_sems[key] = st
        ins = self.eng[q].dma_start(out=out, in_=in_)
        st[1] += 16
        ins.then_inc(st[0], 16)
        tok = (st[0], st[1], "dma")
        self._commit(tok, reads, writes)
        self.ninstr += 1
        return tok

    def finish(self, e, resources):
        for r in resources:
            self._wait(e, self.lastw.get(r))

    def barrier(self):
        toks = [(self.sem[e], self.cnt[e], e) for e in self.eng if self.cnt[e] > 0]
        toks += [(st[0], st[1], "dma") for st in self.dma_sems.values()]
        for e in self.eng:
            for t in toks:
                self._wait(e, t)

import contextlib
import numpy as np

P = 128
EPS = 1e-6


def split_even(n, maxn):
    k = -(-n // maxn)
    base, rem = divmod(n, k)
    out, s = [], 0
    for i in range(k):
        sz = base + (1 if i < rem else 0)
        out.append((s, sz))
        s += sz
    return out


def load_w(S, nc, wt, wd, nk, qname="pool", key="w"):
    v = wd.rearrange("(k p) n -> p k n", p=P)
    n = wd.shape[1]
    step = max(1, 4096 // n)
    for k0 in range(0, nk, step):
        k1 = min(nk, k0 + step)
        S.dma(qname, (key, k0 % 4), wt[:, k0:k1, :], v[:, k0:k1, :], writes=[(key, k0)])
    return [(key, k0) for k0 in range(0, nk, step)]


def rstd_from_psum(S, nc, out, ps, D, eps_col, reads, wres):
    S.op("act", lambda: nc.scalar.activation(out=out, in_=ps, func=AF.Ln, scale=1.0 / D, bias=eps_col), reads=reads, writes=[wres])
    S.op("act", lambda: nc.scalar.activation(out=out, in_=out, func=AF.Exp, scale=-0.5), reads=[wres], writes=[wres])


def emit_dense(nc, S, dm, a, n1max=456, n2max=410):
    D, DFF, BW, NTOK = dm["D"], dm["DFF"], dm["BW"], dm["NTOK"]
    KD, KB, KF = D // P, BW // P, DFF // P
    KBB = KB // 3
    NT = NTOK + 2
    o_gpre, o_gpost, o_gfpre, o_gfpost = 0, KD, 2 * KD, 3 * KD
    o_cw = 4 * KD
    o_cb = o_cw + 3 * 2 * KF
    npar = o_cb + 2 * KF
    xTv = a["xT"].rearrange("(k p) n -> p k n", p=P)
    brTv = a["brT"].rearrange("(k p) n -> p k n", p=P)
    xmv = a["xmid"].rearrange("(k p) n -> p k n", p=P)
    yTv = a["yT"].rearrange("(k p) n -> p k n", p=P)

    with contextlib.ExitStack() as es0:
        E0 = es0.enter_context
        pp = E0(nc.sbuf_tensor("pp_sb", [P, npar], F32))
        ones = E0(nc.sbuf_tensor("ones", [P, P], BF16))
        epsc = E0(nc.sbuf_tensor("epsc", [P, 1], F32))
        S.dma("sp", "pp", pp[:], a["pp"][:, :], writes=["pp"])
        S.op("dve", lambda: nc.vector.memset(ones[:], 1.0), writes=["ones"])
        S.op("dve", lambda: nc.vector.memset(epsc[:], EPS), writes=["epsc"])
        d1_tiles = split_even(NT, n1max)
        N1 = max(n for _, n in d1_tiles)
        with contextlib.ExitStack() as es:
            E = es.enter_context
            wg = E(nc.sbuf_tensor("wg", [P, KD, 3 * D], BF16))
            wbr = E(nc.sbuf_tensor("wbr", [P, KB, D], BF16))
            wo = E(nc.sbuf_tensor("wo", [P, KD, D], BF16))
            r_wg = load_w(S, nc, wg, a["w_gate"], KD, key="wg")
            r_wbr = load_w(S, nc, wbr, a["w_br"], KB, key="wbr")
            r_wo = load_w(S, nc, wo, a["w_out"], KD, key="wo")
            x_sb = E(nc.sbuf_tensor("x_sb", [P, KD, N1], F32))
            br_sb = E(nc.sbuf_tensor("br_sb", [P, KB, N1], BF16))
            xsq = E(nc.sbuf_tensor("xsq", [P, KD, N1], BF16))
            xh = E(nc.sbuf_tensor("xh", [P, KD, N1], BF16))
            rstd = E(nc.sbuf_tensor("rstd", [P, N1], F32))
            rstd2 = E(nc.sbuf_tensor("rstd2", [P, N1], F32))
            gl = [E(nc.sbuf_tensor(f"gl{i}", [P, N1], F32)) for i in range(3)]
            tm = [E(nc.sbuf_tensor(f"tm{i}", [P, N1], F32)) for i in range(3)]
            mf = [E(nc.sbuf_tensor(f"mf{i}", [P, N1], F32)) for i in range(2)]
            mbf = E(nc.sbuf_tensor("mbf", [P, KD, N1], BF16))
            z_sb = E(nc.sbuf_tensor("z_sb", [P, KD, N1], F32))
            ps_st = E(nc.psum_tensor("ps_st", [P, 512], F32))
            ps_g = [E(nc.psum_tensor(f"ps_g{i}", [P, 512], F32)) for i in range(3)]
            ps_p = [E(nc.psum_tensor(f"ps_p{i}", [P, 512], F32)) for i in range(3)]
            ps_z = E(nc.psum_tensor("ps_z", [P, 512], F32))
            it = 0
            for ti, (c0, N) in enumerate(d1_tiles):
                S.dma("sp", "x", x_sb[:, :, :N], xTv[:, :, c0:c0 + N], writes=["x_sb"])
                S.dma("sp", "br", br_sb[:, :, :N], brTv[:, :, c0:c0 + N], writes=["br_sb"])
                S.op("act", lambda: nc.scalar.activation(out=xsq[:, :, :N], in_=x_sb[:, :, :N], func=AF.Square), reads=["x_sb"], writes=["xsq"])
                for k in range(KD):
                    S.op("act", lambda: nc.scalar.activation(out=xh[:, k, :N], in_=x_sb[:, k, :N], func=AF.Identity, scale=pp[:, o_gpre + k:o_gpre + k + 1]),
                         reads=["x_sb", "pp"], writes=[("xh", k)])
                for k in range(KD):
                    S.op("pe", lambda: nc.tensor.matmul(ps_st[:, :N], lhsT=ones[:], rhs=xsq[:, k, :N], start=(k == 0), stop=(k == KD - 1)),
                         reads=["xsq", "ones"], writes=["ps_st"], sig=(k == KD - 1))
                rstd_from_psum(S, nc, rstd[:, :N], ps_st[:, :N], D, epsc[:, 0:1], ["ps_st", "epsc"], "rstd")
                for m in range(KD):
                    for b in range(3):
                        i3 = it % 3
                        it += 1
                        pg, pq, g_t, t_t = ps_g[i3], ps_p[i3], gl[i3], tm[i3]
                        for k in range(KD):
                            S.op("pe", lambda: nc.tensor.matmul(pg[:, :N], lhsT=wg[:, k, b * D + m * P:b * D + (m + 1) * P], rhs=xh[:, k, :N], start=(k == 0), stop=(k == KD - 1)),
                                 reads=[("xh", k)] + r_wg, writes=[("pg", i3)], sig=(k == KD - 1))
                        for k in range(KBB):
                            S.op("pe", lambda: nc.tensor.matmul(pq[:, :N], lhsT=wbr[:, b * KBB + k, m * P:(m + 1) * P], rhs=br_sb[:, b * KBB + k, :N], start=(k == 0), stop=(k == KBB - 1)),
                                 reads=["br_sb"] + r_wbr, writes=[("pq", i3)], sig=(k == KBB - 1))
                        S.op("dve", lambda: nc.vector.tensor_tensor(out=g_t[:, :N], in0=pg[:, :N], in1=rstd[:, :N], op=ALU.mult),
                             reads=[("pg", i3), "rstd"], writes=[("gl", i3)])
                        S.op("act", lambda: nc.scalar.activation(out=g_t[:, :N], in_=g_t[:, :N], func=AF.Sigmoid), reads=[("gl", i3)], writes=[("gl", i3)])
                        mfm = mf[m % 2]
                        if b == 0:
                            S.op("dve", lambda: nc.vector.tensor_tensor(out=mfm[:, :N], in0=g_t[:, :N], in1=pq[:, :N], op=ALU.mult),
                                 reads=[("gl", i3), ("pq", i3)], writes=[("mf", m % 2)])
                        else:
                            S.op("dve", lambda: nc.vector.tensor_tensor(out=t_t[:, :N], in0=g_t[:, :N], in1=pq[:, :N], op=ALU.mult),
                                 reads=[("gl", i3), ("pq", i3)], writes=[("tm", i3)])
                            dst = mfm[:, :N] if b == 1 else mbf[:, m, :N]
                            wr = [("mf", m % 2)] if b == 1 else [("mbf", m)]
                            S.op("pool", lambda: nc.gpsimd.tensor_tensor(out=dst, in0=mfm[:, :N], in1=t_t[:, :N], op=ALU.add),
                                 reads=[("mf", m % 2), ("tm", i3)], writes=wr)
                for m in range(KD):
                    for k in range(KD):
                        S.op("pe", lambda: nc.tensor.matmul(ps_z[:, :N], lhsT=wo[:, k, m * P:(m + 1) * P], rhs=mbf[:, k, :N], start=(k == 0), stop=(k == KD - 1)),
                             reads=[("mbf", k)] + r_wo, writes=["ps_z"], sig=(k == KD - 1))
                    S.op("act", lambda: nc.scalar.copy(out=z_sb[:, m, :N], in_=ps_z[:, :N]), reads=["ps_z"], writes=[("z", m)])
                    S.op("pool", lambda: nc.gpsimd.tensor_tensor(out=xsq[:, m, :N], in0=z_sb[:, m, :N], in1=z_sb[:, m, :N], op=ALU.mult), reads=[("z", m)], writes=["xsq"])
                for k in range(KD):
                    S.op("pe", lambda: nc.tensor.matmul(ps_st[:, :N], lhsT=ones[:], rhs=xsq[:, k, :N], start=(k == 0), stop=(k == KD - 1)),
                         reads=["xsq", "ones"], writes=["ps_st"], sig=(k == KD - 1))
                rstd_from_psum(S, nc, rstd2[:, :N], ps_st[:, :N], D, epsc[:, 0:1], ["ps_st", "epsc"], "rstd2")
                for m in range(KD):
                    S.op("dve", lambda: nc.vector.scalar_tensor_tensor(out=z_sb[:, m, :N], in0=z_sb[:, m, :N], scalar=pp[:, o_gpost + m:o_gpost + m + 1], in1=rstd2[:, :N], op0=ALU.mult, op1=ALU.mult),
                         reads=[("z", m), "rstd2", "pp"], writes=[("z", m)])
                    S.op("pool", lambda: nc.gpsimd.tensor_tensor(out=z_sb[:, m, :N], in0=z_sb[:, m, :N], in1=x_sb[:, m, :N], op=ALU.add),
                         reads=[("z", m), "x_sb"], writes=[("z", m)])
                S.dma("sp", "xm_st", xmv[:, :, c0:c0 + N], z_sb[:, :, :N], reads=[("z", m) for m in range(KD)], writes=[("xmid", ti)])
        S.barrier()
        d2_tiles = split_even(NTOK, n2max)
        N2 = max(n for _, n in d2_tiles)
        with contextlib.ExitStack() as es:
            E = es.enter_context
            wup = E(nc.sbuf_tensor("wup", [P, KD, 2 * DFF], BF16))
            wdn = E(nc.sbuf_tensor("wdn", [P, KF, D], BF16))
            r_wup = load_w(S, nc, wup, a["w_up"], KD, key="wup")
            r_wdn = load_w(S, nc, wdn, a["w_down"], KF, key="wdn")
            xm = E(nc.sbuf_tensor("xm", [P, KD, N2 + 2], F32))
            xsq = E(nc.sbuf_tensor("xsq2", [P, KD, N2 + 2], BF16))
            hn = E(nc.sbuf_tensor("hn", [P, KD, N2 + 2], BF16))
            rstd = E(nc.sbuf_tensor("rstd_b", [P, N2 + 2], F32))
            rstd3 = E(nc.sbuf_tensor("rstd3", [P, N2 + 2], F32))
            a_sb = E(nc.sbuf_tensor("a_sb", [P, KF, N2], BF16))
            cvg = [E(nc.sbuf_tensor(f"cvg{i}", [P, N2], F32)) for i in range(3)]
            cvu = [E(nc.sbuf_tensor(f"cvu{i}", [P, N2], F32)) for i in range(3)]
            y_sb = E(nc.sbuf_tensor("y_sb", [P, KD, N2], F32))
            ps_st = E(nc.psum_tensor("ps_st2", [P, 512], F32))
            ps_ug = [E(nc.psum_tensor(f"ps_ug{i}", [P, 512], F32)) for i in range(3)]
            ps_uu = [E(nc.psum_tensor(f"ps_uu{i}", [P, 512], F32)) for i in range(3)]
            ps_y = [E(nc.psum_tensor(f"ps_y{i}", [P, 512], F32)) for i in range(1)]
            all_xmid = [("xmid", ti) for ti in range(len(d1_tiles))]
            for ti, (o0, N) in enumerate(d2_tiles):
                M = N + 2
                S.dma("sp", "xm", xm[:, :, :M], xmv[:, :, o0:o0 + M], reads=all_xmid, writes=["xm"])
                S.op("act", lambda: nc.scalar.activation(out=xsq[:, :, :M], in_=xm[:, :, :M], func=AF.Square), reads=["xm"], writes=["xsq2"])
                for k in range(KD):
                    S.op("pe", lambda: nc.tensor.matmul(ps_st[:, :M], lhsT=ones[:], rhs=xsq[:, k, :M], start=(k == 0), stop=(k == KD - 1)),
                         reads=["xsq2", "ones"], writes=["ps_st2"], sig=(k == KD - 1))
                rstd_from_psum(S, nc, rstd[:, :M], ps_st[:, :M], D, epsc[:, 0:1], ["ps_st2", "epsc"], "rstd_b")
                for k in range(KD):
                    S.op("dve", lambda: nc.vector.scalar_tensor_tensor(out=hn[:, k, :M], in0=xm[:, k, :M], scalar=pp[:, o_gfpre + k:o_gfpre + k + 1], in1=rstd[:, :M], op0=ALU.mult, op1=ALU.mult),
                         reads=["xm", "rstd_b", "pp"], writes=[("hn", k)])
                for c in range(KF):
                    i2 = c % 3
                    for half, pst, cv, col, nm in ((0, ps_ug[i2], cvg[i2], c, "ug"), (1, ps_uu[i2], cvu[i2], KF + c, "uu")):
                        for k in range(KD):
                            S.op("pe", lambda: nc.tensor.matmul(pst[:, :M], lhsT=wup[:, k, col * P:(col + 1) * P], rhs=hn[:, k, :M], start=(k == 0), stop=(k == KD - 1)),
                                 reads=[("hn", k)] + r_wup, writes=[(nm, i2)], sig=(k == KD - 1))
                        cw = lambda j: pp[:, o_cw + j * 2 * KF + col:o_cw + j * 2 * KF + col + 1]
                        S.op("act", lambda: nc.scalar.activation(out=cv[:, :N], in_=pst[:, 2:N + 2], func=AF.Identity, scale=cw(2), bias=pp[:, o_cb + col:o_cb + col + 1]),
                             reads=[(nm, i2), "pp"], writes=[("cv" + nm, i2)])
                        S.op("dve", lambda: nc.vector.scalar_tensor_tensor(out=cv[:, :N], in0=pst[:, 1:N + 1], scalar=cw(1), in1=cv[:, :N], op0=ALU.mult, op1=ALU.add),
                             reads=[(nm, i2), "pp", ("cv" + nm, i2)], writes=[("cv" + nm, i2)])
                        S.op("dve", lambda: nc.vector.scalar_tensor_tensor(out=cv[:, :N], in0=pst[:, 0:N], scalar=cw(0), in1=cv[:, :N], op0=ALU.mult, op1=ALU.add),
                             reads=[(nm, i2), "pp", ("cv" + nm, i2)], writes=[("cv" + nm, i2)])
                    S.op("pool", lambda: nc.gpsimd.tensor_tensor(out=cvu[i2][:, :N], in0=cvu[i2][:, :N], in1=cvg[i2][:, :N], op=ALU.mult),
                         reads=[("cvug", i2), ("cvuu", i2)], writes=[("cvuu", i2)])
                    S.op("act", lambda: nc.scalar.activation(out=cvg[i2][:, :N], in_=cvg[i2][:, :N], func=AF.Sigmoid), reads=[("cvug", i2)], writes=[("cvug", i2)])
                    S.op("pool", lambda: nc.gpsimd.tensor_tensor(out=a_sb[:, c, :N], in0=cvu[i2][:, :N], in1=cvg[i2][:, :N], op=ALU.mult),
                         reads=[("cvug", i2), ("cvuu", i2)], writes=[("a", c)])
                for m in range(KD):
                    py = ps_y[0]
                    for c in range(KF):
                        S.op("pe", lambda: nc.tensor.matmul(py[:, :N], lhsT=wdn[:, c, m * P:(m + 1) * P], rhs=a_sb[:, c, :N], start=(c == 0), stop=(c == KF - 1)),
                             reads=[("a", c)] + r_wdn, writes=[("py", 0)], sig=(c == KF - 1))
                    S.op("act", lambda: nc.scalar.copy(out=y_sb[:, m, :N], in_=py[:, :N]), reads=[("py", 0)], writes=[("y", m)])
                    S.op("pool", lambda: nc.gpsimd.tensor_tensor(out=xsq[:, m, :N], in0=y_sb[:, m, :N], in1=y_sb[:, m, :N], op=ALU.mult), reads=[("y", m)], writes=["xsq2"])
                for k in range(KD):
                    S.op("pe", lambda: nc.tensor.matmul(ps_st[:, :N], lhsT=ones[:], rhs=xsq[:, k, :N], start=(k == 0), stop=(k == KD - 1)),
                         reads=["xsq2", "ones"], writes=["ps_st2"], sig=(k == KD - 1))
                rstd_from_psum(S, nc, rstd3[:, :N], ps_st[:, :N], D, epsc[:, 0:1], ["ps_st2", "epsc"], "rstd3")
                for m in range(KD):
                    S.op("dve", lambda: nc.vector.scalar_tensor_tensor(out=y_sb[:, m, :N], in0=y_sb[:, m, :N], scalar=pp[:, o_gfpost + m:o_gfpost + m + 1], in1=rstd3[:, :N], op0=ALU.mult, op1=ALU.mult),
                         reads=[("y", m), "rstd3", "pp"], writes=[("y", m)])
                    S.op("pool", lambda: nc.gpsimd.tensor_tensor(out=y_sb[:, m, :N], in0=y_sb[:, m, :N], in1=xm[:, m, 2:N + 2], op=ALU.add),
                         reads=[("y", m), "xm"], writes=[("y", m)])
                S.dma("sp", "y_st", yTv[:, :, o0:o0 + N], y_sb[:, :, :N], reads=[("y", m) for m in range(KD)], writes=[("yT", ti)])
            S.finish("sp", [("yT", ti) for ti in range(len(d2_tiles))])


def pack_params_dense(g_pre, g_post, g_fpre, g_fpost, conv_w, conv_b):
    def col(v):
        return np.ascontiguousarray(v.reshape(-1, P).T)
    cw = np.concatenate([col(conv_w[j]) for j in range(3)], axis=1)
    return np.ascontiguousarray(np.concatenate([col(g_pre), col(g_post), col(g_fpre), col(g_fpost), cw, col(conv_b)], axis=1).astype(np.float32))


def build_dense(dm):
    nc = bass.Bass("TRN2", target_bir_lowering=False)
    D, DFF, BW, NTOK = dm["D"], dm["DFF"], dm["BW"], dm["NTOK"]
    KD, KF = D // P, DFF // P
    npar = 4 * KD + 8 * KF
    a = {
        "xT": nc.dram_tensor("xT", [D, NTOK + 2], F32, kind="ExternalInput").ap(),
        "brT": nc.dram_tensor("brT", [BW, NTOK + 2], BF16, kind="ExternalInput").ap(),
        "w_gate": nc.dram_tensor("w_gate", [D, 3 * D], F32, kind="ExternalInput").ap(),
        "w_br": nc.dram_tensor("w_br", [BW, D], F32, kind="ExternalInput").ap(),
        "w_out": nc.dram_tensor("w_out", [D, D], F32, kind="ExternalInput").ap(),
        "w_up": nc.dram_tensor("w_up", [D, 2 * DFF], F32, kind="ExternalInput").ap(),
        "w_down": nc.dram_tensor("w_down", [DFF, D], F32, kind="ExternalInput").ap(),
        "pp": nc.dram_tensor("pp", [P, npar], F32, kind="ExternalInput").ap(),
        "xmid": nc.dram_tensor("xmid", [D, NTOK + 2], F32).ap(),
        "yT": nc.dram_tensor("yT", [D, NTOK], F32, kind="ExternalOutput").ap(),
    }
    with contextlib.ExitStack() as es:
        S = Sched(nc, es)
        emit_dense(nc, S, dm, a)
        print("dense instrs", S.ninstr, "sems", S.nsem)
    return nc

import contextlib
import numpy as np

P = 128
EPS = 1e-6
BIG = 30000.0
NFM = 912
NTM = 320
FM_GROUPS = [("gq", 0, 64), ("gk", 64, 64), ("gr", 128, 128), ("lr", 256, 16), ("mq", 272, 128), ("mk", 400, 128),
             ("sb", 528, 128), ("sc", 656, 128), ("sx", 784, 128)]
C_U, C_SL, C_TRI, C_ID, C_FILL, C_A0, C_PP = 0, 128, 256, 384, 512, 640, 768
NCONST = 768


def emit_mixer(nc, S, dm, a):
    D, T = dm["D"], dm["T"]
    KD = D // P
    NTILE = T // 512
    NKT = T // P
    o_gpre = C_PP
    o_wlr = o_gpre + KD
    o_gn = o_wlr + 64
    o_scw = o_gn + 1
    o_al = o_scw + 3
    ncc = o_al + 4
    xTv = a["xT"].rearrange("(k p) n -> p k n", p=P)
    brv = a["brT"].rearrange("(k p) n -> p k n", p=P)

    with contextlib.ExitStack() as es:
        E = es.enter_context
        cc = E(nc.sbuf_tensor("cc_sb", [P, ncc], F32))
        S.dma("sp", "cc", cc[:], a["cc"][:, :], writes=["cc"])
        shiftc = E(nc.sbuf_tensor("shiftc_sb", [P, 2 * NKT], F32))
        S.dma("sp", "shiftc", shiftc[:], a["shiftc"][:, :], writes=["shiftc"])
        causneg = E(nc.sbuf_tensor("causneg_sb", [P, 4, 512], BF16))
        S.dma("sp", "causneg", causneg[:], a["causneg"][:, :, :], writes=["causneg"])
        ident_bf = E(nc.sbuf_tensor("ident_bf", [P, P], BF16))
        ones_bf = E(nc.sbuf_tensor("ones_bf", [P, P], BF16))
        ones_f = E(nc.sbuf_tensor("ones_f", [P, P], F32))
        epsc = E(nc.sbuf_tensor("epsc_a", [P, 1], F32))
        S.op("dve", lambda: nc.vector.memset(ones_bf[:], 1.0), writes=["ones_bf"])
        S.op("dve", lambda: nc.vector.memset(ones_f[:], 1.0), writes=["ones_f"])
        S.op("dve", lambda: nc.vector.memset(epsc[:], EPS), writes=["epsc"])
        S.op("dve", lambda: nc.vector.tensor_copy(out=ident_bf[:], in_=cc[:, C_ID:C_ID + P]), reads=["cc"], writes=["ident_bf"])
        U = cc[:, C_U:C_U + P]
        SL = cc[:, C_SL:C_SL + P]
        TRI = cc[:, C_TRI:C_TRI + P]
        IDF = cc[:, C_ID:C_ID + P]
        wf = E(nc.sbuf_tensor("wf", [P, KD, NFM], BF16))
        wt = E(nc.sbuf_tensor("wt", [P, KD, NTM], BF16))
        wfv = a["w_f"].rearrange("(k p) n -> p k n", p=P)
        wtv = a["w_t"].rearrange("(k p) n -> p k n", p=P)
        for k0 in range(0, KD, 4):
            S.dma("pool", ("wf", k0), wf[:, k0:min(KD, k0 + 4), :], wfv[:, k0:min(KD, k0 + 4), :], writes=["wf"])
        S.dma("pool", "wt", wt[:], wtv[:, :, :], writes=["wt"])
        kaug = [E(nc.sbuf_tensor(f"kaug{h}", [P, T], BF16)) for h in range(2)]
        S.dma("sp", "kaug0", kaug[0][64:128, :], a["eblk"][:, :], writes=["kaug0e"])
        S.dma("sp", "kaug1", kaug[1][0:64, :], a["eblk"][:, :], writes=["kaug1e"])
        vaug0 = E(nc.sbuf_tensor("vaug0", [P, NKT, 65], BF16))
        vaug1 = E(nc.sbuf_tensor("vaug1", [P, NKT, 128], BF16))
        S.op("pool", lambda: nc.gpsimd.memset(vaug0[:, :, 64:65], 1.0), writes=["vaug0c"])
        S.op("pool", lambda: nc.gpsimd.memset(vaug1[:, :, 0:64], 0.0), writes=["vaug1c"])
        S.op("pool", lambda: nc.gpsimd.memset(vaug1[:, :, 0:1], 1.0), reads=["vaug1c"], writes=["vaug1c"])
        kmean = E(nc.sbuf_tensor("kmean", [P, 64], F32))
        S.op("dve", lambda: nc.vector.memset(kmean[:], 0.0), writes=["kmean"])
        x_st = [E(nc.sbuf_tensor(f"x_st{i}", [P, 512], F32)) for i in range(2)]
        xh = E(nc.sbuf_tensor("xh_a", [P, KD, 512], BF16))
        xsq = [E(nc.sbuf_tensor(f"xsq_a{i}", [P, 512], BF16)) for i in range(2)]
        rstd = E(nc.sbuf_tensor("rstd_a", [P, 512], F32))
        rstd_tok = E(nc.sbuf_tensor("rstd_tok", [P, 4], F32))
        fo = {nm: E(nc.sbuf_tensor("fo_" + nm, [P, 512], F32)) for nm in ("gq", "gk", "gr", "lr", "mq", "mk", "sb", "sc", "sx")}
        S.op("dve", lambda: nc.vector.memset(fo["lr"][0:32, :], 1.0), writes=["fo_lr1"])
        zc = E(nc.sbuf_tensor("zc", [P, 514], F32))
        S.op("dve", lambda: nc.vector.memset(zc[:, 0:2], 0.0), writes=["zc_h"])
        cva = E(nc.sbuf_tensor("cva", [P, 512], F32))
        gk_tok = E(nc.sbuf_tensor("gk_tok", [P, 4, 64], F32))
        gv = E(nc.sbuf_tensor("gv", [P, 4, 128], BF16))
        qaug = [[E(nc.sbuf_tensor(f"qaug{h}_{i}", [P, 512], BF16)) for i in range(2)] for h in range(2)]
        mstage = E(nc.sbuf_tensor("mstage", [P, 4, P], F32))
        S.op("dve", lambda: nc.vector.memset(mstage[:], 0.0), writes=[("mstage", q) for q in range(4)])
        gm = E(nc.sbuf_tensor("gm", [P, 64], F32))
        top8 = E(nc.sbuf_tensor("top8", [P, 8], F32))
        thr = E(nc.sbuf_tensor("thr", [P, 1], F32))
        tsel = E(nc.sbuf_tensor("tsel", [P, 64], F32))
        pT = [E(nc.sbuf_tensor(f"pT{i}", [P, 512], BF16)) for i in range(2)]
        rs = E(nc.sbuf_tensor("rs", [P, 512], F32))
        rs2 = E(nc.sbuf_tensor("rs2", [P, 512], F32))
        br_sb = [E(nc.sbuf_tensor(f"br_out{i}", [P, 3, 512], BF16)) for i in range(2)]
        la = E(nc.sbuf_tensor("la", [P, 64], F32))
        eb = E(nc.sbuf_tensor("eb", [64, P], F32))
        enb = E(nc.sbuf_tensor("enb", [64, P], F32))
        er = E(nc.sbuf_tensor("er", [P, 64], F32))
        qt_bf = E(nc.sbuf_tensor("qt_bf", [64, P], BF16))
        kt_bf = E(nc.sbuf_tensor("kt_bf", [64, P], BF16))
        kh_bf = E(nc.sbuf_tensor("kh_bf", [P, 64], BF16))
        att_bf = E(nc.sbuf_tensor("att_bf", [P, P], BF16))
        Sst = E(nc.sbuf_tensor("Sst", [64, P], F32))
        Sbf = E(nc.sbuf_tensor("Sbf", [64, P], BF16))
        S.op("dve", lambda: nc.vector.memset(Sst[:], 0.0), writes=["Sst"])
        S.op("dve", lambda: nc.vector.memset(Sbf[:], 0.0), writes=["Sbf"])
        osq = E(nc.sbuf_tensor("osq", [P, 512], BF16))
        B = [E(nc.psum_tensor(f"bank{i}", [P, 512], F32)) for i in range(8)]

        def fixed_part(I):
            t0 = I * 512
            par = I % 2
            brs = br_sb[par]
            for k in range(KD):
                xs = x_st[k % 2]
                xq = xsq[k % 2]
                S.dma("sp", ("x", k % 2), xs[:], xTv[:, k, t0:t0 + 512], writes=[("x_st", k % 2)])
                S.op("pool", lambda: nc.gpsimd.tensor_tensor(out=xq[:], in0=xs[:], in1=xs[:], op=ALU.mult), reads=[("x_st", k % 2)], writes=[("xsq", k % 2)])
                S.op("dve", lambda: nc.vector.tensor_scalar(out=xh[:, k, :], in0=xs[:], scalar1=cc[:, o_gpre + k:o_gpre + k + 1], scalar2=None, op0=ALU.mult),
                     reads=[("x_st", k % 2), "cc"], writes=[("xh", k)])
                S.op("pe", lambda: nc.tensor.matmul(B[3][:, :], lhsT=ones_bf[:], rhs=xq[:], start=(k == 0), stop=(k == KD - 1)),
                     reads=[("xsq", k % 2), "ones_bf"], writes=["B3"])
                yield
            S.op("act", lambda: nc.scalar.activation(out=rstd[:], in_=B[3][:, :], func=AF.Ln, scale=1.0 / D, bias=epsc[:, 0:1]), reads=["B3", "epsc"], writes=["rstd"])
            S.op("act", lambda: nc.scalar.activation(out=rstd[:], in_=rstd[:], func=AF.Exp, scale=-0.5), reads=["rstd"], writes=["rstd"])
            yield
            for c in range(4):
                S.op("pe", lambda: nc.tensor.matmul(B[3][:, c:c + 1], lhsT=rstd[0:1, c * P:(c + 1) * P], rhs=ones_f[0:1, 0:1], start=True, stop=True),
                     reads=["rstd", "ones_f"], writes=["B3"], sig=(c == 3))
            S.op("dve", lambda: nc.vector.tensor_copy(out=rstd_tok[:], in_=B[3][:, 0:4]), reads=["B3"], writes=["rstd_tok"])
            yield
            for gi, (nm, c0, ncol) in enumerate(FM_GROUPS):
                bi_ = 2 if gi % 2 == 0 else 4
                pb = B[bi_]
                pn = "B%d" % bi_
                for k in range(KD):
                    S.op("pe", lambda: nc.tensor.matmul(pb[0:ncol, :], lhsT=wf[:, k, c0:c0 + ncol], rhs=xh[:, k, :], start=(k == 0), stop=(k == KD - 1)),
                         reads=[("xh", k), "wf"], writes=[pn], sig=(k == KD - 1))
                    if k % 4 == 3:
                        yield
                dst = fo[nm]
                if nm in ("mq", "gq"):
                    S.op("dve", lambda: nc.vector.scalar_tensor_tensor(out=dst[0:ncol, :], in0=pb[0:ncol, :], scalar=0.125, in1=rstd[0:ncol, :], op0=ALU.mult, op1=ALU.mult),
                         reads=[pn, "rstd"], writes=["fo_" + nm])
                else:
                    extra = ["fo_lr1"] if nm == "lr" else []
                    S.op("dve", lambda: nc.vector.tensor_tensor(out=dst[0:ncol, :], in0=pb[0:ncol, :], in1=rstd[0:ncol, :], op=ALU.mult),
                         reads=[pn, "rstd"] + extra, writes=["fo_" + nm])
                yield
            S.op("pool", lambda: nc.gpsimd.tensor_copy(out=qaug[0][par][0:64, :], in_=fo["mq"][0:64, :]), reads=["fo_mq"], writes=[("qaugq", 0, par)])
            S.op("pool", lambda: nc.gpsimd.tensor_copy(out=qaug[1][par][64:128, :], in_=fo["mq"][64:128, :]), reads=["fo_mq"], writes=[("qaugq", 1, par)])
            yield
            S.op("pool", lambda: nc.gpsimd.tensor_copy(out=kaug[0][0:64, t0:t0 + 512], in_=fo["mk"][0:64, :]), reads=["fo_mk"], writes=[("kaugk", 0, I)])
            S.op("pool", lambda: nc.gpsimd.tensor_copy(out=kaug[1][64:128, t0:t0 + 512], in_=fo["mk"][64:128, :]), reads=["fo_mk"], writes=[("kaugk", 1, I)])
            S.op("dve", lambda: nc.vector.tensor_reduce(out=kmean[:, 2 * I:2 * I + 2], in_=fo["mk"][:, :].rearrange("p (b n) -> p b n", n=256), axis=AX.X, op=ALU.add),
                 reads=["fo_mk"], writes=["kmean"])
            yield
            for c in range(4):
                bi_ = 2 if c % 2 == 0 else 4
                pb = B[bi_]
                pn = "B%d" % bi_
                for k in range(KD):
                    S.op("pe", lambda: nc.tensor.matmul(pb[:, 0:NTM], lhsT=xh[:, k, c * P:(c + 1) * P], rhs=wt[:, k, :], start=(k == 0), stop=(k == KD - 1)),
                         reads=[("xh", k), "wt"], writes=[pn], sig=(k == KD - 1))
                    if k % 4 == 3:
                        yield
                sc_ = rstd_tok[:, c:c + 1]
                S.op("dve", lambda: nc.vector.tensor_scalar(out=gk_tok[:, c, :], in0=pb[:, 0:64], scalar1=sc_, scalar2=None, op0=ALU.mult), reads=[pn, "rstd_tok"], writes=[("gk_tok", c)])
                S.op("dve", lambda: nc.vector.tensor_scalar(out=gv[:, c, :], in0=pb[:, 64:192], scalar1=sc_, scalar2=None, op0=ALU.mult), reads=[pn, "rstd_tok"], writes=[("gv", c)])
                yield
                S.op("dve", lambda: nc.vector.tensor_scalar(out=vaug0[:, 4 * I + c, 0:64], in0=pb[:, 192:256], scalar1=sc_, scalar2=None, op0=ALU.mult), reads=[pn, "rstd_tok"], writes=[("vaug", 0, I)])
                S.op("dve", lambda: nc.vector.tensor_scalar(out=vaug1[:, 4 * I + c, 64:128], in0=pb[:, 256:320], scalar1=sc_, scalar2=None, op0=ALU.mult), reads=[pn, "rstd_tok"], writes=[("vaug", 1, I)])
                yield
            S.op("pool", lambda: nc.gpsimd.tensor_tensor(out=zc[:, 2:514], in0=fo["sc"][:, :], in1=fo["sx"][:, :], op=ALU.mult), reads=["fo_sc", "fo_sx"], writes=["zc"])
            scw = lambda j: cc[:, o_scw + j:o_scw + j + 1]
            S.op("dve", lambda: nc.vector.tensor_scalar(out=cva[:], in0=zc[:, 2:514], scalar1=scw(2), scalar2=None, op0=ALU.mult), reads=["zc", "cc"], writes=["cva"])
            yield
            S.op("dve", lambda: nc.vector.scalar_tensor_tensor(out=cva[:], in0=zc[:, 1:513], scalar=scw(1), in1=cva[:], op0=ALU.mult, op1=ALU.add), reads=["zc", "zc_h", "cc", "cva"], writes=["cva"])
            yield
            S.op("dve", lambda: nc.vector.scalar_tensor_tensor(out=cva[:], in0=zc[:, 0:512], scalar=scw(0), in1=cva[:], op0=ALU.mult, op1=ALU.add), reads=["zc", "zc_h", "cc", "cva"], writes=["cva"])
            yield
            S.op("pool", lambda: nc.gpsimd.tensor_tensor(out=brs[:, 2, :], in0=cva[:], in1=fo["sb"][:, :], op=ALU.mult), reads=["cva", "fo_sb"], writes=[("br_c", par)])
            S.op("pool", lambda: nc.gpsimd.tensor_copy(out=zc[:, 0:2], in_=zc[:, 512:514]), reads=["zc", "zc_h"], writes=["zc_h"])
            yield
            S.op("act", lambda: nc.scalar.activation(out=fo["gr"][:, :], in_=fo["gr"][:, :], func=AF.Silu), reads=["fo_gr"], writes=["fo_gr"])
            for c in range(4):
                cs = slice(c * P, (c + 1) * P)
                S.op("pe", lambda: nc.tensor.matmul(B[2][:, 0:64], lhsT=fo["lr"][0:32, cs], rhs=cc[0:32, o_wlr:o_wlr + 64], start=True, stop=True),
                     reads=["fo_lr", "fo_lr1", "cc"], writes=["B2"])
                S.op("act", lambda: nc.scalar.activation(out=la[:], in_=B[2][:, 0:64], func=AF.Exp, scale=-1.0), reads=["B2"], writes=["la"])
                yield
                S.op("act", lambda: nc.scalar.activation(out=la[:], in_=la[:], func=AF.Ln, bias=1.0), reads=["la"], writes=["la"])
                yield
                S.op("pe", lambda: nc.tensor.matmul(B[4][0:64, 0:128], lhsT=la[:], rhs=U, start=True, stop=True), reads=["la", "cc"], writes=["B4"])
                S.op("pe", lambda: nc.tensor.matmul(B[2][:, 64:128], lhsT=SL, rhs=la[:], start=True, stop=True), reads=["la", "cc"], writes=["B2"])
                S.op("act", lambda: nc.scalar.activation(out=eb[:], in_=B[4][0:64, 0:128], func=AF.Exp), reads=["B4"], writes=["eb"])
                yield
                S.op("act", lambda: nc.scalar.activation(out=enb[:], in_=B[4][0:64, 0:128], func=AF.Exp, scale=-1.0), reads=["B4"], writes=["enb"])
                S.op("act", lambda: nc.scalar.activation(out=er[:], in_=B[2][:, 64:128], func=AF.Exp), reads=["B2"], writes=["er"])
                S.op("dve", lambda: nc.vector.tensor_tensor(out=qt_bf[:], in0=fo["gq"][0:64, cs], in1=eb[:], op=ALU.mult), reads=["fo_gq", "eb"], writes=["qt_bf"])
                yield
                S.op("dve", lambda: nc.vector.tensor_tensor(out=kt_bf[:], in0=fo["gk"][0:64, cs], in1=enb[:], op=ALU.mult), reads=["fo_gk", "enb"], writes=["kt_bf"])
                S.op("pool", lambda: nc.gpsimd.tensor_tensor(out=kh_bf[:], in0=gk_tok[:, c, :], in1=er[:], op=ALU.mult), reads=[("gk_tok", c), "er"], writes=["kh_bf"])
                yield
                S.op("pe", lambda: nc.tensor.matmul(B[4][:, 128:256], lhsT=kt_bf[:], rhs=qt_bf[:], start=True, stop=True), reads=["kt_bf", "qt_bf"], writes=["B4"])
                S.op("dve", lambda: nc.vector.tensor_tensor(out=att_bf[:], in0=B[4][:, 128:256], in1=TRI, op=ALU.mult), reads=["B4", "cc"], writes=["att_bf"])
                yield
                S.op("pe", lambda: nc.tensor.matmul(B[5][:, cs], lhsT=gv[:, c, :], rhs=att_bf[:], start=True, stop=False), reads=[("gv", c), "att_bf"], writes=["B5"], sig=False)
                S.op("pe", lambda: nc.tensor.matmul(B[5][:, cs], lhsT=Sbf[:], rhs=qt_bf[:], start=False, stop=True), reads=["Sbf", "qt_bf"], writes=["B5"])
                S.op("pe", lambda: nc.tensor.matmul(B[4][0:64, 256:384], lhsT=kh_bf[:], rhs=gv[:, c, :], start=True, stop=True), reads=["kh_bf", ("gv", c)], writes=["B4"])
                yield
                S.op("dve", lambda: nc.vector.scalar_tensor_tensor(out=Sst[:], in0=Sst[:], scalar=eb[:, 127:128], in1=B[4][0:64, 256:384], op0=ALU.mult, op1=ALU.add),
                     reads=["Sst", "eb", "B4"], writes=["Sst"])
                yield
                S.op("pool", lambda: nc.gpsimd.tensor_copy(out=Sbf[:], in_=Sst[:]), reads=["Sst"], writes=["Sbf"])
                yield
            S.op("act", lambda: nc.scalar.activation(out=osq[:], in_=B[5][:, :], func=AF.Square), reads=["B5"], writes=["osq"])
            S.op("pe", lambda: nc.tensor.matmul(B[3][:, :], lhsT=ones_bf[:], rhs=osq[:], start=True, stop=True), reads=["osq", "ones_bf"], writes=["B3"])
            yield
            S.op("act", lambda: nc.scalar.activation(out=rs[:], in_=B[3][:, :], func=AF.Ln, scale=1.0 / P, bias=epsc[:, 0:1]), reads=["B3", "epsc"], writes=["rs"])
            yield
            S.op("act", lambda: nc.scalar.activation(out=rs[:], in_=rs[:], func=AF.Exp, scale=-0.5), reads=["rs"], writes=["rs"])
            S.op("dve", lambda: nc.vector.tensor_tensor(out=cva[:], in0=B[5][:, :], in1=rs[:], op=ALU.mult), reads=["B5", "rs"], writes=["cva"])
            yield
            S.op("dve", lambda: nc.vector.scalar_tensor_tensor(out=brs[:, 0, :], in0=cva[:], scalar=cc[:, o_gn:o_gn + 1], in1=fo["gr"][:, :], op0=ALU.mult, op1=ALU.mult),
                 reads=["cva", "cc", "fo_gr"], writes=[("br_a", par)])
            yield
            S.dma("sp", ("br_st_ac", par), brv[:, 0, t0:t0 + 512], brs[:, 0, :], reads=[("br_a", par)], writes=[("brT_a", I)])
            S.dma("sp", ("br_st_cc", par), brv[:, 2, t0:t0 + 512], brs[:, 2, :], reads=[("br_c", par)], writes=[("brT_c", I)])
            for h in range(2):
                hs = slice(64 * h, 64 * h + 64)
                ms = slice(64 * (1 - h), 64 * (1 - h) + 64)
                for qs in range(4):
                    qt = 4 * I + qs
                    bi = qt // 2
                    S.op("pe", lambda: nc.tensor.matmul(B[3][:, 0:64], lhsT=fo["mq"][hs, qs * P:(qs + 1) * P], rhs=kmean[hs, :], start=True, stop=True),
                         reads=["fo_mq", "kmean"], writes=["B3"])
                    S.op("dve", lambda: nc.vector.tensor_tensor(out=gm[:], in0=B[3][:, 0:64], in1=cc[:, C_FILL + 63 - bi:C_FILL + 127 - bi], op=ALU.add), reads=["B3", "cc"], writes=["gm"])
                    yield
                    S.op("dve", lambda: nc.vector.max(out=top8[:], in_=gm[:]), reads=["gm"], writes=["top8"])
                    yield
                    S.op("dve", lambda: nc.vector.tensor_scalar(out=thr[:], in0=top8[:, 3:4], scalar1=-1e29, scalar2=None, op0=ALU.max), reads=["top8"], writes=["thr"])
                    yield
                    S.op("dve", lambda: nc.vector.tensor_scalar(out=tsel[:], in0=gm[:], scalar1=thr[:, 0:1], scalar2=BIG, op0=ALU.is_ge, op1=ALU.mult), reads=["gm", "thr"], writes=["tsel"])
                    yield
                    S.op("dve", lambda: nc.vector.scalar_tensor_tensor(out=mstage[:, qs, ms], in0=tsel[:], scalar=shiftc[:, h * NKT + qt:h * NKT + qt + 1], in1=cc[:, C_A0 + 64 * h:C_A0 + 64 * h + 64], op0=ALU.add, op1=ALU.add),
                         reads=["tsel", "shiftc", "cc"], writes=[("mstage", qs)])
                    yield
                for qs in range(4):
                    S.op("pe", lambda: nc.tensor.transpose(B[5][:, qs * P:(qs + 1) * P], mstage[:, qs, :], IDF), reads=[("mstage", qs), "cc"], writes=["B5"], sig=(qs == 3))
                S.op("act", lambda: nc.scalar.copy(out=qaug[h][par][ms, :], in_=B[5][ms, :]), reads=["B5"], writes=[("qaugm", h, par)])
                yield

        def moba_part(I, filler):
            t0 = I * 512
            par = I % 2
            brs = br_sb[par]
            nkt = 4 * I + 4
            nfill = max(1, -(-330 // (2 * nkt)))

            def fill():
                if filler is not None:
                    for _ in range(nfill):
                        try:
                            next(filler)
                        except StopIteration:
                            return

            for h in range(2):
                hs = slice(64 * h, 64 * h + 64)
                po = B[6 + h]
                qa = qaug[h][par]

                def emit_s(kt):
                    ib = kt % 2
                    ps = B[ib]
                    pn = "B%d" % ib
                    diag = kt >= 4 * I
                    S.op("pe", lambda: nc.tensor.matmul(ps[:, :], lhsT=kaug[h][:, kt * P:(kt + 1) * P], rhs=qa[:, :], start=True, stop=not diag),
                         reads=[("kaugk", h, kt // 4), f"kaug{h}e", ("qaugq", h, par), ("qaugm", h, par)], writes=[pn], sig=not diag)
                    if diag:
                        S.op("pe", lambda: nc.tensor.matmul(ps[:, :], lhsT=ident_bf[:], rhs=causneg[:, kt - 4 * I, :], start=False, stop=True),
                             reads=["ident_bf", "causneg"], writes=[pn])

                def emit_pv(kt):
                    ib = kt % 2
                    ps = B[ib]
                    pn = "B%d" % ib
                    S.op("act", lambda: nc.scalar.activation(out=pT[ib][:], in_=ps[:, :], func=AF.Exp, bias=cc[:, o_al + 2 * h + (kt % 2):o_al + 2 * h + (kt % 2) + 1]),
                         reads=[pn, "cc"], writes=[("pT", ib)])
                    if h == 0:
                        S.op("pe", lambda: nc.tensor.matmul(po[0:65, :], lhsT=vaug0[:, kt, :], rhs=pT[ib][:], start=(kt == 0), stop=(kt == nkt - 1)),
                             reads=[("pT", ib), ("vaug", 0, kt // 4), "vaug0c"], writes=["B6"])
                    else:
                        S.op("pe", lambda: nc.tensor.matmul(po[:, :], lhsT=vaug1[:, kt, :], rhs=pT[ib][:], start=(kt == 0), stop=(kt == nkt - 1)),
                             reads=[("pT", ib), ("vaug", 1, kt // 4), "vaug1c"], writes=["B7"])

                emit_s(0)
                for kt in range(nkt):
                    if kt + 1 < nkt:
                        emit_s(kt + 1)
                    emit_pv(kt)
                    fill()
                srow = 64 if h == 0 else 0
                pnm = "B%d" % (6 + h)
                S.op("dve", lambda: nc.vector.reciprocal(out=rs2[srow:srow + 1, :], in_=po[srow:srow + 1, :]), reads=[pnm], writes=["rs2"])
                S.op("pe", lambda: nc.tensor.matmul(B[3][:, :], lhsT=ones_f[srow:srow + 1, :], rhs=rs2[srow:srow + 1, :], start=True, stop=True), reads=["rs2", "ones_f"], writes=["B3"])
                S.op("act", lambda: nc.scalar.copy(out=rs2[hs, :], in_=B[3][hs, :]), reads=["B3", "rs2"], writes=["rs2"])
                S.op("dve", lambda: nc.vector.tensor_tensor(out=brs[hs, 1, :], in0=po[hs, :], in1=rs2[hs, :], op=ALU.mult), reads=[pnm, "rs2"], writes=[("br_b", par, h)])
            S.dma("sp", ("br_st_b", par), brv[:, 1, t0:t0 + 512], brs[:, 1, :], reads=[("br_b", par, 0), ("br_b", par, 1)], writes=[("brT_b", I)])

        for _ in fixed_part(0):
            pass
        for I in range(NTILE):
            nxt = fixed_part(I + 1) if I + 1 < NTILE else None
            moba_part(I, nxt)
            if nxt is not None:
                for _ in nxt:
                    pass
        S.finish("sp", [("brT_a", I) for I in range(NTILE)] + [("brT_b", I) for I in range(NTILE)] + [("brT_c", I) for I in range(NTILE)])


def mixer_consts(D, T, slopes2, g_pre, wlr2_g, blr_g, gnorm, scw_g):
    KD = D // P
    NKT = T // P
    ncc = C_PP + KD + 64 + 1 + 3 + 4
    cc = np.zeros((P, ncc), np.float32)
    j = np.arange(P)[:, None]
    i = np.arange(P)[None, :]
    cc[:, C_U:C_U + P] = (j <= i) * (-1.0 / 16.0)
    cc[:, C_SL:C_SL + P] = (j > i) * (-1.0 / 16.0)
    cc[:, C_TRI:C_TRI + P] = (j <= i) * 1.0
    cc[:, C_ID:C_ID + P] = np.eye(P)
    c = np.arange(128)
    cc[:, C_FILL:C_FILL + 128] = np.where(c < 63, 0.0, np.where(c == 63, 1e30, -1e30))[None, :]
    il = np.arange(P)[:, None]
    b = np.arange(64)[None, :]
    for h in range(2):
        cc[:, C_A0 + 64 * h:C_A0 + 64 * h + 64] = -slopes2[h] * (il - 256.0 * b)
    o = C_PP
    cc[:, o:o + KD] = g_pre.reshape(KD, P).T
    o += KD
    cc[0:16, o:o + 64] = wlr2_g
    cc[16, o:o + 64] = blr_g
    o += 64
    cc[:, o] = gnorm
    o += 1
    cc[:, o:o + 3] = scw_g.T
    o += 3
    for h in range(2):
        for par in range(2):
            cc[:, o + 2 * h + par] = slopes2[h] * (128.0 * par + np.arange(P))
    shiftc = np.zeros((P, 2 * NKT), np.float32)
    for h in range(2):
        shiftc[:, h * NKT:(h + 1) * NKT] = (-BIG - slopes2[h] * 128.0 * np.arange(NKT))[None, :]
    import ml_dtypes
    jl = np.arange(P)[:, None, None]
    kk = np.arange(4)[None, :, None]
    ii = np.arange(512)[None, None, :]
    causneg = np.where(128 * kk + jl <= ii, 0.0, -BIG).astype(ml_dtypes.bfloat16)
    eblk = np.zeros((64, T), np.float32)
    for bb in range(min(64, T // 256)):
        eblk[bb, 256 * bb:256 * bb + 256] = 1.0
    return cc, shiftc, causneg, eblk.astype(ml_dtypes.bfloat16)


def build_mixer(dm):
    nc = bass.Bass("TRN2", target_bir_lowering=False)
    D, T = dm["D"], dm["T"]
    KD, NKT = D // P, T // P
    ncc = C_PP + KD + 64 + 1 + 3 + 4
    a = {
        "xT": nc.dram_tensor("xT", [D, T], F32, kind="ExternalInput").ap(),
        "w_f": nc.dram_tensor("w_f", [D, NFM], F32, kind="ExternalInput").ap(),
        "w_t": nc.dram_tensor("w_t", [D, NTM], F32, kind="ExternalInput").ap(),
        "cc": nc.dram_tensor("cc", [P, ncc], F32, kind="ExternalInput").ap(),
        "shiftc": nc.dram_tensor("shiftc", [P, 2 * NKT], F32, kind="ExternalInput").ap(),
        "causneg": nc.dram_tensor("causneg", [P, 4, 512], BF16, kind="ExternalInput").ap(),
        "eblk": nc.dram_tensor("eblk", [64, T], BF16, kind="ExternalInput").ap(),
        "brT": nc.dram_tensor("brT", [3 * P, T], BF16, kind="ExternalOutput").ap(),
    }
    with contextlib.ExitStack() as es:
        S = Sched(nc, es)
        emit_mixer(nc, S, dm, a)
        print("mixer instrs", S.ninstr, "sems", S.nsem)
    return nc


from concourse.bass_utils import run_bass_kernel_spmd
import ml_dtypes

D_MODEL, SEQ, BATCH, DEPTH, D_FF = 1024, 16384, 2, 2, 2816
ALIBI = 2.0 ** (-8.0 * (np.arange(8) + 1.0) / 8)
_NC_CACHE = {}


def _get(name, fn):
    if name not in _NC_CACHE:
        _NC_CACHE[name] = fn()
    return _NC_CACHE[name]


def _mixer_inputs(xT_b, l, g, w_in, gla_w_lr2, gla_b_lr, gla_norm, sc_conv_w, g_mix_pre):
    W = w_in[l]
    sl = lambda o, n: W[:, o + n * g: o + n * g + n]
    w_f = np.ascontiguousarray(np.concatenate([sl(0, 64), sl(256, 64), sl(1024, 128), W[:, 1536:1552], sl(1552, 128), sl(2064, 128),
                                               sl(3088, 128), sl(3600, 128), sl(4112, 128)], axis=1))
    w_t = np.ascontiguousarray(np.concatenate([sl(256, 64), sl(512, 128), sl(2576, 128)], axis=1))
    cc, shiftc, causneg, eblk = mixer_consts(D_MODEL, SEQ, ALIBI[2 * g:2 * g + 2], g_mix_pre[l], gla_w_lr2[l][:, 64 * g:64 * g + 64],
                                             gla_b_lr[l][64 * g:64 * g + 64], gla_norm[l], sc_conv_w[l][:, 128 * g:128 * g + 128])
    return {"xT": xT_b, "w_f": w_f, "w_t": w_t, "cc": cc, "shiftc": shiftc, "causneg": causneg, "eblk": eblk}


def kernel(x, g_mix_pre, w_in, gla_w_lr2, gla_b_lr, gla_norm, sc_conv_w, w_br_gla, w_br_moba, w_br_sc, w_out, g_mix_post,
           g_ffn_pre, ffn_w_up, ffn_conv_w, ffn_conv_b, ffn_w_down, g_ffn_post):
    f32 = lambda v: np.ascontiguousarray(np.asarray(v, dtype=np.float32))
    (x, g_mix_pre, w_in, gla_w_lr2, gla_b_lr, gla_norm, sc_conv_w, w_br_gla, w_br_moba, w_br_sc, w_out, g_mix_post,
     g_ffn_pre, ffn_w_up, ffn_conv_w, ffn_conv_b, ffn_w_down, g_ffn_post) = map(f32, (
        x, g_mix_pre, w_in, gla_w_lr2, gla_b_lr, gla_norm, sc_conv_w, w_br_gla, w_br_moba, w_br_sc, w_out, g_mix_post,
        g_ffn_pre, ffn_w_up, ffn_conv_w, ffn_conv_b, ffn_w_down, g_ffn_post))
    NTOK = SEQ // 4
    nc_a = _get("mixer", lambda: build_mixer(dict(D=D_MODEL, T=SEQ)))
    nc_b = _get("dense", lambda: build_dense(dict(D=D_MODEL, DFF=D_FF, BW=1536, NTOK=NTOK)))
    xT = [np.ascontiguousarray(x[b].T) for b in range(BATCH)]
    for l in range(DEPTH):
        in_maps = [_mixer_inputs(xT[c // 4], l, c % 4, w_in, gla_w_lr2, gla_b_lr, gla_norm, sc_conv_w, g_mix_pre) for c in range(8)]
        res = run_bass_kernel_spmd(nc_a, in_maps, core_ids=list(range(8)))
        brT = []
        for b in range(BATCH):
            parts = [np.asarray(res.results[4 * b + g]["brT"]) for g in range(4)]
            brT.append(np.concatenate([p[0:128] for p in parts] + [p[128:256] for p in parts] + [p[256:384] for p in parts], axis=0))
        w_gate = np.ascontiguousarray(w_in[l][:, 4624:7696])
        w_br = np.ascontiguousarray(np.concatenate([w_br_gla[l], w_br_moba[l], w_br_sc[l]], axis=0))
        pp = pack_params_dense(g_mix_pre[l], g_mix_post[l], g_ffn_pre[l], g_ffn_post[l], ffn_conv_w[l], ffn_conv_b[l])
        in_maps = []
        for c in range(8):
            b, j = c // 4, c % 4
            t0 = j * NTOK
            xs = np.zeros((D_MODEL, NTOK + 2), np.float32)
            bs = np.zeros((1536, NTOK + 2), ml_dtypes.bfloat16)
            lo = max(t0 - 2, 0)
            xs[:, 2 - (t0 - lo):] = xT[b][:, lo:t0 + NTOK]
            bs[:, 2 - (t0 - lo):] = brT[b][:, lo:t0 + NTOK]
            in_maps.append({"xT": xs, "brT": bs, "w_gate": w_gate, "w_br": w_br, "w_out": w_out[l], "w_up": ffn_w_up[l],
                            "w_down": ffn_w_down[l], "pp": pp})
        res = run_bass_kernel_spmd(nc_b, in_maps, core_ids=list(range(8)))
        xT = [np.ascontiguousarray(np.concatenate([np.asarray(res.results[4 * b + j]["yT"]) for j in range(4)], axis=1)) for b in range(BATCH)]
    return np.ascontiguousarray(np.stack([xT[b].T for b in range(BATCH)], axis=0).astype(np.float32))
```

```python
import concourse.bass as bass
import concourse.mybir as mybir

F32 = mybir.dt.float32
BF16 = mybir.dt.bfloat16
AF = mybir.ActivationFunctionType
ALU = mybir.AluOpType
AX = mybir.AxisListType

SEM_EPOCH = 40000


class Sched:
    def __init__(self, nc, es, same_engine_sync=True):
        self.nc = nc
        self.es = es
        self.same = same_engine_sync
        self.eng = {"pe": nc.tensor, "act": nc.scalar, "dve": nc.vector, "pool": nc.gpsimd, "sp": nc.sync}
        self.sem = {}
        self.cnt = {}
        self.nsem = 0
        for e in self.eng:
            self._new_sem(e)
        self.waited = {e: {} for e in self.eng}
        self.lastw = {}
        self.readers = {}
        self.dma_sems = {}
        self.ninstr = 0
        self.pending = {}

    def _new_sem(self, e):
        self.nsem += 1
        self.sem[e] = self.es.enter_context(self.nc.semaphore(f"s_{e}_{self.nsem}"))
        self.cnt[e] = 0

    def _wait(self, e, tok):
        if tok is None:
            return
        sem, val, src = tok
        if src == e and (not self.same or e == "pe"):
            return
        w = self.waited[e]
        k = id(sem)
        if w.get(k, 0) >= val:
            return
        self.eng[e].wait_ge(sem, val)
        w[k] = val

    def _deps(self, e, reads, writes):
        for r in reads:
            self._wait(e, self.lastw.get(r))
        for r in writes:
            self._wait(e, self.lastw.get(r))
            for t in list(self.readers.get(r, {}).values()):
                self._wait(e, t)

    def _commit(self, tok, reads, writes):
        for r in reads:
            self.readers.setdefault(r, {})[(tok[2], id(tok[0]))] = tok
        for r in writes:
            self.lastw[r] = tok
            self.readers[r] = {}

    def op(self, e, fn, reads=(), writes=(), sig=True):
        self._deps(e, reads, writes)
        if self.cnt[e] >= SEM_EPOCH and not self.pending.get(e):
            self._new_sem(e)
        ins = fn()
        if sig:
            self.cnt[e] += 1
            ins.then_inc(self.sem[e], 1)
            tok = (self.sem[e], self.cnt[e], e)
            self.pending[e] = False
        else:
            tok = (self.sem[e], self.cnt[e] + 1, e)
            self.pending[e] = True
        self._commit(tok, reads, writes)
        self.ninstr += 1
        return tok

    def dma(self, q, key, out, in_, reads=(), writes=()):
        self._deps(q, reads, writes)
        st = self.dma_sems.get(key)
        if st is None or st[1] >= SEM_EPOCH:
            self.nsem += 1
            st = [self.es.enter_context(self.nc.semaphore(f"d_{self.nsem}")), 0]
            self.dma_sems[key] = st
        ins = self.eng[q].dma_start(out=out, in_=in_)
        st[1] += 16
        ins.then_inc(st[0], 16)
        tok = (st[0], st[1], "dma")
        self._commit(tok, reads, writes)
        self.ninstr += 1
        return tok

    def finish(self, e, resources):
        for r in resources:
            self._wait(e, self.lastw.get(r))

    def barrier(self):
        toks = [(self.sem[e], self.cnt[e], e) for e in self.eng if self.cnt[e] > 0]
        toks += [(st[0], st[1], "dma") for st in self.dma_sems.values()]
        for e in self.eng:
            for t in toks:
                self._wait(e, t)

import contextlib
import numpy as np

P = 128
EPS = 1e-6


def split_even(n, maxn):
    k = -(-n // maxn)
    base, rem = divmod(n, k)
    out, s = [], 0
    for i in range(k):
        sz = base + (1 if i < rem else 0)
        out.append((s, sz))
        s += sz
    return out


def load_w(S, nc, wt, wd, nk, qname="pool", key="w"):
    v = wd.rearrange("(k p) n -> p k n", p=P)
    n = wd.shape[1]
    step = max(1, 4096 // n)
    for k0 in range(0, nk, step):
        k1 = min(nk, k0 + step)
        S.dma(qname, (key, k0 % 4), wt[:, k0:k1, :], v[:, k0:k1, :], writes=[(key, k0)])
    return [(key, k0) for k0 in range(0, nk, step)]


def rstd_from_psum(S, nc, out, ps, D, eps_col, reads, wres):
    S.op("act", lambda: nc.scalar.activation(out=out, in_=ps, func=AF.Ln, scale=1.0 / D, bias=eps_col), reads=reads, writes=[wres])
    S.op("act", lambda: nc.scalar.activation(out=out, in_=out, func=AF.Exp, scale=-0.5), reads=[wres], writes=[wres])


def emit_dense(nc, S, dm, a, n1max=456, n2max=410):
    D, DFF, BW, NTOK = dm["D"], dm["DFF"], dm["BW"], dm["NTOK"]
    KD, KB, KF = D // P, BW // P, DFF // P
    KBB = KB // 3
    NT = NTOK + 2
    o_gpre, o_gpost, o_gfpre, o_gfpost = 0, KD, 2 * KD, 3 * KD
    o_cw = 4 * KD
    o_cb = o_cw + 3 * 2 * KF
    npar = o_cb + 2 * KF
    xTv = a["xT"].rearrange("(k p) n -> p k n", p=P)
    brTv = a["brT"].rearrange("(k p) n -> p k n", p=P)
    xmv = a["xmid"].rearrange("(k p) n -> p k n", p=P)
    yTv = a["yT"].rearrange("(k p) n -> p k n", p=P)

    with contextlib.ExitStack() as es0:
        E0 = es0.enter_context
        pp = E0(nc.sbuf_tensor("pp_sb", [P, npar], F32))
        ones = E0(nc.sbuf_tensor("ones", [P, P], BF16))
        epsc = E0(nc.sbuf_tensor("epsc", [P, 1], F32))
        S.dma("sp", "pp", pp[:], a["pp"][:, :], writes=["pp"])
        S.op("dve", lambda: nc.vector.memset(ones[:], 1.0), writes=["ones"])
        S.op("dve", lambda: nc.vector.memset(epsc[:], EPS), writes=["epsc"])
        d1_tiles = split_even(NT, n1max)
        N1 = max(n for _, n in d1_tiles)
        with contextlib.ExitStack() as es:
            E = es.enter_context
            wg = E(nc.sbuf_tensor("wg", [P, KD, 3 * D], BF16))
            wbr = E(nc.sbuf_tensor("wbr", [P, KB, D], BF16))
            wo = E(nc.sbuf_tensor("wo", [P, KD, D], BF16))
            r_wg = load_w(S, nc, wg, a["w_gate"], KD, key="wg")
            r_wbr = load_w(S, nc, wbr, a["w_br"], KB, key="wbr")
            r_wo = load_w(S, nc, wo, a["w_out"], KD, key="wo")
            x_sb = E(nc.sbuf_tensor("x_sb", [P, KD, N1], F32))
            br_sb = E(nc.sbuf_tensor("br_sb", [P, KB, N1], BF16))
            xsq = E(nc.sbuf_tensor("xsq", [P, KD, N1], BF16))
            xh = E(nc.sbuf_tensor("xh", [P, KD, N1], BF16))
            rstd = E(nc.sbuf_tensor("rstd", [P, N1], F32))
            rstd2 = E(nc.sbuf_tensor("rstd2", [P, N1], F32))
            gl = [E(nc.sbuf_tensor(f"gl{i}", [P, N1], F32)) for i in range(3)]
            tm = [E(nc.sbuf_tensor(f"tm{i}", [P, N1], F32)) for i in range(3)]
            mf = [E(nc.sbuf_tensor(f"mf{i}", [P, N1], F32)) for i in range(2)]
            mbf = E(nc.sbuf_tensor("mbf", [P, KD, N1], BF16))
            z_sb = E(nc.sbuf_tensor("z_sb", [P, KD, N1], F32))
            ps_st = E(nc.psum_tensor("ps_st", [P, 512], F32))
            ps_g = [E(nc.psum_tensor(f"ps_g{i}", [P, 512], F32)) for i in range(3)]
            ps_p = [E(nc.psum_tensor(f"ps_p{i}", [P, 512], F32)) for i in range(3)]
            ps_z = E(nc.psum_tensor("ps_z", [P, 512], F32))
            it = 0
            for ti, (c0, N) in enumerate(d1_tiles):
                S.dma("sp", "x", x_sb[:, :, :N], xTv[:, :, c0:c0 + N], writes=["x_sb"])
                S.dma("sp", "br", br_sb[:, :, :N], brTv[:, :, c0:c0 + N], writes=["br_sb"])
                S.op("act", lambda: nc.scalar.activation(out=xsq[:, :, :N], in_=x_sb[:, :, :N], func=AF.Square), reads=["x_sb"], writes=["xsq"])
                for k in range(KD):
                    S.op("act", lambda: nc.scalar.activation(out=xh[:, k, :N], in_=x_sb[:, k, :N], func=AF.Identity, scale=pp[:, o_gpre + k:o_gpre + k + 1]),
                         reads=["x_sb", "pp"], writes=[("xh", k)])
                for k in range(KD):
                    S.op("pe", lambda: nc.tensor.matmul(ps_st[:, :N], lhsT=ones[:], rhs=xsq[:, k, :N], start=(k == 0), stop=(k == KD - 1)),
                         reads=["xsq", "ones"], writes=["ps_st"], sig=(k == KD - 1))
                rstd_from_psum(S, nc, rstd[:, :N], ps_st[:, :N], D, epsc[:, 0:1], ["ps_st", "epsc"], "rstd")
                def d1_head(m, b, i3):
                    pg, pq, g_t = ps_g[i3], ps_p[i3], gl[i3]
                    for k in range(KD):
                        S.op("pe", lambda: nc.tensor.matmul(pg[:, :N], lhsT=wg[:, k, b * D + m * P:b * D + (m + 1) * P], rhs=xh[:, k, :N], start=(k == 0), stop=(k == KD - 1)),
                             reads=[("xh", k)] + r_wg, writes=[("pg", i3)], sig=(k == KD - 1))
                    for k in range(KBB):
                        S.op("pe", lambda: nc.tensor.matmul(pq[:, :N], lhsT=wbr[:, b * KBB + k, m * P:(m + 1) * P], rhs=br_sb[:, b * KBB + k, :N], start=(k == 0), stop=(k == KBB - 1)),
                             reads=["br_sb"] + r_wbr, writes=[("pq", i3)], sig=(k == KBB - 1))
                    S.op("dve", lambda: nc.vector.tensor_tensor(out=g_t[:, :N], in0=pg[:, :N], in1=rstd[:, :N], op=ALU.mult),
                         reads=[("pg", i3), "rstd"], writes=[("gl", i3)])
                    S.op("act", lambda: nc.scalar.activation(out=g_t[:, :N], in_=g_t[:, :N], func=AF.Sigmoid), reads=[("gl", i3)], writes=[("gl", i3)])

                def d1_tail(m, b, i3):
                    pq, g_t, t_t = ps_p[i3], gl[i3], tm[i3]
                    mfm = mf[m % 2]
                    if b == 0:
                        S.op("dve", lambda: nc.vector.tensor_tensor(out=mfm[:, :N], in0=g_t[:, :N], in1=pq[:, :N], op=ALU.mult),
                             reads=[("gl", i3), ("pq", i3)], writes=[("mf", m % 2)])
                    else:
                        S.op("dve", lambda: nc.vector.tensor_tensor(out=t_t[:, :N], in0=g_t[:, :N], in1=pq[:, :N], op=ALU.mult),
                             reads=[("gl", i3), ("pq", i3)], writes=[("tm", i3)])
                        dst = mfm[:, :N] if b == 1 else mbf[:, m, :N]
                        wr = [("mf", m % 2)] if b == 1 else [("mbf", m)]
                        S.op("pool", lambda: nc.gpsimd.tensor_tensor(out=dst, in0=mfm[:, :N], in1=t_t[:, :N], op=ALU.add),
                             reads=[("mf", m % 2), ("tm", i3)], writes=wr)

                groups = [(m, b) for m in range(KD) for b in range(3)]
                prev = None
                for (m, b) in groups:
                    i3 = it % 3
                    it += 1
                    d1_head(m, b, i3)
                    if prev is not None:
                        d1_tail(*prev)
                    prev = (m, b, i3)
                d1_tail(*prev)
                for m in range(KD):
                    for k in range(KD):
                        S.op("pe", lambda: nc.tensor.matmul(ps_z[:, :N], lhsT=wo[:, k, m * P:(m + 1) * P], rhs=mbf[:, k, :N], start=(k == 0), stop=(k == KD - 1)),
                             reads=[("mbf", k)] + r_wo, writes=["ps_z"], sig=(k == KD - 1))
                    S.op("act", lambda: nc.scalar.copy(out=z_sb[:, m, :N], in_=ps_z[:, :N]), reads=["ps_z"], writes=[("z", m)])
                    S.op("pool", lambda: nc.gpsimd.tensor_tensor(out=xsq[:, m, :N], in0=z_sb[:, m, :N], in1=z_sb[:, m, :N], op=ALU.mult), reads=[("z", m)], writes=["xsq"])
                for k in range(KD):
                    S.op("pe", lambda: nc.tensor.matmul(ps_st[:, :N], lhsT=ones[:], rhs=xsq[:, k, :N], start=(k == 0), stop=(k == KD - 1)),
                         reads=["xsq", "ones"], writes=["ps_st"], sig=(k == KD - 1))
                rstd_from_psum(S, nc, rstd2[:, :N], ps_st[:, :N], D, epsc[:, 0:1], ["ps_st", "epsc"], "rstd2")
                for m in range(KD):
                    S.op("dve", lambda: nc.vector.scalar_tensor_tensor(out=z_sb[:, m, :N], in0=z_sb[:, m, :N], scalar=pp[:, o_gpost + m:o_gpost + m + 1], in1=rstd2[:, :N], op0=ALU.mult, op1=ALU.mult),
                         reads=[("z", m), "rstd2", "pp"], writes=[("z", m)])
                    S.op("pool", lambda: nc.gpsimd.tensor_tensor(out=z_sb[:, m, :N], in0=z_sb[:, m, :N], in1=x_sb[:, m, :N], op=ALU.add),
                         reads=[("z", m), "x_sb"], writes=[("z", m)])
                S.dma("sp", "xm_st", xmv[:, :, c0:c0 + N], z_sb[:, :, :N], reads=[("z", m) for m in range(KD)], writes=[("xmid", ti)])
        S.barrier()
        d2_tiles = split_even(NTOK, n2max)
        N2 = max(n for _, n in d2_tiles)
        with contextlib.ExitStack() as es:
            E = es.enter_context
            wup = E(nc.sbuf_tensor("wup", [P, KD, 2 * DFF], BF16))
            wdn = E(nc.sbuf_tensor("wdn", [P, KF, D], BF16))
            r_wup = load_w(S, nc, wup, a["w_up"], KD, key="wup")
            r_wdn = load_w(S, nc, wdn, a["w_down"], KF, key="wdn")
            xm = E(nc.sbuf_tensor("xm", [P, KD, N2 + 2], F32))
            xsq = E(nc.sbuf_tensor("xsq2", [P, KD, N2 + 2], BF16))
            hn = E(nc.sbuf_tensor("hn", [P, KD, N2 + 2], BF16))
            rstd = E(nc.sbuf_tensor("rstd_b", [P, N2 + 2], F32))
            rstd3 = E(nc.sbuf_tensor("rstd3", [P, N2 + 2], F32))
            a_sb = E(nc.sbuf_tensor("a_sb", [P, KF, N2], BF16))
            cvg = [E(nc.sbuf_tensor(f"cvg{i}", [P, N2], F32)) for i in range(3)]
            cvu = [E(nc.sbuf_tensor(f"cvu{i}", [P, N2], F32)) for i in range(3)]
            y_sb = E(nc.sbuf_tensor("y_sb", [P, KD, N2], F32))
            ps_st = E(nc.psum_tensor("ps_st2", [P, 512], F32))
            ps_ug = [E(nc.psum_tensor(f"ps_ug{i}", [P, 512], F32)) for i in range(3)]
            ps_uu = [E(nc.psum_tensor(f"ps_uu{i}", [P, 512], F32)) for i in range(3)]
            ps_y = [E(nc.psum_tensor(f"ps_y{i}", [P, 512], F32)) for i in range(1)]
            all_xmid = [("xmid", ti) for ti in range(len(d1_tiles))]
            for ti, (o0, N) in enumerate(d2_tiles):
                M = N + 2
                S.dma("sp", "xm", xm[:, :, :M], xmv[:, :, o0:o0 + M], reads=all_xmid, writes=["xm"])
                S.op("act", lambda: nc.scalar.activation(out=xsq[:, :, :M], in_=xm[:, :, :M], func=AF.Square), reads=["xm"], writes=["xsq2"])
                for k in range(KD):
                    S.op("pe", lambda: nc.tensor.matmul(ps_st[:, :M], lhsT=ones[:], rhs=xsq[:, k, :M], start=(k == 0), stop=(k == KD - 1)),
                         reads=["xsq2", "ones"], writes=["ps_st2"], sig=(k == KD - 1))
                rstd_from_psum(S, nc, rstd[:, :M], ps_st[:, :M], D, epsc[:, 0:1], ["ps_st2", "epsc"], "rstd_b")
                for k in range(KD):
                    S.op("dve", lambda: nc.vector.scalar_tensor_tensor(out=hn[:, k, :M], in0=xm[:, k, :M], scalar=pp[:, o_gfpre + k:o_gfpre + k + 1], in1=rstd[:, :M], op0=ALU.mult, op1=ALU.mult),
                         reads=["xm", "rstd_b", "pp"], writes=[("hn", k)])
                def d2_head(c):
                    i2 = c % 3
                    for half, pst, cv, col, nm in ((0, ps_ug[i2], cvg[i2], c, "ug"), (1, ps_uu[i2], cvu[i2], KF + c, "uu")):
                        for k in range(KD):
                            S.op("pe", lambda: nc.tensor.matmul(pst[:, :M], lhsT=wup[:, k, col * P:(col + 1) * P], rhs=hn[:, k, :M], start=(k == 0), stop=(k == KD - 1)),
                                 reads=[("hn", k)] + r_wup, writes=[(nm, i2)], sig=(k == KD - 1))
                        cw = lambda j: pp[:, o_cw + j * 2 * KF + col:o_cw + j * 2 * KF + col + 1]
                        S.op("act", lambda: nc.scalar.activation(out=cv[:, :N], in_=pst[:, 2:N + 2], func=AF.Identity, scale=cw(2), bias=pp[:, o_cb + col:o_cb + col + 1]),
                             reads=[(nm, i2), "pp"], writes=[("cv" + nm, i2)])
                        S.op("dve", lambda: nc.vector.scalar_tensor_tensor(out=cv[:, :N], in0=pst[:, 1:N + 1], scalar=cw(1), in1=cv[:, :N], op0=ALU.mult, op1=ALU.add),
                             reads=[(nm, i2), "pp", ("cv" + nm, i2)], writes=[("cv" + nm, i2)])
                        S.op("dve", lambda: nc.vector.scalar_tensor_tensor(out=cv[:, :N], in0=pst[:, 0:N], scalar=cw(0), in1=cv[:, :N], op0=ALU.mult, op1=ALU.add),
                             reads=[(nm, i2), "pp", ("cv" + nm, i2)], writes=[("cv" + nm, i2)])

                def d2_tail(c):
                    i2 = c % 3
                    S.op("pool", lambda: nc.gpsimd.tensor_tensor(out=cvu[i2][:, :N], in0=cvu[i2][:, :N], in1=cvg[i2][:, :N], op=ALU.mult),
                         reads=[("cvug", i2), ("cvuu", i2)], writes=[("cvuu", i2)])
                    S.op("act", lambda: nc.scalar.activation(out=cvg[i2][:, :N], in_=cvg[i2][:, :N], func=AF.Sigmoid), reads=[("cvug", i2)], writes=[("cvug", i2)])
                    S.op("pool", lambda: nc.gpsimd.tensor_tensor(out=a_sb[:, c, :N], in0=cvu[i2][:, :N], in1=cvg[i2][:, :N], op=ALU.mult),
                         reads=[("cvug", i2), ("cvuu", i2)], writes=[("a", c)])

                for c in range(KF + 1):
                    if c < KF:
                        d2_head(c)
                    if c >= 1:
                        d2_tail(c - 1)
                for m in range(KD):
                    py = ps_y[0]
                    for c in range(KF):
                        S.op("pe", lambda: nc.tensor.matmul(py[:, :N], lhsT=wdn[:, c, m * P:(m + 1) * P], rhs=a_sb[:, c, :N], start=(c == 0), stop=(c == KF - 1)),
                             reads=[("a", c)] + r_wdn, writes=[("py", 0)], sig=(c == KF - 1))
                    S.op("act", lambda: nc.scalar.copy(out=y_sb[:, m, :N], in_=py[:, :N]), reads=[("py", 0)], writes=[("y", m)])
                    S.op("pool", lambda: nc.gpsimd.tensor_tensor(out=xsq[:, m, :N], in0=y_sb[:, m, :N], in1=y_sb[:, m, :N], op=ALU.mult), reads=[("y", m)], writes=["xsq2"])
                for k in range(KD):
                    S.op("pe", lambda: nc.tensor.matmul(ps_st[:, :N], lhsT=ones[:], rhs=xsq[:, k, :N], start=(k == 0), stop=(k == KD - 1)),
                         reads=["xsq2", "ones"], writes=["ps_st2"], sig=(k == KD - 1))
                rstd_from_psum(S, nc, rstd3[:, :N], ps_st[:, :N], D, epsc[:, 0:1], ["ps_st2", "epsc"], "rstd3")
                for m in range(KD):
                    S.op("dve", lambda: nc.vector.scalar_tensor_tensor(out=y_sb[:, m, :N], in0=y_sb[:, m, :N], scalar=pp[:, o_gfpost + m:o_gfpost + m + 1], in1=rstd3[:, :N], op0=ALU.mult, op1=ALU.mult),
                         reads=[("y", m), "rstd3", "pp"], writes=[("y", m)])
                    S.op("pool", lambda: nc.gpsimd.tensor_tensor(out=y_sb[:, m, :N], in0=y_sb[:, m, :N], in1=xm[:, m, 2:N + 2], op=ALU.add),
                         reads=[("y", m), "xm"], writes=[("y", m)])
                S.dma("sp", "y_st", yTv[:, :, o0:o0 + N], y_sb[:, :, :N], reads=[("y", m) for m in range(KD)], writes=[("yT", ti)])
            S.finish("sp", [("yT", ti) for ti in range(len(d2_tiles))])


def pack_params_dense(g_pre, g_post, g_fpre, g_fpost, conv_w, conv_b):
    def col(v):
        return np.ascontiguousarray(v.reshape(-1, P).T)
    cw = np.concatenate([col(conv_w[j]) for j in range(3)], axis=1)
    return np.ascontiguousarray(np.concatenate([col(g_pre), col(g_post), col(g_fpre), col(g_fpost), cw, col(conv_b)], axis=1).astype(np.float32))


def build_dense(dm):
    nc = bass.Bass("TRN2", target_bir_lowering=False)
    D, DFF, BW, NTOK = dm["D"], dm["DFF"], dm["BW"], dm["NTOK"]
    KD, KF = D // P, DFF // P
    npar = 4 * KD + 8 * KF
    a = {
        "xT": nc.dram_tensor("xT", [D, NTOK + 2], F32, kind="ExternalInput").ap(),
        "brT": nc.dram_tensor("brT", [BW, NTOK + 2], BF16, kind="ExternalInput").ap(),
        "w_gate": nc.dram_tensor("w_gate", [D, 3 * D], F32, kind="ExternalInput").ap(),
        "w_br": nc.dram_tensor("w_br", [BW, D], F32, kind="ExternalInput").ap(),
        "w_out": nc.dram_tensor("w_out", [D, D], F32, kind="ExternalInput").ap(),
        "w_up": nc.dram_tensor("w_up", [D, 2 * DFF], F32, kind="ExternalInput").ap(),
        "w_down": nc.dram_tensor("w_down", [DFF, D], F32, kind="ExternalInput").ap(),
        "pp": nc.dram_tensor("pp", [P, npar], F32, kind="ExternalInput").ap(),
        "xmid": nc.dram_tensor("xmid", [D, NTOK + 2], F32).ap(),
        "yT": nc.dram_tensor("yT", [D, NTOK], F32, kind="ExternalOutput").ap(),
    }
    with contextlib.ExitStack() as es:
        S = Sched(nc, es)
        emit_dense(nc, S, dm, a)
        print("dense instrs", S.ninstr, "sems", S.nsem)
    return nc

import contextlib
import numpy as np

P = 128
EPS = 1e-6
BIG = 30000.0
NFM = 912
NTM = 320
FM_GROUPS = [("gq", 0, 64), ("gk", 64, 64), ("gr", 128, 128), ("lr", 256, 16), ("mq", 272, 128), ("mk", 400, 128),
             ("sb", 528, 128), ("sc", 656, 128), ("sx", 784, 128)]
C_U, C_SL, C_TRI, C_ID, C_FILL, C_A0, C_PP = 0, 128, 256, 384, 512, 640, 768
NCONST = 768


def emit_mixer(nc, S, dm, a):
    D, T = dm["D"], dm["T"]
    KD = D // P
    NTILE = T // 512
    NKT = T // P
    o_gpre = C_PP
    o_wlr = o_gpre + KD
    o_gn = o_wlr + 64
    o_scw = o_gn + 1
    o_al = o_scw + 3
    ncc = o_al + 4
    xTv = a["xT"].rearrange("(k p) n -> p k n", p=P)
    brv = a["brT"].rearrange("(k p) n -> p k n", p=P)

    with contextlib.ExitStack() as es:
        E = es.enter_context
        cc = E(nc.sbuf_tensor("cc_sb", [P, ncc], F32))
        S.dma("sp", "cc", cc[:], a["cc"][:, :], writes=["cc"])
        shiftc = E(nc.sbuf_tensor("shiftc_sb", [P, 2 * NKT], F32))
        S.dma("sp", "shiftc", shiftc[:], a["shiftc"][:, :], writes=["shiftc"])
        causneg = E(nc.sbuf_tensor("causneg_sb", [P, 4, 512], BF16))
        S.dma("sp", "causneg", causneg[:], a["causneg"][:, :, :], writes=["causneg"])
        ident_bf = E(nc.sbuf_tensor("ident_bf", [P, P], BF16))
        ones_bf = E(nc.sbuf_tensor("ones_bf", [P, P], BF16))
        ones_f = E(nc.sbuf_tensor("ones_f", [P, P], F32))
        epsc = E(nc.sbuf_tensor("epsc_a", [P, 1], F32))
        S.op("dve", lambda: nc.vector.memset(ones_bf[:], 1.0), writes=["ones_bf"])
        S.op("dve", lambda: nc.vector.memset(ones_f[:], 1.0), writes=["ones_f"])
        S.op("dve", lambda: nc.vector.memset(epsc[:], EPS), writes=["epsc"])
        S.op("dve", lambda: nc.vector.tensor_copy(out=ident_bf[:], in_=cc[:, C_ID:C_ID + P]), reads=["cc"], writes=["ident_bf"])
        U = cc[:, C_U:C_U + P]
        SL = cc[:, C_SL:C_SL + P]
        TRI = cc[:, C_TRI:C_TRI + P]
        IDF = cc[:, C_ID:C_ID + P]
        wf = E(nc.sbuf_tensor("wf", [P, KD, NFM], BF16))
        wt = E(nc.sbuf_tensor("wt", [P, KD, NTM], BF16))
        wfv = a["w_f"].rearrange("(k p) n -> p k n", p=P)
        wtv = a["w_t"].rearrange("(k p) n -> p k n", p=P)
        for k0 in range(0, KD, 4):
            S.dma("pool", ("wf", k0), wf[:, k0:min(KD, k0 + 4), :], wfv[:, k0:min(KD, k0 + 4), :], writes=["wf"])
        S.dma("pool", "wt", wt[:], wtv[:, :, :], writes=["wt"])
        kaug = [E(nc.sbuf_tensor(f"kaug{h}", [P, T], BF16)) for h in range(2)]
        S.dma("sp", "kaug0", kaug[0][64:128, :], a["eblk"][:, :], writes=["kaug0e"])
        S.dma("sp", "kaug1", kaug[1][0:64, :], a["eblk"][:, :], writes=["kaug1e"])
        vaug0 = E(nc.sbuf_tensor("vaug0", [P, NKT, 65], BF16))
        vaug1 = E(nc.sbuf_tensor("vaug1", [P, NKT, 128], BF16))
        S.op("pool", lambda: nc.gpsimd.memset(vaug0[:, :, 64:65], 1.0), writes=["vaug0c"])
        S.op("pool", lambda: nc.gpsimd.memset(vaug1[:, :, 0:64], 0.0), writes=["vaug1c"])
        S.op("pool", lambda: nc.gpsimd.memset(vaug1[:, :, 0:1], 1.0), reads=["vaug1c"], writes=["vaug1c"])
        kmean = E(nc.sbuf_tensor("kmean", [P, 64], F32))
        S.op("dve", lambda: nc.vector.memset(kmean[:], 0.0), writes=["kmean"])
        x_st = [E(nc.sbuf_tensor(f"x_st{i}", [P, 512], F32)) for i in range(3)]
        xh = E(nc.sbuf_tensor("xh_a", [P, KD, 512], BF16))
        xsq = E(nc.sbuf_tensor("xsq_a", [P, KD, 512], BF16))
        rstd = E(nc.sbuf_tensor("rstd_a", [P, 512], F32))
        rstd_tok = E(nc.sbuf_tensor("rstd_tok", [P, 4], F32))
        fo = {nm: E(nc.sbuf_tensor("fo_" + nm, [P, 512], F32)) for nm in ("gq", "gk", "gr", "lr", "mq", "mk", "sb", "sc", "sx")}
        S.op("dve", lambda: nc.vector.memset(fo["lr"][0:32, :], 1.0), writes=["fo_lr1"])
        zc = E(nc.sbuf_tensor("zc", [P, 514], F32))
        S.op("dve", lambda: nc.vector.memset(zc[:, 0:2], 0.0), writes=["zc_h"])
        cva = E(nc.sbuf_tensor("cva", [P, 512], F32))
        gk_tok = E(nc.sbuf_tensor("gk_tok", [P, 4, 64], F32))
        gv = E(nc.sbuf_tensor("gv", [P, 4, 128], BF16))
        qaug = [E(nc.sbuf_tensor(f"qaug{h}", [P, 512], BF16)) for h in range(2)]
        mstage = E(nc.sbuf_tensor("mstage", [P, 4, P], F32))
        S.op("dve", lambda: nc.vector.memset(mstage[:], 0.0), writes=["mstage"])
        gm = E(nc.sbuf_tensor("gm", [P, 64], F32))
        top8 = E(nc.sbuf_tensor("top8", [P, 8], F32))
        thr = E(nc.sbuf_tensor("thr", [P, 1], F32))
        tsel = E(nc.sbuf_tensor("tsel", [P, 64], F32))
        pT = [E(nc.sbuf_tensor(f"pT{i}", [P, 512], BF16)) for i in range(3)]
        rs = E(nc.sbuf_tensor("rs", [P, 512], F32))
        br_sb = E(nc.sbuf_tensor("br_out", [P, 3, 512], BF16))
        la = E(nc.sbuf_tensor("la", [P, 64], F32))
        eb = E(nc.sbuf_tensor("eb", [64, P], F32))
        enb = E(nc.sbuf_tensor("enb", [64, P], F32))
        er = E(nc.sbuf_tensor("er", [P, 64], F32))
        qt_bf = E(nc.sbuf_tensor("qt_bf", [64, P], BF16))
        kt_bf = E(nc.sbuf_tensor("kt_bf", [64, P], BF16))
        kh_bf = E(nc.sbuf_tensor("kh_bf", [P, 64], BF16))
        att_bf = E(nc.sbuf_tensor("att_bf", [P, P], BF16))
        Sst = E(nc.sbuf_tensor("Sst", [64, P], F32))
        Sbf = E(nc.sbuf_tensor("Sbf", [64, P], BF16))
        S.op("dve", lambda: nc.vector.memset(Sst[:], 0.0), writes=["Sst"])
        S.op("dve", lambda: nc.vector.memset(Sbf[:], 0.0), writes=["Sbf"])
        osq = E(nc.sbuf_tensor("osq", [P, 512], BF16))
        rstd_o = rs
        ot = cva
        o_sb = cva
        B = [E(nc.psum_tensor(f"bank{i}", [P, 512], F32)) for i in range(8)]

        def prologue(I):
            t0 = I * 512
            for k in range(KD):
                xs = x_st[k % 3]
                S.dma("sp", ("x", k % 3), xs[:], xTv[:, k, t0:t0 + 512], writes=[("x_st", k % 3)])
                S.op("act", lambda: nc.scalar.activation(out=xsq[:, k, :], in_=xs[:], func=AF.Square), reads=[("x_st", k % 3)], writes=[("xsq", k)])
                S.op("dve", lambda: nc.vector.tensor_scalar(out=xh[:, k, :], in0=xs[:], scalar1=cc[:, o_gpre + k:o_gpre + k + 1], scalar2=None, op0=ALU.mult),
                     reads=[("x_st", k % 3), "cc"], writes=[("xh", k)])

        prologue(0)
        for I in range(NTILE):
            t0 = I * 512
            for k in range(KD):
                S.op("pe", lambda: nc.tensor.matmul(B[3][:, :], lhsT=ones_bf[:], rhs=xsq[:, k, :], start=(k == 0), stop=(k == KD - 1)),
                     reads=[("xsq", k), "ones_bf"], writes=["B3"], sig=(k == KD - 1))
            S.op("act", lambda: nc.scalar.activation(out=rstd[:], in_=B[3][:, :], func=AF.Ln, scale=1.0 / D, bias=epsc[:, 0:1]), reads=["B3", "epsc"], writes=["rstd"])
            S.op("act", lambda: nc.scalar.activation(out=rstd[:], in_=rstd[:], func=AF.Exp, scale=-0.5), reads=["rstd"], writes=["rstd"])
            for c in range(4):
                S.op("pe", lambda: nc.tensor.matmul(B[3][:, c:c + 1], lhsT=rstd[0:1, c * P:(c + 1) * P], rhs=ones_f[0:1, 0:1], start=True, stop=True),
                     reads=["rstd", "ones_f"], writes=["B3"])
            S.op("dve", lambda: nc.vector.tensor_copy(out=rstd_tok[:], in_=B[3][:, 0:4]), reads=["B3"], writes=["rstd_tok"])
            for gi, (nm, c0, ncol) in enumerate(FM_GROUPS):
                pb = B[gi % 2]
                pn = "B%d" % (gi % 2)
                for k in range(KD):
                    S.op("pe", lambda: nc.tensor.matmul(pb[0:ncol, :], lhsT=wf[:, k, c0:c0 + ncol], rhs=xh[:, k, :], start=(k == 0), stop=(k == KD - 1)),
                         reads=[("xh", k), "wf"], writes=[pn], sig=(k == KD - 1))
                dst = fo[nm]
                if nm in ("mq", "gq"):
                    S.op("dve", lambda: nc.vector.scalar_tensor_tensor(out=dst[0:ncol, :], in0=pb[0:ncol, :], scalar=0.125, in1=rstd[0:ncol, :], op0=ALU.mult, op1=ALU.mult),
                         reads=[pn, "rstd"], writes=["fo_" + nm])
                else:
                    extra = ["fo_lr1"] if nm == "lr" else []
                    S.op("dve", lambda: nc.vector.tensor_tensor(out=dst[0:ncol, :], in0=pb[0:ncol, :], in1=rstd[0:ncol, :], op=ALU.mult),
                         reads=[pn, "rstd"] + extra, writes=["fo_" + nm])
            S.op("act", lambda: nc.scalar.copy(out=qaug[0][0:64, :], in_=fo["mq"][0:64, :]), reads=["fo_mq"], writes=["qaug0q"])
            S.op("act", lambda: nc.scalar.copy(out=qaug[1][64:128, :], in_=fo["mq"][64:128, :]), reads=["fo_mq"], writes=["qaug1q"])
            S.op("act", lambda: nc.scalar.copy(out=kaug[0][0:64, t0:t0 + 512], in_=fo["mk"][0:64, :]), reads=["fo_mk"], writes=["kaug0k"])
            S.op("act", lambda: nc.scalar.copy(out=kaug[1][64:128, t0:t0 + 512], in_=fo["mk"][64:128, :]), reads=["fo_mk"], writes=["kaug1k"])
            S.op("dve", lambda: nc.vector.tensor_reduce(out=kmean[:, 2 * I:2 * I + 2], in_=fo["mk"][:, :].rearrange("p (b n) -> p b n", n=256), axis=AX.X, op=ALU.add),
                 reads=["fo_mk"], writes=["kmean"])
            for c in range(4):
                for k in range(KD):
                    S.op("pe", lambda: nc.tensor.matmul(B[2][:, 0:NTM], lhsT=xh[:, k, c * P:(c + 1) * P], rhs=wt[:, k, :], start=(k == 0), stop=(k == KD - 1)),
                         reads=[("xh", k), "wt"], writes=["B2"], sig=(k == KD - 1))
                sc_ = rstd_tok[:, c:c + 1]
                S.op("act", lambda: nc.scalar.activation(out=gk_tok[:, c, :], in_=B[2][:, 0:64], func=AF.Identity, scale=sc_), reads=["B2", "rstd_tok"], writes=[("gk_tok", c)])
                S.op("act", lambda: nc.scalar.activation(out=gv[:, c, :], in_=B[2][:, 64:192], func=AF.Identity, scale=sc_), reads=["B2", "rstd_tok"], writes=[("gv", c)])
                S.op("act", lambda: nc.scalar.activation(out=vaug0[:, 4 * I + c, 0:64], in_=B[2][:, 192:256], func=AF.Identity, scale=sc_), reads=["B2", "rstd_tok"], writes=["vaug0"])
                S.op("act", lambda: nc.scalar.activation(out=vaug1[:, 4 * I + c, 64:128], in_=B[2][:, 256:320], func=AF.Identity, scale=sc_), reads=["B2", "rstd_tok"], writes=["vaug1"])
            S.op("pool", lambda: nc.gpsimd.tensor_tensor(out=zc[:, 2:514], in0=fo["sc"][:, :], in1=fo["sx"][:, :], op=ALU.mult), reads=["fo_sc", "fo_sx"], writes=["zc"])
            scw = lambda j: cc[:, o_scw + j:o_scw + j + 1]
            S.op("dve", lambda: nc.vector.tensor_scalar(out=cva[:], in0=zc[:, 2:514], scalar1=scw(2), scalar2=None, op0=ALU.mult), reads=["zc", "cc"], writes=["cva"])
            S.op("dve", lambda: nc.vector.scalar_tensor_tensor(out=cva[:], in0=zc[:, 1:513], scalar=scw(1), in1=cva[:], op0=ALU.mult, op1=ALU.add), reads=["zc", "zc_h", "cc", "cva"], writes=["cva"])
            S.op("dve", lambda: nc.vector.scalar_tensor_tensor(out=cva[:], in0=zc[:, 0:512], scalar=scw(0), in1=cva[:], op0=ALU.mult, op1=ALU.add), reads=["zc", "zc_h", "cc", "cva"], writes=["cva"])
            S.op("pool", lambda: nc.gpsimd.tensor_tensor(out=br_sb[:, 2, :], in0=cva[:], in1=fo["sb"][:, :], op=ALU.mult), reads=["cva", "fo_sb"], writes=["br_c"])
            S.op("pool", lambda: nc.gpsimd.tensor_copy(out=zc[:, 0:2], in_=zc[:, 512:514]), reads=["zc", "zc_h"], writes=["zc_h"])
            S.op("act", lambda: nc.scalar.activation(out=fo["gr"][:, :], in_=fo["gr"][:, :], func=AF.Silu), reads=["fo_gr"], writes=["fo_gr"])
            G = B[4]
            for c in range(4):
                cs = slice(c * P, (c + 1) * P)
                S.op("pe", lambda: nc.tensor.matmul(B[0][:, 0:64], lhsT=fo["lr"][0:32, cs], rhs=cc[0:32, o_wlr:o_wlr + 64], start=True, stop=True),
                     reads=["fo_lr", "fo_lr1", "cc"], writes=["B0"])
                S.op("act", lambda: nc.scalar.activation(out=la[:], in_=B[0][:, 0:64], func=AF.Exp, scale=-1.0), reads=["B0"], writes=["la"])
                S.op("act", lambda: nc.scalar.activation(out=la[:], in_=la[:], func=AF.Ln, bias=1.0), reads=["la"], writes=["la"])
                S.op("pe", lambda: nc.tensor.matmul(B[1][0:64, 0:128], lhsT=la[:], rhs=U, start=True, stop=True), reads=["la", "cc"], writes=["B1"])
                S.op("pe", lambda: nc.tensor.matmul(B[2][:, 0:64], lhsT=SL, rhs=la[:], start=True, stop=True), reads=["la", "cc"], writes=["B2"])
                S.op("act", lambda: nc.scalar.activation(out=eb[:], in_=B[1][0:64, 0:128], func=AF.Exp), reads=["B1"], writes=["eb"])
                S.op("act", lambda: nc.scalar.activation(out=enb[:], in_=B[1][0:64, 0:128], func=AF.Exp, scale=-1.0), reads=["B1"], writes=["enb"])
                S.op("act", lambda: nc.scalar.activation(out=er[:], in_=B[2][:, 0:64], func=AF.Exp), reads=["B2"], writes=["er"])
                S.op("dve", lambda: nc.vector.tensor_tensor(out=qt_bf[:], in0=fo["gq"][0:64, cs], in1=eb[:], op=ALU.mult), reads=["fo_gq", "eb"], writes=["qt_bf"])
                S.op("dve", lambda: nc.vector.tensor_tensor(out=kt_bf[:], in0=fo["gk"][0:64, cs], in1=enb[:], op=ALU.mult), reads=["fo_gk", "enb"], writes=["kt_bf"])
                S.op("dve", lambda: nc.vector.tensor_tensor(out=kh_bf[:], in0=gk_tok[:, c, :], in1=er[:], op=ALU.mult), reads=[("gk_tok", c), "er"], writes=["kh_bf"])
                S.op("pe", lambda: nc.tensor.matmul(B[4][:, 0:128], lhsT=kt_bf[:], rhs=qt_bf[:], start=True, stop=True), reads=["kt_bf", "qt_bf"], writes=["B4"])
                S.op("dve", lambda: nc.vector.tensor_tensor(out=att_bf[:], in0=B[4][:, 0:128], in1=TRI, op=ALU.mult), reads=["B4", "cc"], writes=["att_bf"])
                S.op("pe", lambda: nc.tensor.matmul(B[5][:, cs], lhsT=gv[:, c, :], rhs=att_bf[:], start=True, stop=False), reads=[("gv", c), "att_bf"], writes=["B5"])
                S.op("pe", lambda: nc.tensor.matmul(B[5][:, cs], lhsT=Sbf[:], rhs=qt_bf[:], start=False, stop=True), reads=["Sbf", "qt_bf"], writes=["B5"])
                S.op("pe", lambda: nc.tensor.matmul(B[4][0:64, 128:256], lhsT=kh_bf[:], rhs=gv[:, c, :], start=True, stop=True), reads=["kh_bf", ("gv", c)], writes=["B4"])
                S.op("dve", lambda: nc.vector.scalar_tensor_tensor(out=Sst[:], in0=Sst[:], scalar=eb[:, 127:128], in1=B[4][0:64, 128:256], op0=ALU.mult, op1=ALU.add),
                     reads=["Sst", "eb", "B4"], writes=["Sst"])
                S.op("act", lambda: nc.scalar.copy(out=Sbf[:], in_=Sst[:]), reads=["Sst"], writes=["Sbf"])
            S.op("act", lambda: nc.scalar.activation(out=osq[:], in_=B[5][:, :], func=AF.Square), reads=["B5"], writes=["osq"])
            S.op("pe", lambda: nc.tensor.matmul(B[3][:, :], lhsT=ones_bf[:], rhs=osq[:], start=True, stop=True), reads=["osq", "ones_bf"], writes=["B3"])
            S.op("act", lambda: nc.scalar.activation(out=rstd_o[:], in_=B[3][:, :], func=AF.Ln, scale=1.0 / P, bias=epsc[:, 0:1]), reads=["B3", "epsc"], writes=["rs"])
            S.op("act", lambda: nc.scalar.activation(out=rstd_o[:], in_=rstd_o[:], func=AF.Exp, scale=-0.5), reads=["rs"], writes=["rs"])
            S.op("dve", lambda: nc.vector.tensor_tensor(out=ot[:], in0=B[5][:, :], in1=rstd_o[:], op=ALU.mult), reads=["B5", "rs"], writes=["cva"])
            S.op("dve", lambda: nc.vector.scalar_tensor_tensor(out=br_sb[:, 0, :], in0=ot[:], scalar=cc[:, o_gn:o_gn + 1], in1=fo["gr"][:, :], op0=ALU.mult, op1=ALU.mult),
                 reads=["cva", "cc", "fo_gr"], writes=["br_a"])
            if I + 1 < NTILE:
                prologue(I + 1)
            for h in range(2):
                hs = slice(64 * h, 64 * h + 64)
                ms = slice(64 * (1 - h), 64 * (1 - h) + 64)
                for qs in range(4):
                    qt = 4 * I + qs
                    bi = qt // 2
                    S.op("pe", lambda: nc.tensor.matmul(B[3][:, 0:64], lhsT=fo["mq"][hs, qs * P:(qs + 1) * P], rhs=kmean[hs, :], start=True, stop=True),
                         reads=["fo_mq", "kmean"], writes=["B3"])
                    S.op("dve", lambda: nc.vector.tensor_tensor(out=gm[:], in0=B[3][:, 0:64], in1=cc[:, C_FILL + 63 - bi:C_FILL + 127 - bi], op=ALU.add), reads=["B3", "cc"], writes=["gm"])
                    S.op("dve", lambda: nc.vector.max(out=top8[:], in_=gm[:]), reads=["gm"], writes=["top8"])
                    S.op("dve", lambda: nc.vector.tensor_scalar(out=thr[:], in0=top8[:, 3:4], scalar1=-1e29, scalar2=None, op0=ALU.max), reads=["top8"], writes=["thr"])
                    S.op("dve", lambda: nc.vector.tensor_scalar(out=tsel[:], in0=gm[:], scalar1=thr[:, 0:1], scalar2=BIG, op0=ALU.is_ge, op1=ALU.mult), reads=["gm", "thr"], writes=["tsel"])
                    S.op("dve", lambda: nc.vector.scalar_tensor_tensor(out=mstage[:, qs, ms], in0=tsel[:], scalar=shiftc[:, h * NKT + qt:h * NKT + qt + 1], in1=cc[:, C_A0 + 64 * h:C_A0 + 64 * h + 64], op0=ALU.add, op1=ALU.add),
                         reads=["tsel", "shiftc", "cc"], writes=[("mstage", qs)])
                for qs in range(4):
                    S.op("pe", lambda: nc.tensor.transpose(B[5][:, qs * P:(qs + 1) * P], mstage[:, qs, :], IDF), reads=[("mstage", qs), "cc"], writes=["B5"])
                S.op("act", lambda: nc.scalar.copy(out=qaug[h][ms, :], in_=B[5][ms, :]), reads=["B5"], writes=[f"qaug{h}m"])
                po = B[6 + h]
                nkt = 4 * I + 4
                def emit_s(kt):
                    ib = kt % 3
                    ps = B[ib]
                    pn = "B%d" % ib
                    diag = kt >= 4 * I
                    S.op("pe", lambda: nc.tensor.matmul(ps[:, :], lhsT=kaug[h][:, kt * P:(kt + 1) * P], rhs=qaug[h][:, :], start=True, stop=not diag),
                         reads=[f"kaug{h}k", f"kaug{h}e", f"qaug{h}q", f"qaug{h}m"], writes=[pn], sig=not diag)
                    if diag:
                        S.op("pe", lambda: nc.tensor.matmul(ps[:, :], lhsT=ident_bf[:], rhs=causneg[:, kt - 4 * I, :], start=False, stop=True),
                             reads=["ident_bf", "causneg"], writes=[pn])

                def emit_pv(kt):
                    ib = kt % 3
                    ps = B[ib]
                    pn = "B%d" % ib
                    S.op("act", lambda: nc.scalar.activation(out=pT[ib][:], in_=ps[:, :], func=AF.Exp, bias=cc[:, o_al + 2 * h + (kt % 2):o_al + 2 * h + (kt % 2) + 1]),
                         reads=[pn, "cc"], writes=[("pT", ib)])
                    if h == 0:
                        S.op("pe", lambda: nc.tensor.matmul(po[0:65, :], lhsT=vaug0[:, kt, :], rhs=pT[ib][:], start=(kt == 0), stop=(kt == nkt - 1)),
                             reads=[("pT", ib), "vaug0", "vaug0c"], writes=["B6"], sig=(kt == nkt - 1))
                    else:
                        S.op("pe", lambda: nc.tensor.matmul(po[:, :], lhsT=vaug1[:, kt, :], rhs=pT[ib][:], start=(kt == 0), stop=(kt == nkt - 1)),
                             reads=[("pT", ib), "vaug1", "vaug1c"], writes=["B7"], sig=(kt == nkt - 1))

                emit_s(0)
                if nkt > 1:
                    emit_s(1)
                for kt in range(nkt):
                    if kt + 2 < nkt:
                        emit_s(kt + 2)
                    emit_pv(kt)
                srow = 64 if h == 0 else 0
                pnm = "B%d" % (6 + h)
                S.op("dve", lambda: nc.vector.reciprocal(out=rs[srow:srow + 1, :], in_=po[srow:srow + 1, :]), reads=[pnm], writes=["rs"])
                S.op("pe", lambda: nc.tensor.matmul(B[3][:, :], lhsT=ones_f[srow:srow + 1, :], rhs=rs[srow:srow + 1, :], start=True, stop=True), reads=["rs", "ones_f"], writes=["B3"])
                S.op("act", lambda: nc.scalar.copy(out=o_sb[hs, :], in_=po[hs, :]), reads=[pnm], writes=["cva"])
                S.op("dve", lambda: nc.vector.tensor_tensor(out=br_sb[hs, 1, :], in0=o_sb[hs, :], in1=B[3][hs, :], op=ALU.mult), reads=["cva", "B3"], writes=[("br_b", h)])
            S.dma("sp", "br_st", brv[:, :, t0:t0 + 512], br_sb[:], reads=["br_a", ("br_b", 0), ("br_b", 1), "br_c"], writes=[("brT", I)])
        S.finish("sp", [("brT", I) for I in range(NTILE)])


def mixer_consts(D, T, slopes2, g_pre, wlr2_g, blr_g, gnorm, scw_g):
    KD = D // P
    NKT = T // P
    ncc = C_PP + KD + 64 + 1 + 3 + 4
    cc = np.zeros((P, ncc), np.float32)
    j = np.arange(P)[:, None]
    i = np.arange(P)[None, :]
    cc[:, C_U:C_U + P] = (j <= i) * (-1.0 / 16.0)
    cc[:, C_SL:C_SL + P] = (j > i) * (-1.0 / 16.0)
    cc[:, C_TRI:C_TRI + P] = (j <= i) * 1.0
    cc[:, C_ID:C_ID + P] = np.eye(P)
    c = np.arange(128)
    cc[:, C_FILL:C_FILL + 128] = np.where(c < 63, 0.0, np.where(c == 63, 1e30, -1e30))[None, :]
    il = np.arange(P)[:, None]
    b = np.arange(64)[None, :]
    for h in range(2):
        cc[:, C_A0 + 64 * h:C_A0 + 64 * h + 64] = -slopes2[h] * (il - 256.0 * b)
    o = C_PP
    cc[:, o:o + KD] = g_pre.reshape(KD, P).T
    o += KD
    cc[0:16, o:o + 64] = wlr2_g
    cc[16, o:o + 64] = blr_g
    o += 64
    cc[:, o] = gnorm
    o += 1
    cc[:, o:o + 3] = scw_g.T
    o += 3
    for h in range(2):
        for par in range(2):
            cc[:, o + 2 * h + par] = slopes2[h] * (128.0 * par + np.arange(P))
    shiftc = np.zeros((P, 2 * NKT), np.float32)
    for h in range(2):
        shiftc[:, h * NKT:(h + 1) * NKT] = (-BIG - slopes2[h] * 128.0 * np.arange(NKT))[None, :]
    import ml_dtypes
    jl = np.arange(P)[:, None, None]
    kk = np.arange(4)[None, :, None]
    ii = np.arange(512)[None, None, :]
    causneg = np.where(128 * kk + jl <= ii, 0.0, -BIG).astype(ml_dtypes.bfloat16)
    eblk = np.zeros((64, T), np.float32)
    for bb in range(min(64, T // 256)):
        eblk[bb, 256 * bb:256 * bb + 256] = 1.0
    return cc, shiftc, causneg, eblk.astype(ml_dtypes.bfloat16)


def build_mixer(dm):
    nc = bass.Bass("TRN2", target_bir_lowering=False)
    D, T = dm["D"], dm["T"]
    KD, NKT = D // P, T // P
    ncc = C_PP + KD + 64 + 1 + 3 + 4
    a = {
        "xT": nc.dram_tensor("xT", [D, T], F32, kind="ExternalInput").ap(),
        "w_f": nc.dram_tensor("w_f", [D, NFM], F32, kind="ExternalInput").ap(),
        "w_t": nc.dram_tensor("w_t", [D, NTM], F32, kind="ExternalInput").ap(),
        "cc": nc.dram_tensor("cc", [P, ncc], F32, kind="ExternalInput").ap(),
        "shiftc": nc.dram_tensor("shiftc", [P, 2 * NKT], F32, kind="ExternalInput").ap(),
        "causneg": nc.dram_tensor("causneg", [P, 4, 512], BF16, kind="ExternalInput").ap(),
        "eblk": nc.dram_tensor("eblk", [64, T], BF16, kind="ExternalInput").ap(),
        "brT": nc.dram_tensor("brT", [3 * P, T], BF16, kind="ExternalOutput").ap(),
    }
    with contextlib.ExitStack() as es:
        S = Sched(nc, es)
        emit_mixer(nc, S, dm, a)
        print("mixer instrs", S.ninstr, "sems", S.nsem)
    return nc


from concourse.bass_utils import run_bass_kernel_spmd
import ml_dtypes

D_MODEL, SEQ, BATCH, DEPTH, D_FF = 1024, 16384, 2, 2, 2816
ALIBI = 2.0 ** (-8.0 * (np.arange(8) + 1.0) / 8)
_NC_CACHE = {}


def _get(name, fn):
    if name not in _NC_CACHE:
        _NC_CACHE[name] = fn()
    return _NC_CACHE[name]


def _mixer_inputs(xT_b, l, g, w_in, gla_w_lr2, gla_b_lr, gla_norm, sc_conv_w, g_mix_pre):
    W = w_in[l]
    sl = lambda o, n: W[:, o + n * g: o + n * g + n]
    w_f = np.ascontiguousarray(np.concatenate([sl(0, 64), sl(256, 64), sl(1024, 128), W[:, 1536:1552], sl(1552, 128), sl(2064, 128),
                                               sl(3088, 128), sl(3600, 128), sl(4112, 128)], axis=1))
    w_t = np.ascontiguousarray(np.concatenate([sl(256, 64), sl(512, 128), sl(2576, 128)], axis=1))
    cc, shiftc, causneg, eblk = mixer_consts(D_MODEL, SEQ, ALIBI[2 * g:2 * g + 2], g_mix_pre[l], gla_w_lr2[l][:, 64 * g:64 * g + 64],
                                             gla_b_lr[l][64 * g:64 * g + 64], gla_norm[l], sc_conv_w[l][:, 128 * g:128 * g + 128])
    return {"xT": xT_b, "w_f": w_f, "w_t": w_t, "cc": cc, "shiftc": shiftc, "causneg": causneg, "eblk": eblk}


def kernel(x, g_mix_pre, w_in, gla_w_lr2, gla_b_lr, gla_norm, sc_conv_w, w_br_gla, w_br_moba, w_br_sc, w_out, g_mix_post,
           g_ffn_pre, ffn_w_up, ffn_conv_w, ffn_conv_b, ffn_w_down, g_ffn_post):
    f32 = lambda v: np.ascontiguousarray(np.asarray(v, dtype=np.float32))
    (x, g_mix_pre, w_in, gla_w_lr2, gla_b_lr, gla_norm, sc_conv_w, w_br_gla, w_br_moba, w_br_sc, w_out, g_mix_post,
     g_ffn_pre, ffn_w_up, ffn_conv_w, ffn_conv_b, ffn_w_down, g_ffn_post) = map(f32, (
        x, g_mix_pre, w_in, gla_w_lr2, gla_b_lr, gla_norm, sc_conv_w, w_br_gla, w_br_moba, w_br_sc, w_out, g_mix_post,
        g_ffn_pre, ffn_w_up, ffn_conv_w, ffn_conv_b, ffn_w_down, g_ffn_post))
    NTOK = SEQ // 4
    nc_a = _get("mixer", lambda: build_mixer(dict(D=D_MODEL, T=SEQ)))
    nc_b = _get("dense", lambda: build_dense(dict(D=D_MODEL, DFF=D_FF, BW=1536, NTOK=NTOK)))
    xT = [np.ascontiguousarray(x[b].T) for b in range(BATCH)]
    for l in range(DEPTH):
        in_maps = [_mixer_inputs(xT[c // 4], l, c % 4, w_in, gla_w_lr2, gla_b_lr, gla_norm, sc_conv_w, g_mix_pre) for c in range(8)]
        res = run_bass_kernel_spmd(nc_a, in_maps, core_ids=list(range(8)))
        brT = []
        for b in range(BATCH):
            parts = [np.asarray(res.results[4 * b + g]["brT"]) for g in range(4)]
            brT.append(np.concatenate([p[0:128] for p in parts] + [p[128:256] for p in parts] + [p[256:384] for p in parts], axis=0))
        w_gate = np.ascontiguousarray(w_in[l][:, 4624:7696])
        w_br = np.ascontiguousarray(np.concatenate([w_br_gla[l], w_br_moba[l], w_br_sc[l]], axis=0))
        pp = pack_params_dense(g_mix_pre[l], g_mix_post[l], g_ffn_pre[l], g_ffn_post[l], ffn_conv_w[l], ffn_conv_b[l])
        in_maps = []
        for c in range(8):
            b, j = c // 4, c % 4
            t0 = j * NTOK
            xs = np.zeros((D_MODEL, NTOK + 2), np.float32)
            bs = np.zeros((1536, NTOK + 2), ml_dtypes.bfloat16)
            lo = max(t0 - 2, 0)
            xs[:, 2 - (t0 - lo):] = xT[b][:, lo:t0 + NTOK]
            bs[:, 2 - (t0 - lo):] = brT[b][:, lo:t0 + NTOK]
            in_maps.append({"xT": xs, "brT": bs, "w_gate": w_gate, "w_br": w_br, "w_out": w_out[l], "w_up": ffn_w_up[l],
                            "w_down": ffn_w_down[l], "pp": pp})
        res = run_bass_kernel_spmd(nc_b, in_maps, core_ids=list(range(8)))
        xT = [np.ascontiguousarray(np.concatenate([np.asarray(res.results[4 * b + j]["yT"]) for j in range(4)], axis=1)) for b in range(BATCH)]
    return np.ascontiguousarray(np.stack([xT[b].T for b in range(BATCH)], axis=0).astype(np.float32))
```

```python
import concourse.bass as bass
import concourse.mybir as mybir

F32 = mybir.dt.float32
BF16 = mybir.dt.bfloat16
AF = mybir.ActivationFunctionType
ALU = mybir.AluOpType
AX = mybir.AxisListType

SEM_EPOCH = 40000


class Sched:
    def __init__(self, nc, es, same_engine_sync=True):
        self.nc = nc
        self.es = es
        self.same = same_engine_sync
        self.eng = {"pe": nc.tensor, "act": nc.scalar, "dve": nc.vector, "pool": nc.gpsimd, "sp": nc.sync}
        self.sem = {}
        self.cnt = {}
        self.nsem = 0
        for e in self.eng:
            self._new_sem(e)
        self.waited = {e: {} for e in self.eng}
        self.lastw = {}
        self.readers = {}
        self.dma_sems = {}
        self.ninstr = 0
        self.pending = {}

    def _new_sem(self, e):
        self.nsem += 1
        self.sem[e] = self.es.enter_context(self.nc.semaphore(f"s_{e}_{self.nsem}"))
        self.cnt[e] = 0

    def _wait(self, e, tok):
        if tok is None:
            return
        sem, val, src = tok
        if src == e and (not self.same or e == "pe"):
            return
        w = self.waited[e]
        k = id(sem)
        if w.get(k, 0) >= val:
            return
        self.eng[e].wait_ge(sem, val)
        w[k] = val

    def _deps(self, e, reads, writes):
        for r in reads:
            self._wait(e, self.lastw.get(r))
        for r in writes:
            self._wait(e, self.lastw.get(r))
            for t in list(self.readers.get(r, {}).values()):
                self._wait(e, t)

    def _commit(self, tok, reads, writes):
        for r in reads:
            self.readers.setdefault(r, {})[(tok[2], id(tok[0]))] = tok
        for r in writes:
            self.lastw[r] = tok
            self.readers[r] = {}

    def op(self, e, fn, reads=(), writes=(), sig=True):
        self._deps(e, reads, writes)
        if self.cnt[e] >= SEM_EPOCH and not self.pending.get(e):
            self._new_sem(e)
        ins = fn()
        if sig:
            self.cnt[e] += 1
            ins.then_inc(self.sem[e], 1)
            tok = (self.sem[e], self.cnt[e], e)
            self.pending[e] = False
        else:
            tok = (self.sem[e], self.cnt[e] + 1, e)
            self.pending[e] = True
        self._commit(tok, reads, writes)
        self.ninstr += 1
        return tok

    def dma(self, q, key, out, in_, reads=(), writes=()):
        self._deps(q, reads, writes)
        st = self.dma_sems.get(key)
        if st is None or st[1] >= SEM_EPOCH:
            self.nsem += 1
            st = [self.es.enter_context(self.nc.semaphore(f"d_{self.nsem}")), 0]
            self.dma_sems[key] = st
        ins = self.eng[q].dma_start(out=out, in_=in_)
        st[1] += 16
        ins.then_inc(st[0], 16)
        tok = (st[0], st[1], "dma")
        self._commit(tok, reads, writes)
        self.ninstr += 1
        return tok

    def finish(self, e, resources):
        for r in resources:
            self._wait(e, self.lastw.get(r))

    def barrier(self):
        toks = [(self.sem[e], self.cnt[e], e) for e in self.eng if self.cnt[e] > 0]
        toks += [(st[0], st[1], "dma") for st in self.dma_sems.values()]
        for e in self.eng:
            for t in toks:
                self._wait(e, t)

import contextlib
import numpy as np

P = 128
EPS = 1e-6


def split_even(n, maxn):
    k = -(-n // maxn)
    base, rem = divmod(n, k)
    out, s = [], 0
    for i in range(k):
        sz = base + (1 if i < rem else 0)
        out.append((s, sz))
        s += sz
    return out


def load_w(S, nc, wt, wd, nk, qname="pool", key="w"):
    v = wd.rearrange("(k p) n -> p k n", p=P)
    n = wd.shape[1]
    step = max(1, 4096 // n)
    for k0 in range(0, nk, step):
        k1 = min(nk, k0 + step)
        S.dma(qname, (key, k0 % 4), wt[:, k0:k1, :], v[:, k0:k1, :], writes=[(key, k0)])
    return [(key, k0) for k0 in range(0, nk, step)]


def rstd_from_psum(S, nc, out, ps, D, eps_col, reads, wres):
    S.op("act", lambda: nc.scalar.activation(out=out, in_=ps, func=AF.Ln, scale=1.0 / D, bias=eps_col), reads=reads, writes=[wres])
    S.op("act", lambda: nc.scalar.activation(out=out, in_=out, func=AF.Exp, scale=-0.5), reads=[wres], writes=[wres])


def emit_dense(nc, S, dm, a, n1max=456, n2max=410):
    D, DFF, BW, NTOK = dm["D"], dm["DFF"], dm["BW"], dm["NTOK"]
    KD, KB, KF = D // P, BW // P, DFF // P
    KBB = KB // 3
    NT = NTOK + 2
    o_gpre, o_gpost, o_gfpre, o_gfpost = 0, KD, 2 * KD, 3 * KD
    o_cw = 4 * KD
    o_cb = o_cw + 3 * 2 * KF
    npar = o_cb + 2 * KF
    xTv = a["xT"].rearrange("(k p) n -> p k n", p=P)
    brTv = a["brT"].rearrange("(k p) n -> p k n", p=P)
    xmv = a["xmid"].rearrange("(k p) n -> p k n", p=P)
    yTv = a["yT"].rearrange("(k p) n -> p k n", p=P)

    with contextlib.ExitStack() as es0:
        E0 = es0.enter_context
        pp = E0(nc.sbuf_tensor("pp_sb", [P, npar], F32))
        ones = E0(nc.sbuf_tensor("ones", [P, P], BF16))
        epsc = E0(nc.sbuf_tensor("epsc", [P, 1], F32))
        S.dma("sp", "pp", pp[:], a["pp"][:, :], writes=["pp"])
        S.op("dve", lambda: nc.vector.memset(ones[:], 1.0), writes=["ones"])
        S.op("dve", lambda: nc.vector.memset(epsc[:], EPS), writes=["epsc"])
        d1_tiles = split_even(NT, n1max)
        N1 = max(n for _, n in d1_tiles)
        with contextlib.ExitStack() as es:
            E = es.enter_context
            wg = E(nc.sbuf_tensor("wg", [P, KD, 3 * D], BF16))
            wbr = E(nc.sbuf_tensor("wbr", [P, KB, D], BF16))
            wo = E(nc.sbuf_tensor("wo", [P, KD, D], BF16))
            r_wg = load_w(S, nc, wg, a["w_gate"], KD, key="wg")
            r_wbr = load_w(S, nc, wbr, a["w_br"], KB, key="wbr")
            r_wo = load_w(S, nc, wo, a["w_out"], KD, key="wo")
            x_sb = E(nc.sbuf_tensor("x_sb", [P, KD, N1], F32))
            br_sb = E(nc.sbuf_tensor("br_sb", [P, KB, N1], BF16))
            xsq = E(nc.sbuf_tensor("xsq", [P, KD, N1], BF16))
            xh = E(nc.sbuf_tensor("xh", [P, KD, N1], BF16))
            rstd = E(nc.sbuf_tensor("rstd", [P, N1], F32))
            rstd2 = E(nc.sbuf_tensor("rstd2", [P, N1], F32))
            gl = [E(nc.sbuf_tensor(f"gl{i}", [P, N1], F32)) for i in range(3)]
            tm = [E(nc.sbuf_tensor(f"tm{i}", [P, N1], F32)) for i in range(3)]
            mf = [E(nc.sbuf_tensor(f"mf{i}", [P, N1], F32)) for i in range(2)]
            mbf = E(nc.sbuf_tensor("mbf", [P, KD, N1], BF16))
            z_sb = E(nc.sbuf_tensor("z_sb", [P, KD, N1], F32))
            ps_st = E(nc.psum_tensor("ps_st", [P, 512], F32))
            ps_g = [E(nc.psum_tensor(f"ps_g{i}", [P, 512], F32)) for i in range(3)]
            ps_p = [E(nc.psum_tensor(f"ps_p{i}", [P, 512], F32)) for i in range(3)]
            ps_z = E(nc.psum_tensor("ps_z", [P, 512], F32))
            it = 0
            for ti, (c0, N) in enumerate(d1_tiles):
                S.dma("sp", "x", x_sb[:, :, :N], xTv[:, :, c0:c0 + N], writes=["x_sb"])
                S.dma("sp", "br", br_sb[:, :, :N], brTv[:, :, c0:c0 + N], writes=["br_sb"])
                S.op("act", lambda: nc.scalar.activation(out=xsq[:, :, :N], in_=x_sb[:, :, :N], func=AF.Square), reads=["x_sb"], writes=["xsq"])
                for k in range(KD):
                    S.op("act", lambda: nc.scalar.activation(out=xh[:, k, :N], in_=x_sb[:, k, :N], func=AF.Identity, scale=pp[:, o_gpre + k:o_gpre + k + 1]),
                         reads=["x_sb", "pp"], writes=[("xh", k)])
                for k in range(KD):
                    S.op("pe", lambda: nc.tensor.matmul(ps_st[:, :N], lhsT=ones[:], rhs=xsq[:, k, :N], start=(k == 0), stop=(k == KD - 1)),
                         reads=["xsq", "ones"], writes=["ps_st"], sig=(k == KD - 1))
                rstd_from_psum(S, nc, rstd[:, :N], ps_st[:, :N], D, epsc[:, 0:1], ["ps_st", "epsc"], "rstd")
                def d1_head(m, b, i3):
                    pg, pq, g_t = ps_g[i3], ps_p[i3], gl[i3]
                    for k in range(KD):
                        S.op("pe", lambda: nc.tensor.matmul(pg[:, :N], lhsT=wg[:, k, b * D + m * P:b * D + (m + 1) * P], rhs=xh[:, k, :N], start=(k == 0), stop=(k == KD - 1)),
                             reads=[("xh", k)] + r_wg, writes=[("pg", i3)], sig=(k == KD - 1))
                    for k in range(KBB):
                        S.op("pe", lambda: nc.tensor.matmul(pq[:, :N], lhsT=wbr[:, b * KBB + k, m * P:(m + 1) * P], rhs=br_sb[:, b * KBB + k, :N], start=(k == 0), stop=(k == KBB - 1)),
                             reads=["br_sb"] + r_wbr, writes=[("pq", i3)], sig=(k == KBB - 1))
                    S.op("dve", lambda: nc.vector.tensor_tensor(out=g_t[:, :N], in0=pg[:, :N], in1=rstd[:, :N], op=ALU.mult),
                         reads=[("pg", i3), "rstd"], writes=[("gl", i3)])
                    S.op("act", lambda: nc.scalar.activation(out=g_t[:, :N], in_=g_t[:, :N], func=AF.Sigmoid), reads=[("gl", i3)], writes=[("gl", i3)])

                def d1_tail(m, b, i3):
                    pq, g_t, t_t = ps_p[i3], gl[i3], tm[i3]
                    mfm = mf[m % 2]
                    if b == 0:
                        S.op("dve", lambda: nc.vector.tensor_tensor(out=mfm[:, :N], in0=g_t[:, :N], in1=pq[:, :N], op=ALU.mult),
                             reads=[("gl", i3), ("pq", i3)], writes=[("mf", m % 2)])
                    else:
                        S.op("dve", lambda: nc.vector.tensor_tensor(out=t_t[:, :N], in0=g_t[:, :N], in1=pq[:, :N], op=ALU.mult),
                             reads=[("gl", i3), ("pq", i3)], writes=[("tm", i3)])
                        dst = mfm[:, :N] if b == 1 else mbf[:, m, :N]
                        wr = [("mf", m % 2)] if b == 1 else [("mbf", m)]
                        S.op("pool", lambda: nc.gpsimd.tensor_tensor(out=dst, in0=mfm[:, :N], in1=t_t[:, :N], op=ALU.add),
                             reads=[("mf", m % 2), ("tm", i3)], writes=wr)

                groups = [(m, b) for m in range(KD) for b in range(3)]
                prev = None
                for (m, b) in groups:
                    i3 = it % 3
                    it += 1
                    d1_head(m, b, i3)
                    if prev is not None:
                        d1_tail(*prev)
                    prev = (m, b, i3)
                d1_tail(*prev)
                for m in range(KD):
                    for k in range(KD):
                        S.op("pe", lambda: nc.tensor.matmul(ps_z[:, :N], lhsT=wo[:, k, m * P:(m + 1) * P], rhs=mbf[:, k, :N], start=(k == 0), stop=(k == KD - 1)),
                             reads=[("mbf", k)] + r_wo, writes=["ps_z"], sig=(k == KD - 1))
                    S.op("act", lambda: nc.scalar.copy(out=z_sb[:, m, :N], in_=ps_z[:, :N]), reads=["ps_z"], writes=[("z", m)])
                    S.op("pool", lambda: nc.gpsimd.tensor_tensor(out=xsq[:, m, :N], in0=z_sb[:, m, :N], in1=z_sb[:, m, :N], op=ALU.mult), reads=[("z", m)], writes=["xsq"])
                for k in range(KD):
                    S.op("pe", lambda: nc.tensor.matmul(ps_st[:, :N], lhsT=ones[:], rhs=xsq[:, k, :N], start=(k == 0), stop=(k == KD - 1)),
                         reads=["xsq", "ones"], writes=["ps_st"], sig=(k == KD - 1))
                rstd_from_psum(S, nc, rstd2[:, :N], ps_st[:, :N], D, epsc[:, 0:1], ["ps_st", "epsc"], "rstd2")
                for m in range(KD):
                    S.op("dve", lambda: nc.vector.scalar_tensor_tensor(out=z_sb[:, m, :N], in0=z_sb[:, m, :N], scalar=pp[:, o_gpost + m:o_gpost + m + 1], in1=rstd2[:, :N], op0=ALU.mult, op1=ALU.mult),
                         reads=[("z", m), "rstd2", "pp"], writes=[("z", m)])
                    S.op("pool", lambda: nc.gpsimd.tensor_tensor(out=z_sb[:, m, :N], in0=z_sb[:, m, :N], in1=x_sb[:, m, :N], op=ALU.add),
                         reads=[("z", m), "x_sb"], writes=[("z", m)])
                S.dma("sp", "xm_st", xmv[:, :, c0:c0 + N], z_sb[:, :, :N], reads=[("z", m) for m in range(KD)], writes=[("xmid", ti)])
        S.barrier()
        d2_tiles = split_even(NTOK, n2max)
        N2 = max(n for _, n in d2_tiles)
        with contextlib.ExitStack() as es:
            E = es.enter_context
            wup = E(nc.sbuf_tensor("wup", [P, KD, 2 * DFF], BF16))
            wdn = E(nc.sbuf_tensor("wdn", [P, KF, D], BF16))
            r_wup = load_w(S, nc, wup, a["w_up"], KD, key="wup")
            r_wdn = load_w(S, nc, wdn, a["w_down"], KF, key="wdn")
            xm = E(nc.sbuf_tensor("xm", [P, KD, N2 + 2], F32))
            xsq = E(nc.sbuf_tensor("xsq2", [P, KD, N2 + 2], BF16))
            hn = E(nc.sbuf_tensor("hn", [P, KD, N2 + 2], BF16))
            rstd = E(nc.sbuf_tensor("rstd_b", [P, N2 + 2], F32))
            rstd3 = E(nc.sbuf_tensor("rstd3", [P, N2 + 2], F32))
            a_sb = E(nc.sbuf_tensor("a_sb", [P, KF, N2], BF16))
            cvg = [E(nc.sbuf_tensor(f"cvg{i}", [P, N2], F32)) for i in range(3)]
            cvu = [E(nc.sbuf_tensor(f"cvu{i}", [P, N2], F32)) for i in range(3)]
            y_sb = E(nc.sbuf_tensor("y_sb", [P, KD, N2], F32))
            ps_st = E(nc.psum_tensor("ps_st2", [P, 512], F32))
            ps_ug = [E(nc.psum_tensor(f"ps_ug{i}", [P, 512], F32)) for i in range(3)]
            ps_uu = [E(nc.psum_tensor(f"ps_uu{i}", [P, 512], F32)) for i in range(3)]
            ps_y = [E(nc.psum_tensor(f"ps_y{i}", [P, 512], F32)) for i in range(1)]
            all_xmid = [("xmid", ti) for ti in range(len(d1_tiles))]
            for ti, (o0, N) in enumerate(d2_tiles):
                M = N + 2
                S.dma("sp", "xm", xm[:, :, :M], xmv[:, :, o0:o0 + M], reads=all_xmid, writes=["xm"])
                S.op("act", lambda: nc.scalar.activation(out=xsq[:, :, :M], in_=xm[:, :, :M], func=AF.Square), reads=["xm"], writes=["xsq2"])
                for k in range(KD):
                    S.op("pe", lambda: nc.tensor.matmul(ps_st[:, :M], lhsT=ones[:], rhs=xsq[:, k, :M], start=(k == 0), stop=(k == KD - 1)),
                         reads=["xsq2", "ones"], writes=["ps_st2"], sig=(k == KD - 1))
                rstd_from_psum(S, nc, rstd[:, :M], ps_st[:, :M], D, epsc[:, 0:1], ["ps_st2", "epsc"], "rstd_b")
                for k in range(KD):
                    S.op("dve", lambda: nc.vector.scalar_tensor_tensor(out=hn[:, k, :M], in0=xm[:, k, :M], scalar=pp[:, o_gfpre + k:o_gfpre + k + 1], in1=rstd[:, :M], op0=ALU.mult, op1=ALU.mult),
                         reads=["xm", "rstd_b", "pp"], writes=[("hn", k)])
                def d2_head(c):
                    i2 = c % 3
                    for half, pst, cv, col, nm in ((0, ps_ug[i2], cvg[i2], c, "ug"), (1, ps_uu[i2], cvu[i2], KF + c, "uu")):
                        for k in range(KD):
                            S.op("pe", lambda: nc.tensor.matmul(pst[:, :M], lhsT=wup[:, k, col * P:(col + 1) * P], rhs=hn[:, k, :M], start=(k == 0), stop=(k == KD - 1)),
                                 reads=[("hn", k)] + r_wup, writes=[(nm, i2)], sig=(k == KD - 1))
                        cw = lambda j: pp[:, o_cw + j * 2 * KF + col:o_cw + j * 2 * KF + col + 1]
                        S.op("act", lambda: nc.scalar.activation(out=cv[:, :N], in_=pst[:, 2:N + 2], func=AF.Identity, scale=cw(2), bias=pp[:, o_cb + col:o_cb + col + 1]),
                             reads=[(nm, i2), "pp"], writes=[("cv" + nm, i2)])
                        S.op("dve", lambda: nc.vector.scalar_tensor_tensor(out=cv[:, :N], in0=pst[:, 1:N + 1], scalar=cw(1), in1=cv[:, :N], op0=ALU.mult, op1=ALU.add),
                             reads=[(nm, i2), "pp", ("cv" + nm, i2)], writes=[("cv" + nm, i2)])
                        S.op("dve", lambda: nc.vector.scalar_tensor_tensor(out=cv[:, :N], in0=pst[:, 0:N], scalar=cw(0), in1=cv[:, :N], op0=ALU.mult, op1=ALU.add),
                             reads=[(nm, i2), "pp", ("cv" + nm, i2)], writes=[("cv" + nm, i2)])

                def d2_tail(c):
                    i2 = c % 3
                    S.op("pool", lambda: nc.gpsimd.tensor_tensor(out=cvu[i2][:, :N], in0=cvu[i2][:, :N], in1=cvg[i2][:, :N], op=ALU.mult),
                         reads=[("cvug", i2), ("cvuu", i2)], writes=[("cvuu", i2)])
                    S.op("act", lambda: nc.scalar.activation(out=cvg[i2][:, :N], in_=cvg[i2][:, :N], func=AF.Sigmoid), reads=[("cvug", i2)], writes=[("cvug", i2)])
                    S.op("pool", lambda: nc.gpsimd.tensor_tensor(out=a_sb[:, c, :N], in0=cvu[i2][:, :N], in1=cvg[i2][:, :N], op=ALU.mult),
                         reads=[("cvug", i2), ("cvuu", i2)], writes=[("a", c)])

                for c in range(KF + 1):
                    if c < KF:
                        d2_head(c)
                    if c >= 1:
                        d2_tail(c - 1)
                for m in range(KD):
                    py = ps_y[0]
                    for c in range(KF):
                        S.op("pe", lambda: nc.tensor.matmul(py[:, :N], lhsT=wdn[:, c, m * P:(m + 1) * P], rhs=a_sb[:, c, :N], start=(c == 0), stop=(c == KF - 1)),
                             reads=[("a", c)] + r_wdn, writes=[("py", 0)], sig=(c == KF - 1))
                    S.op("act", lambda: nc.scalar.copy(out=y_sb[:, m, :N], in_=py[:, :N]), reads=[("py", 0)], writes=[("y", m)])
                    S.op("pool", lambda: nc.gpsimd.tensor_tensor(out=xsq[:, m, :N], in0=y_sb[:, m, :N], in1=y_sb[:, m, :N], op=ALU.mult), reads=[("y", m)], writes=["xsq2"])
                for k in range(KD):
                    S.op("pe", lambda: nc.tensor.matmul(ps_st[:, :N], lhsT=ones[:], rhs=xsq[:, k, :N], start=(k == 0), stop=(k == KD - 1)),
                         reads=["xsq2", "ones"], writes=["ps_st2"], sig=(k == KD - 1))
                rstd_from_psum(S, nc, rstd3[:, :N], ps_st[:, :N], D, epsc[:, 0:1], ["ps_st2", "epsc"], "rstd3")
                for m in range(KD):
                    S.op("dve", lambda: nc.vector.scalar_tensor_tensor(out=y_sb[:, m, :N], in0=y_sb[:, m, :N], scalar=pp[:, o_gfpost + m:o_gfpost + m + 1], in1=rstd3[:, :N], op0=ALU.mult, op1=ALU.mult),
                         reads=[("y", m), "rstd3", "pp"], writes=[("y", m)])
                    S.op("pool", lambda: nc.gpsimd.tensor_tensor(out=y_sb[:, m, :N], in0=y_sb[:, m, :N], in1=xm[:, m, 2:N + 2], op=ALU.add),
                         reads=[("y", m), "xm"], writes=[("y", m)])
                S.dma("sp", "y_st", yTv[:, :, o0:o0 + N], y_sb[:, :, :N], reads=[("y", m) for m in range(KD)], writes=[("yT", ti)])
            S.finish("sp", [("yT", ti) for ti in range(len(d2_tiles))])


def pack_params_dense(g_pre, g_post, g_fpre, g_fpost, conv_w, conv_b):
    def col(v):
        return np.ascontiguousarray(v.reshape(-1, P).T)
    cw = np.concatenate([col(conv_w[j]) for j in range(3)], axis=1)
    return np.ascontiguousarray(np.concatenate([col(g_pre), col(g_post), col(g_fpre), col(g_fpost), cw, col(conv_b)], axis=1).astype(np.float32))


def build_dense(dm):
    nc = bass.Bass("TRN2", target_bir_lowering=False)
    D, DFF, BW, NTOK = dm["D"], dm["DFF"], dm["BW"], dm["NTOK"]
    KD, KF = D // P, DFF // P
    npar = 4 * KD + 8 * KF
    a = {
        "xT": nc.dram_tensor("xT", [D, NTOK + 2], F32, kind="ExternalInput").ap(),
        "brT": nc.dram_tensor("brT", [BW, NTOK + 2], BF16, kind="ExternalInput").ap(),
        "w_gate": nc.dram_tensor("w_gate", [D, 3 * D], F32, kind="ExternalInput").ap(),
        "w_br": nc.dram_tensor("w_br", [BW, D], F32, kind="ExternalInput").ap(),
        "w_out": nc.dram_tensor("w_out", [D, D], F32, kind="ExternalInput").ap(),
        "w_up": nc.dram_tensor("w_up", [D, 2 * DFF], F32, kind="ExternalInput").ap(),
        "w_down": nc.dram_tensor("w_down", [DFF, D], F32, kind="ExternalInput").ap(),
        "pp": nc.dram_tensor("pp", [P, npar], F32, kind="ExternalInput").ap(),
        "xmid": nc.dram_tensor("xmid", [D, NTOK + 2], F32).ap(),
        "yT": nc.dram_tensor("yT", [D, NTOK], F32, kind="ExternalOutput").ap(),
    }
    with contextlib.ExitStack() as es:
        S = Sched(nc, es)
        emit_dense(nc, S, dm, a)
        print("dense instrs", S.ninstr, "sems", S.nsem)
    return nc

import contextlib
import numpy as np

P = 128
EPS = 1e-6
BIG = 30000.0
NFM = 912
NTM = 320
FM_GROUPS = [("gq", 0, 64), ("gk", 64, 64), ("gr", 128, 128), ("lr", 256, 16), ("mq", 272, 128), ("mk", 400, 128),
             ("sb", 528, 128), ("sc", 656, 128), ("sx", 784, 128)]
C_U, C_SL, C_TRI, C_ID, C_FILL, C_A0, C_PP = 0, 128, 256, 384, 512, 640, 768
NCONST = 768


def emit_mixer(nc, S, dm, a):
    D, T = dm["D"], dm["T"]
    KD = D // P
    NTILE = T // 512
    NKT = T // P
    o_gpre = C_PP
    o_wlr = o_gpre + KD
    o_gn = o_wlr + 64
    o_scw = o_gn + 1
    o_al = o_scw + 3
    ncc = o_al + 4
    xTv = a["xT"].rearrange("(k p) n -> p k n", p=P)
    brv = a["brT"].rearrange("(k p) n -> p k n", p=P)

    with contextlib.ExitStack() as es:
        E = es.enter_context
        cc = E(nc.sbuf_tensor("cc_sb", [P, ncc], F32))
        S.dma("sp", "cc", cc[:], a["cc"][:, :], writes=["cc"])
        shiftc = E(nc.sbuf_tensor("shiftc_sb", [P, 2 * NKT], F32))
        S.dma("sp", "shiftc", shiftc[:], a["shiftc"][:, :], writes=["shiftc"])
        causneg = E(nc.sbuf_tensor("causneg_sb", [P, 4, 512], BF16))
        S.dma("sp", "causneg", causneg[:], a["causneg"][:, :, :], writes=["causneg"])
        ident_bf = E(nc.sbuf_tensor("ident_bf", [P, P], BF16))
        ones_bf = E(nc.sbuf_tensor("ones_bf", [P, P], BF16))
        ones_f = E(nc.sbuf_tensor("ones_f", [P, P], F32))
        epsc = E(nc.sbuf_tensor("epsc_a", [P, 1], F32))
        S.op("dve", lambda: nc.vector.memset(ones_bf[:], 1.0), writes=["ones_bf"])
        S.op("dve", lambda: nc.vector.memset(ones_f[:], 1.0), writes=["ones_f"])
        S.op("dve", lambda: nc.vector.memset(epsc[:], EPS), writes=["epsc"])
        S.op("dve", lambda: nc.vector.tensor_copy(out=ident_bf[:], in_=cc[:, C_ID:C_ID + P]), reads=["cc"], writes=["ident_bf"])
        U = cc[:, C_U:C_U + P]
        SL = cc[:, C_SL:C_SL + P]
        TRI = cc[:, C_TRI:C_TRI + P]
        IDF = cc[:, C_ID:C_ID + P]
        wf = E(nc.sbuf_tensor("wf", [P, KD, NFM], BF16))
        wt = E(nc.sbuf_tensor("wt", [P, KD, NTM], BF16))
        wfv = a["w_f"].rearrange("(k p) n -> p k n", p=P)
        wtv = a["w_t"].rearrange("(k p) n -> p k n", p=P)
        for k0 in range(0, KD, 4):
            S.dma("pool", ("wf", k0), wf[:, k0:min(KD, k0 + 4), :], wfv[:, k0:min(KD, k0 + 4), :], writes=["wf"])
        S.dma("pool", "wt", wt[:], wtv[:, :, :], writes=["wt"])
        kaug = [E(nc.sbuf_tensor(f"kaug{h}", [P, T], BF16)) for h in range(2)]
        S.dma("sp", "kaug0", kaug[0][64:128, :], a["eblk"][:, :], writes=["kaug0e"])
        S.dma("sp", "kaug1", kaug[1][0:64, :], a["eblk"][:, :], writes=["kaug1e"])
        vaug0 = E(nc.sbuf_tensor("vaug0", [P, NKT, 65], BF16))
        vaug1 = E(nc.sbuf_tensor("vaug1", [P, NKT, 128], BF16))
        S.op("pool", lambda: nc.gpsimd.memset(vaug0[:, :, 64:65], 1.0), writes=["vaug0c"])
        S.op("pool", lambda: nc.gpsimd.memset(vaug1[:, :, 0:64], 0.0), writes=["vaug1c"])
        S.op("pool", lambda: nc.gpsimd.memset(vaug1[:, :, 0:1], 1.0), reads=["vaug1c"], writes=["vaug1c"])
        kmean = E(nc.sbuf_tensor("kmean", [P, 64], F32))
        S.op("dve", lambda: nc.vector.memset(kmean[:], 0.0), writes=["kmean"])
        x_st = [E(nc.sbuf_tensor(f"x_st{i}", [P, 512], F32)) for i in range(2)]
        xh = E(nc.sbuf_tensor("xh_a", [P, KD, 512], BF16))
        xsq = E(nc.sbuf_tensor("xsq_a", [P, KD, 512], BF16))
        rstd = E(nc.sbuf_tensor("rstd_a", [P, 512], F32))
        rstd_tok = E(nc.sbuf_tensor("rstd_tok", [P, 4], F32))
        fo = {nm: E(nc.sbuf_tensor("fo_" + nm, [P, 512], F32)) for nm in ("gq", "gk", "gr", "lr", "mq", "mk", "sb", "sc", "sx")}
        S.op("dve", lambda: nc.vector.memset(fo["lr"][0:32, :], 1.0), writes=["fo_lr1"])
        zc = E(nc.sbuf_tensor("zc", [P, 514], F32))
        S.op("dve", lambda: nc.vector.memset(zc[:, 0:2], 0.0), writes=["zc_h"])
        cva = E(nc.sbuf_tensor("cva", [P, 512], F32))
        gk_tok = E(nc.sbuf_tensor("gk_tok", [P, 4, 64], F32))
        gv = E(nc.sbuf_tensor("gv", [P, 4, 128], BF16))
        qaug = [E(nc.sbuf_tensor(f"qaug{h}", [P, 512], BF16)) for h in range(2)]
        mstage = E(nc.sbuf_tensor("mstage", [P, 4, P], F32))
        S.op("dve", lambda: nc.vector.memset(mstage[:], 0.0), writes=["mstage"])
        gm = E(nc.sbuf_tensor("gm", [P, 64], F32))
        top8 = E(nc.sbuf_tensor("top8", [P, 8], F32))
        thr = E(nc.sbuf_tensor("thr", [P, 1], F32))
        tsel = E(nc.sbuf_tensor("tsel", [P, 64], F32))
        pT = [E(nc.sbuf_tensor(f"pT{i}", [P, 512], BF16)) for i in range(3)]
        rs = E(nc.sbuf_tensor("rs", [P, 512], F32))
        br_sb = E(nc.sbuf_tensor("br_out", [P, 3, 512], BF16))
        la = E(nc.sbuf_tensor("la", [P, 4, 64], F32))
        er = E(nc.sbuf_tensor("er", [P, 4, 64], F32))
        qt_bf = E(nc.sbuf_tensor("qt_bf", [64, 512], BF16))
        kt_bf = E(nc.sbuf_tensor("kt_bf", [64, 512], BF16))
        kh_bf = E(nc.sbuf_tensor("kh_bf", [P, 4, 64], BF16))
        att_bf = E(nc.sbuf_tensor("att_bf", [P, P], BF16))
        Sst = E(nc.sbuf_tensor("Sst", [64, P], F32))
        Sbf = E(nc.sbuf_tensor("Sbf", [64, P], BF16))
        S.op("dve", lambda: nc.vector.memset(Sst[:], 0.0), writes=["Sst"])
        S.op("dve", lambda: nc.vector.memset(Sbf[:], 0.0), writes=["Sbf"])
        osq = E(nc.sbuf_tensor("osq", [P, 512], BF16))
        rstd_o = rs
        ot = cva
        o_sb = cva
        B = [E(nc.psum_tensor(f"bank{i}", [P, 512], F32)) for i in range(8)]

        def prologue(I):
            t0 = I * 512
            for k in range(KD):
                xs = x_st[k % 2]
                S.dma("sp", ("x", k % 2), xs[:], xTv[:, k, t0:t0 + 512], writes=[("x_st", k % 2)])
                S.op("act", lambda: nc.scalar.activation(out=xsq[:, k, :], in_=xs[:], func=AF.Square), reads=[("x_st", k % 2)], writes=[("xsq", k)])
                S.op("dve", lambda: nc.vector.tensor_scalar(out=xh[:, k, :], in0=xs[:], scalar1=cc[:, o_gpre + k:o_gpre + k + 1], scalar2=None, op0=ALU.mult),
                     reads=[("x_st", k % 2), "cc"], writes=[("xh", k)])

        prologue(0)
        for I in range(NTILE):
            t0 = I * 512
            for k in range(KD):
                S.op("pe", lambda: nc.tensor.matmul(B[3][:, :], lhsT=ones_bf[:], rhs=xsq[:, k, :], start=(k == 0), stop=(k == KD - 1)),
                     reads=[("xsq", k), "ones_bf"], writes=["B3"], sig=(k == KD - 1))
            S.op("act", lambda: nc.scalar.activation(out=rstd[:], in_=B[3][:, :], func=AF.Ln, scale=1.0 / D, bias=epsc[:, 0:1]), reads=["B3", "epsc"], writes=["rstd"])
            S.op("act", lambda: nc.scalar.activation(out=rstd[:], in_=rstd[:], func=AF.Exp, scale=-0.5), reads=["rstd"], writes=["rstd"])
            for c in range(4):
                S.op("pe", lambda: nc.tensor.matmul(B[3][:, c:c + 1], lhsT=rstd[0:1, c * P:(c + 1) * P], rhs=ones_f[0:1, 0:1], start=True, stop=True),
                     reads=["rstd", "ones_f"], writes=["B3"])
            S.op("dve", lambda: nc.vector.tensor_copy(out=rstd_tok[:], in_=B[3][:, 0:4]), reads=["B3"], writes=["rstd_tok"])
            for gi, (nm, c0, ncol) in enumerate(FM_GROUPS):
                pb = B[gi % 2]
                pn = "B%d" % (gi % 2)
                for k in range(KD):
                    S.op("pe", lambda: nc.tensor.matmul(pb[0:ncol, :], lhsT=wf[:, k, c0:c0 + ncol], rhs=xh[:, k, :], start=(k == 0), stop=(k == KD - 1)),
                         reads=[("xh", k), "wf"], writes=[pn], sig=(k == KD - 1))
                dst = fo[nm]
                if nm in ("mq", "gq"):
                    S.op("dve", lambda: nc.vector.scalar_tensor_tensor(out=dst[0:ncol, :], in0=pb[0:ncol, :], scalar=0.125, in1=rstd[0:ncol, :], op0=ALU.mult, op1=ALU.mult),
                         reads=[pn, "rstd"], writes=["fo_" + nm])
                else:
                    extra = ["fo_lr1"] if nm == "lr" else []
                    S.op("dve", lambda: nc.vector.tensor_tensor(out=dst[0:ncol, :], in0=pb[0:ncol, :], in1=rstd[0:ncol, :], op=ALU.mult),
                         reads=[pn, "rstd"] + extra, writes=["fo_" + nm])
            S.op("act", lambda: nc.scalar.copy(out=qaug[0][0:64, :], in_=fo["mq"][0:64, :]), reads=["fo_mq"], writes=["qaug0q"])
            S.op("act", lambda: nc.scalar.copy(out=qaug[1][64:128, :], in_=fo["mq"][64:128, :]), reads=["fo_mq"], writes=["qaug1q"])
            S.op("act", lambda: nc.scalar.copy(out=kaug[0][0:64, t0:t0 + 512], in_=fo["mk"][0:64, :]), reads=["fo_mk"], writes=["kaug0k"])
            S.op("act", lambda: nc.scalar.copy(out=kaug[1][64:128, t0:t0 + 512], in_=fo["mk"][64:128, :]), reads=["fo_mk"], writes=["kaug1k"])
            S.op("dve", lambda: nc.vector.tensor_reduce(out=kmean[:, 2 * I:2 * I + 2], in_=fo["mk"][:, :].rearrange("p (b n) -> p b n", n=256), axis=AX.X, op=ALU.add),
                 reads=["fo_mk"], writes=["kmean"])
            for c in range(4):
                for k in range(KD):
                    S.op("pe", lambda: nc.tensor.matmul(B[2][:, 0:NTM], lhsT=xh[:, k, c * P:(c + 1) * P], rhs=wt[:, k, :], start=(k == 0), stop=(k == KD - 1)),
                         reads=[("xh", k), "wt"], writes=["B2"], sig=(k == KD - 1))
                sc_ = rstd_tok[:, c:c + 1]
                S.op("act", lambda: nc.scalar.activation(out=gk_tok[:, c, :], in_=B[2][:, 0:64], func=AF.Identity, scale=sc_), reads=["B2", "rstd_tok"], writes=[("gk_tok", c)])
                S.op("act", lambda: nc.scalar.activation(out=gv[:, c, :], in_=B[2][:, 64:192], func=AF.Identity, scale=sc_), reads=["B2", "rstd_tok"], writes=[("gv", c)])
                S.op("act", lambda: nc.scalar.activation(out=vaug0[:, 4 * I + c, 0:64], in_=B[2][:, 192:256], func=AF.Identity, scale=sc_), reads=["B2", "rstd_tok"], writes=["vaug0"])
                S.op("act", lambda: nc.scalar.activation(out=vaug1[:, 4 * I + c, 64:128], in_=B[2][:, 256:320], func=AF.Identity, scale=sc_), reads=["B2", "rstd_tok"], writes=["vaug1"])
            S.op("pool", lambda: nc.gpsimd.tensor_tensor(out=zc[:, 2:514], in0=fo["sc"][:, :], in1=fo["sx"][:, :], op=ALU.mult), reads=["fo_sc", "fo_sx"], writes=["zc"])
            scw = lambda j: cc[:, o_scw + j:o_scw + j + 1]
            S.op("dve", lambda: nc.vector.tensor_scalar(out=cva[:], in0=zc[:, 2:514], scalar1=scw(2), scalar2=None, op0=ALU.mult), reads=["zc", "cc"], writes=["cva"])
            S.op("dve", lambda: nc.vector.scalar_tensor_tensor(out=cva[:], in0=zc[:, 1:513], scalar=scw(1), in1=cva[:], op0=ALU.mult, op1=ALU.add), reads=["zc", "zc_h", "cc", "cva"], writes=["cva"])
            S.op("dve", lambda: nc.vector.scalar_tensor_tensor(out=cva[:], in0=zc[:, 0:512], scalar=scw(0), in1=cva[:], op0=ALU.mult, op1=ALU.add), reads=["zc", "zc_h", "cc", "cva"], writes=["cva"])
            S.op("pool", lambda: nc.gpsimd.tensor_tensor(out=br_sb[:, 2, :], in0=cva[:], in1=fo["sb"][:, :], op=ALU.mult), reads=["cva", "fo_sb"], writes=["br_c"])
            S.op("pool", lambda: nc.gpsimd.tensor_copy(out=zc[:, 0:2], in_=zc[:, 512:514]), reads=["zc", "zc_h"], writes=["zc_h"])
            S.op("act", lambda: nc.scalar.activation(out=fo["gr"][:, :], in_=fo["gr"][:, :], func=AF.Silu), reads=["fo_gr"], writes=["fo_gr"])
            eb = rs[0:64, :]
            enb = cva[0:64, :]
            for c in range(4):
                S.op("pe", lambda: nc.tensor.matmul(B[0][:, c * 64:(c + 1) * 64], lhsT=fo["lr"][0:32, c * P:(c + 1) * P], rhs=cc[0:32, o_wlr:o_wlr + 64], start=True, stop=True),
                     reads=["fo_lr", "fo_lr1", "cc"], writes=["B0"], sig=(c == 3))
            S.op("act", lambda: nc.scalar.activation(out=la[:].rearrange("p c d -> p (c d)"), in_=B[0][:, 0:256], func=AF.Exp, scale=-1.0), reads=["B0"], writes=["la"])
            S.op("act", lambda: nc.scalar.activation(out=la[:].rearrange("p c d -> p (c d)"), in_=la[:].rearrange("p c d -> p (c d)"), func=AF.Ln, bias=1.0), reads=["la"], writes=["la"])
            for c in range(4):
                S.op("pe", lambda: nc.tensor.matmul(B[1][0:64, c * P:(c + 1) * P], lhsT=la[:, c, :], rhs=U, start=True, stop=True), reads=["la", "cc"], writes=["B1"], sig=(c == 3))
            for c in range(4):
                S.op("pe", lambda: nc.tensor.matmul(B[2][:, c * 64:(c + 1) * 64], lhsT=SL, rhs=la[:, c, :], start=True, stop=True), reads=["la", "cc"], writes=["B2"], sig=(c == 3))
            S.op("act", lambda: nc.scalar.activation(out=eb, in_=B[1][0:64, :], func=AF.Exp), reads=["B1", "rs"], writes=["rs"])
            S.op("act", lambda: nc.scalar.activation(out=enb, in_=B[1][0:64, :], func=AF.Exp, scale=-1.0), reads=["B1", "cva"], writes=["cva"])
            S.op("act", lambda: nc.scalar.activation(out=er[:].rearrange("p c d -> p (c d)"), in_=B[2][:, 0:256], func=AF.Exp), reads=["B2"], writes=["er"])
            S.op("dve", lambda: nc.vector.tensor_tensor(out=qt_bf[:], in0=fo["gq"][0:64, :], in1=eb, op=ALU.mult), reads=["fo_gq", "rs"], writes=["qt_bf"])
            S.op("dve", lambda: nc.vector.tensor_tensor(out=kt_bf[:], in0=fo["gk"][0:64, :], in1=enb, op=ALU.mult), reads=["fo_gk", "cva"], writes=["kt_bf"])
            S.op("pool", lambda: nc.gpsimd.tensor_tensor(out=kh_bf[:].rearrange("p c d -> p (c d)"), in0=gk_tok[:].rearrange("p c d -> p (c d)"), in1=er[:].rearrange("p c d -> p (c d)"), op=ALU.mult), reads=[("gk_tok", c) for c in range(4)] + ["er"], writes=["kh_bf"])
            for c in range(4):
                cs = slice(c * P, (c + 1) * P)
                S.op("pe", lambda: nc.tensor.matmul(B[4][:, 0:128], lhsT=kt_bf[:, cs], rhs=qt_bf[:, cs], start=True, stop=True), reads=["kt_bf", "qt_bf"], writes=["B4"])
                S.op("dve", lambda: nc.vector.tensor_tensor(out=att_bf[:], in0=B[4][:, 0:128], in1=TRI, op=ALU.mult), reads=["B4", "cc"], writes=["att_bf"])
                S.op("pe", lambda: nc.tensor.matmul(B[3][0:64, 0:128], lhsT=kh_bf[:, c, :], rhs=gv[:, c, :], start=True, stop=True), reads=["kh_bf", ("gv", c)], writes=["B3"])
                S.op("pe", lambda: nc.tensor.matmul(B[5][:, cs], lhsT=gv[:, c, :], rhs=att_bf[:], start=True, stop=False), reads=[("gv", c), "att_bf"], writes=["B5"], sig=False)
                S.op("pe", lambda: nc.tensor.matmul(B[5][:, cs], lhsT=Sbf[:], rhs=qt_bf[:, cs], start=False, stop=True), reads=["Sbf", "qt_bf"], writes=["B5"])
                S.op("dve", lambda: nc.vector.scalar_tensor_tensor(out=Sst[:], in0=Sst[:], scalar=eb[:, c * P + 127:c * P + 128], in1=B[3][0:64, 0:128], op0=ALU.mult, op1=ALU.add),
                     reads=["Sst", "rs", "B3"], writes=["Sst"])
                S.op("pool", lambda: nc.gpsimd.tensor_copy(out=Sbf[:], in_=Sst[:]), reads=["Sst"], writes=["Sbf"])
            S.op("act", lambda: nc.scalar.activation(out=osq[:], in_=B[5][:, :], func=AF.Square), reads=["B5"], writes=["osq"])
            S.op("pe", lambda: nc.tensor.matmul(B[3][:, :], lhsT=ones_bf[:], rhs=osq[:], start=True, stop=True), reads=["osq", "ones_bf"], writes=["B3"])
            S.op("act", lambda: nc.scalar.activation(out=rstd_o[:], in_=B[3][:, :], func=AF.Ln, scale=1.0 / P, bias=epsc[:, 0:1]), reads=["B3", "epsc"], writes=["rs"])
            S.op("act", lambda: nc.scalar.activation(out=rstd_o[:], in_=rstd_o[:], func=AF.Exp, scale=-0.5), reads=["rs"], writes=["rs"])
            S.op("dve", lambda: nc.vector.tensor_tensor(out=ot[:], in0=B[5][:, :], in1=rstd_o[:], op=ALU.mult), reads=["B5", "rs"], writes=["cva"])
            S.op("dve", lambda: nc.vector.scalar_tensor_tensor(out=br_sb[:, 0, :], in0=ot[:], scalar=cc[:, o_gn:o_gn + 1], in1=fo["gr"][:, :], op0=ALU.mult, op1=ALU.mult),
                 reads=["cva", "cc", "fo_gr"], writes=["br_a"])
            if I + 1 < NTILE:
                prologue(I + 1)
            for h in range(2):
                hs = slice(64 * h, 64 * h + 64)
                ms = slice(64 * (1 - h), 64 * (1 - h) + 64)
                for qs in range(4):
                    qt = 4 * I + qs
                    bi = qt // 2
                    S.op("pe", lambda: nc.tensor.matmul(B[3][:, 0:64], lhsT=fo["mq"][hs, qs * P:(qs + 1) * P], rhs=kmean[hs, :], start=True, stop=True),
                         reads=["fo_mq", "kmean"], writes=["B3"])
                    S.op("dve", lambda: nc.vector.tensor_tensor(out=gm[:], in0=B[3][:, 0:64], in1=cc[:, C_FILL + 63 - bi:C_FILL + 127 - bi], op=ALU.add), reads=["B3", "cc"], writes=["gm"])
                    S.op("dve", lambda: nc.vector.max(out=top8[:], in_=gm[:]), reads=["gm"], writes=["top8"])
                    S.op("dve", lambda: nc.vector.tensor_scalar(out=thr[:], in0=top8[:, 3:4], scalar1=-1e29, scalar2=None, op0=ALU.max), reads=["top8"], writes=["thr"])
                    S.op("dve", lambda: nc.vector.tensor_scalar(out=tsel[:], in0=gm[:], scalar1=thr[:, 0:1], scalar2=BIG, op0=ALU.is_ge, op1=ALU.mult), reads=["gm", "thr"], writes=["tsel"])
                    S.op("dve", lambda: nc.vector.scalar_tensor_tensor(out=mstage[:, qs, ms], in0=tsel[:], scalar=shiftc[:, h * NKT + qt:h * NKT + qt + 1], in1=cc[:, C_A0 + 64 * h:C_A0 + 64 * h + 64], op0=ALU.add, op1=ALU.add),
                         reads=["tsel", "shiftc", "cc"], writes=[("mstage", qs)])
                for qs in range(4):
                    S.op("pe", lambda: nc.tensor.transpose(B[5][:, qs * P:(qs + 1) * P], mstage[:, qs, :], IDF), reads=[("mstage", qs), "cc"], writes=["B5"])
                S.op("act", lambda: nc.scalar.copy(out=qaug[h][ms, :], in_=B[5][ms, :]), reads=["B5"], writes=[f"qaug{h}m"])
                po = B[6 + h]
                nkt = 4 * I + 4
                def emit_s(kt):
                    ib = kt % 3
                    ps = B[ib]
                    pn = "B%d" % ib
                    diag = kt >= 4 * I
                    S.op("pe", lambda: nc.tensor.matmul(ps[:, :], lhsT=kaug[h][:, kt * P:(kt + 1) * P], rhs=qaug[h][:, :], start=True, stop=not diag),
                         reads=[f"kaug{h}k", f"kaug{h}e", f"qaug{h}q", f"qaug{h}m"], writes=[pn], sig=not diag)
                    if diag:
                        S.op("pe", lambda: nc.tensor.matmul(ps[:, :], lhsT=ident_bf[:], rhs=causneg[:, kt - 4 * I, :], start=False, stop=True),
                             reads=["ident_bf", "causneg"], writes=[pn])

                def emit_pv(kt):
                    ib = kt % 3
                    ps = B[ib]
                    pn = "B%d" % ib
                    S.op("act", lambda: nc.scalar.activation(out=pT[ib][:], in_=ps[:, :], func=AF.Exp, bias=cc[:, o_al + 2 * h + (kt % 2):o_al + 2 * h + (kt % 2) + 1]),
                         reads=[pn, "cc"], writes=[("pT", ib)])
                    if h == 0:
                        S.op("pe", lambda: nc.tensor.matmul(po[0:65, :], lhsT=vaug0[:, kt, :], rhs=pT[ib][:], start=(kt == 0), stop=(kt == nkt - 1)),
                             reads=[("pT", ib), "vaug0", "vaug0c"], writes=["B6"], sig=(kt == nkt - 1))
                    else:
                        S.op("pe", lambda: nc.tensor.matmul(po[:, :], lhsT=vaug1[:, kt, :], rhs=pT[ib][:], start=(kt == 0), stop=(kt == nkt - 1)),
                             reads=[("pT", ib), "vaug1", "vaug1c"], writes=["B7"], sig=(kt == nkt - 1))

                emit_s(0)
                if nkt > 1:
                    emit_s(1)
                for kt in range(nkt):
                    if kt + 2 < nkt:
                        emit_s(kt + 2)
                    emit_pv(kt)
                srow = 64 if h == 0 else 0
                pnm = "B%d" % (6 + h)
                S.op("dve", lambda: nc.vector.reciprocal(out=rs[srow:srow + 1, :], in_=po[srow:srow + 1, :]), reads=[pnm], writes=["rs"])
                S.op("pe", lambda: nc.tensor.matmul(B[3][:, :], lhsT=ones_f[srow:srow + 1, :], rhs=rs[srow:srow + 1, :], start=True, stop=True), reads=["rs", "ones_f"], writes=["B3"])
                S.op("act", lambda: nc.scalar.copy(out=o_sb[hs, :], in_=po[hs, :]), reads=[pnm], writes=["cva"])
                S.op("dve", lambda: nc.vector.tensor_tensor(out=br_sb[hs, 1, :], in0=o_sb[hs, :], in1=B[3][hs, :], op=ALU.mult), reads=["cva", "B3"], writes=[("br_b", h)])
            S.dma("sp", "br_st", brv[:, :, t0:t0 + 512], br_sb[:], reads=["br_a", ("br_b", 0), ("br_b", 1), "br_c"], writes=[("brT", I)])
        S.finish("sp", [("brT", I) for I in range(NTILE)])


def mixer_consts(D, T, slopes2, g_pre, wlr2_g, blr_g, gnorm, scw_g):
    KD = D // P
    NKT = T // P
    ncc = C_PP + KD + 64 + 1 + 3 + 4
    cc = np.zeros((P, ncc), np.float32)
    j = np.arange(P)[:, None]
    i = np.arange(P)[None, :]
    cc[:, C_U:C_U + P] = (j <= i) * (-1.0 / 16.0)
    cc[:, C_SL:C_SL + P] = (j > i) * (-1.0 / 16.0)
    cc[:, C_TRI:C_TRI + P] = (j <= i) * 1.0
    cc[:, C_ID:C_ID + P] = np.eye(P)
    c = np.arange(128)
    cc[:, C_FILL:C_FILL + 128] = np.where(c < 63, 0.0, np.where(c == 63, 1e30, -1e30))[None, :]
    il = np.arange(P)[:, None]
    b = np.arange(64)[None, :]
    for h in range(2):
        cc[:, C_A0 + 64 * h:C_A0 + 64 * h + 64] = -slopes2[h] * (il - 256.0 * b)
    o = C_PP
    cc[:, o:o + KD] = g_pre.reshape(KD, P).T
    o += KD
    cc[0:16, o:o + 64] = wlr2_g
    cc[16, o:o + 64] = blr_g
    o += 64
    cc[:, o] = gnorm
    o += 1
    cc[:, o:o + 3] = scw_g.T
    o += 3
    for h in range(2):
        for par in range(2):
            cc[:, o + 2 * h + par] = slopes2[h] * (128.0 * par + np.arange(P))
    shiftc = np.zeros((P, 2 * NKT), np.float32)
    for h in range(2):
        shiftc[:, h * NKT:(h + 1) * NKT] = (-BIG - slopes2[h] * 128.0 * np.arange(NKT))[None, :]
    import ml_dtypes
    jl = np.arange(P)[:, None, None]
    kk = np.arange(4)[None, :, None]
    ii = np.arange(512)[None, None, :]
    causneg = np.where(128 * kk + jl <= ii, 0.0, -BIG).astype(ml_dtypes.bfloat16)
    eblk = np.zeros((64, T), np.float32)
    for bb in range(min(64, T // 256)):
        eblk[bb, 256 * bb:256 * bb + 256] = 1.0
    return cc, shiftc, causneg, eblk.astype(ml_dtypes.bfloat16)


def build_mixer(dm):
    nc = bass.Bass("TRN2", target_bir_lowering=False)
    D, T = dm["D"], dm["T"]
    KD, NKT = D // P, T // P
    ncc = C_PP + KD + 64 + 1 + 3 + 4
    a = {
        "xT": nc.dram_tensor("xT", [D, T], F32, kind="ExternalInput").ap(),
        "w_f": nc.dram_tensor("w_f", [D, NFM], F32, kind="ExternalInput").ap(),
        "w_t": nc.dram_tensor("w_t", [D, NTM], F32, kind="ExternalInput").ap(),
        "cc": nc.dram_tensor("cc", [P, ncc], F32, kind="ExternalInput").ap(),
        "shiftc": nc.dram_tensor("shiftc", [P, 2 * NKT], F32, kind="ExternalInput").ap(),
        "causneg": nc.dram_tensor("causneg", [P, 4, 512], BF16, kind="ExternalInput").ap(),
        "eblk": nc.dram_tensor("eblk", [64, T], BF16, kind="ExternalInput").ap(),
        "brT": nc.dram_tensor("brT", [3 * P, T], BF16, kind="ExternalOutput").ap(),
    }
    with contextlib.ExitStack() as es:
        S = Sched(nc, es)
        emit_mixer(nc, S, dm, a)
        print("mixer instrs", S.ninstr, "sems", S.nsem)
    return nc


from concourse.bass_utils import run_bass_kernel_spmd
import ml_dtypes

D_MODEL, SEQ, BATCH, DEPTH, D_FF = 1024, 16384, 2, 2, 2816
ALIBI = 2.0 ** (-8.0 * (np.arange(8) + 1.0) / 8)
_NC_CACHE = {}


def _get(name, fn):
    if name not in _NC_CACHE:
        _NC_CACHE[name] = fn()
    return _NC_CACHE[name]


def _mixer_inputs(xT_b, l, g, w_in, gla_w_lr2, gla_b_lr, gla_norm, sc_conv_w, g_mix_pre):
    W = w_in[l]
    sl = lambda o, n: W[:, o + n * g: o + n * g + n]
    w_f = np.ascontiguousarray(np.concatenate([sl(0, 64), sl(256, 64), sl(1024, 128), W[:, 1536:1552], sl(1552, 128), sl(2064, 128),
                                               sl(3088, 128), sl(3600, 128), sl(4112, 128)], axis=1))
    w_t = np.ascontiguousarray(np.concatenate([sl(256, 64), sl(512, 128), sl(2576, 128)], axis=1))
    cc, shiftc, causneg, eblk = mixer_consts(D_MODEL, SEQ, ALIBI[2 * g:2 * g + 2], g_mix_pre[l], gla_w_lr2[l][:, 64 * g:64 * g + 64],
                                             gla_b_lr[l][64 * g:64 * g + 64], gla_norm[l], sc_conv_w[l][:, 128 * g:128 * g + 128])
    return {"xT": xT_b, "w_f": w_f, "w_t": w_t, "cc": cc, "shiftc": shiftc, "causneg": causneg, "eblk": eblk}


def kernel(x, g_mix_pre, w_in, gla_w_lr2, gla_b_lr, gla_norm, sc_conv_w, w_br_gla, w_br_moba, w_br_sc, w_out, g_mix_post,
           g_ffn_pre, ffn_w_up, ffn_conv_w, ffn_conv_b, ffn_w_down, g_ffn_post):
    f32 = lambda v: np.ascontiguousarray(np.asarray(v, dtype=np.float32))
    (x, g_mix_pre, w_in, gla_w_lr2, gla_b_lr, gla_norm, sc_conv_w, w_br_gla, w_br_moba, w_br_sc, w_out, g_mix_post,
     g_ffn_pre, ffn_w_up, ffn_conv_w, ffn_conv_b, ffn_w_down, g_ffn_post) = map(f32, (
        x, g_mix_pre, w_in, gla_w_lr2, gla_b_lr, gla_norm, sc_conv_w, w_br_gla, w_br_moba, w_br_sc, w_out, g_mix_post,
        g_ffn_pre, ffn_w_up, ffn_conv_w, ffn_conv_b, ffn_w_down, g_ffn_post))
    NTOK = SEQ // 4
    nc_a = _get("mixer", lambda: build_mixer(dict(D=D_MODEL, T=SEQ)))
    nc_b = _get("dense", lambda: build_dense(dict(D=D_MODEL, DFF=D_FF, BW=1536, NTOK=NTOK)))
    xT = [np.ascontiguousarray(x[b].T) for b in range(BATCH)]
    for l in range(DEPTH):
        in_maps = [_mixer_inputs(xT[c // 4], l, c % 4, w_in, gla_w_lr2, gla_b_lr, gla_norm, sc_conv_w, g_mix_pre) for c in range(8)]
        res = run_bass_kernel_spmd(nc_a, in_maps, core_ids=list(range(8)))
        brT = []
        for b in range(BATCH):
            parts = [np.asarray(res.results[4 * b + g]["brT"]) for g in range(4)]
            brT.append(np.concatenate([p[0:128] for p in parts] + [p[128:256] for p in parts] + [p[256:384] for p in parts], axis=0))
        w_gate = np.ascontiguousarray(w_in[l][:, 4624:7696])
        w_br = np.ascontiguousarray(np.concatenate([w_br_gla[l], w_br_moba[l], w_br_sc[l]], axis=0))
        pp = pack_params_dense(g_mix_pre[l], g_mix_post[l], g_ffn_pre[l], g_ffn_post[l], ffn_conv_w[l], ffn_conv_b[l])
        in_maps = []
        for c in range(8):
            b, j = c // 4, c % 4
            t0 = j * NTOK
            xs = np.zeros((D_MODEL, NTOK + 2), np.float32)
            bs = np.zeros((1536, NTOK + 2), ml_dtypes.bfloat16)
            lo = max(t0 - 2, 0)
            xs[:, 2 - (t0 - lo):] = xT[b][:, lo:t0 + NTOK]
            bs[:, 2 - (t0 - lo):] = brT[b][:, lo:t0 + NTOK]
            in_maps.append({"xT": xs, "brT": bs, "w_gate": w_gate, "w_br": w_br, "w_out": w_out[l], "w_up": ffn_w_up[l],
                            "w_down": ffn_w_down[l], "pp": pp})
        res = run_bass_kernel_spmd(nc_b, in_maps, core_ids=list(range(8)))
        xT = [np.ascontiguousarray(np.concatenate([np.asarray(res.results[4 * b + j]["yT"]) for j in range(4)], axis=1)) for b in range(BATCH)]
    return np.ascontiguousarray(np.stack([xT[b].T for b in range(BATCH)], axis=0).astype(np.float32))
```

```python
import concourse.bass as bass
import concourse.mybir as mybir

F32 = mybir.dt.float32
BF16 = mybir.dt.bfloat16
AF = mybir.ActivationFunctionType
ALU = mybir.AluOpType
AX = mybir.AxisListType

SEM_EPOCH = 40000


class Sched:
    def __init__(self, nc, es, same_engine_sync=True):
        self.nc = nc
        self.es = es
        self.same = same_engine_sync
        self.eng = {"pe": nc.tensor, "act": nc.scalar, "dve": nc.vector, "pool": nc.gpsimd, "sp": nc.sync}
        self.sem = {}
        self.cnt = {}
        self.nsem = 0
        for e in self.eng:
            self._new_sem(e)
        self.waited = {e: {} for e in self.eng}
        self.lastw = {}
        self.readers = {}
        self.dma_sems = {}
        self.ninstr = 0
        self.pending = {}

    def _new_sem(self, e):
        self.nsem += 1
        self.sem[e] = self.es.enter_context(self.nc.semaphore(f"s_{e}_{self.nsem}"))
        self.cnt[e] = 0

    def _wait(self, e, tok):
        if tok is None:
            return
        sem, val, src = tok
        if src == e and (not self.same or e == "pe"):
            return
        w = self.waited[e]
        k = id(sem)
        if w.get(k, 0) >= val:
            return
        self.eng[e].wait_ge(sem, val)
        w[k] = val

    def _deps(self, e, reads, writes):
        for r in reads:
            self._wait(e, self.lastw.get(r))
        for r in writes:
            self._wait(e, self.lastw.get(r))
            for t in list(self.readers.get(r, {}).values()):
                self._wait(e, t)

    def _commit(self, tok, reads, writes):
        for r in reads:
            self.readers.setdefault(r, {})[(tok[2], id(tok[0]))] = tok
        for r in writes:
            self.lastw[r] = tok
            self.readers[r] = {}

    def op(self, e, fn, reads=(), writes=(), sig=True):
        self._deps(e, reads, writes)
        if self.cnt[e] >= SEM_EPOCH and not self.pending.get(e):
            self._new_sem(e)
        ins = fn()
        if sig:
            self.cnt[e] += 1
            ins.then_inc(self.sem[e], 1)
            tok = (self.sem[e], self.cnt[e], e)
            self.pending[e] = False
        else:
            tok = (self.sem[e], self.cnt[e] + 1, e)
            self.pending[e] = True
        self._commit(tok, reads, writes)
        self.ninstr += 1
        return tok

    def dma(self, q, key, out, in_, reads=(), writes=()):
        self._deps(q, reads, writes)
        st = self.dma_sems.get(key)
        if st is None or st[1] >= SEM_EPOCH:
            self.nsem += 1
            st = [self.es.enter_context(self.nc.semaphore(f"d_{self.nsem}")), 0]
            self.dma_sems[key] = st
        ins = self.eng[q].dma_start(out=out, in_=in_)
        st[1] += 16
        ins.then_inc(st[0], 16)
        tok = (st[0], st[1], "dma")
        self._commit(tok, reads, writes)
        self.ninstr += 1
        return tok

    def finish(self, e, resources):
        for r in resources:
            self._wait(e, self.lastw.get(r))

    def barrier(self):
        toks = [(self.sem[e], self.cnt[e], e) for e in self.eng if self.cnt[e] > 0]
        toks += [(st[0], st[1], "dma") for st in self.dma_sems.values()]
        for e in self.eng:
            for t in toks:
                self._wait(e, t)

import contextlib
import numpy as np

P = 128
EPS = 1e-6


def split_even(n, maxn):
    k = -(-n // maxn)
    base, rem = divmod(n, k)
    out, s = [], 0
    for i in range(k):
        sz = base + (1 if i < rem else 0)
        out.append((s, sz))
        s += sz
    return out


def load_w(S, nc, wt, wd, nk, qname="pool", key="w"):
    v = wd.rearrange("(k p) n -> p k n", p=P)
    n = wd.shape[1]
    step = max(1, 4096 // n)
    for k0 in range(0, nk, step):
        k1 = min(nk, k0 + step)
        S.dma(qname, (key, k0 % 4), wt[:, k0:k1, :], v[:, k0:k1, :], writes=[(key, k0)])
    return [(key, k0) for k0 in range(0, nk, step)]


def rstd_from_psum(S, nc, out, ps, D, eps_col, reads, wres):
    S.op("act", lambda: nc.scalar.activation(out=out, in_=ps, func=AF.Ln, scale=1.0 / D, bias=eps_col), reads=reads, writes=[wres])
    S.op("act", lambda: nc.scalar.activation(out=out, in_=out, func=AF.Exp, scale=-0.5), reads=[wres], writes=[wres])


def emit_dense(nc, S, dm, a, n1max=456, n2max=410):
    D, DFF, BW, NTOK = dm["D"], dm["DFF"], dm["BW"], dm["NTOK"]
    KD, KB, KF = D // P, BW // P, DFF // P
    KBB = KB // 3
    NT = NTOK + 2
    o_gpre, o_gpost, o_gfpre, o_gfpost = 0, KD, 2 * KD, 3 * KD
    o_cw = 4 * KD
    o_cb = o_cw + 3 * 2 * KF
    npar = o_cb + 2 * KF
    xTv = a["xT"].rearrange("(k p) n -> p k n", p=P)
    brTv = a["brT"].rearrange("(k p) n -> p k n", p=P)
    xmv = a["xmid"].rearrange("(k p) n -> p k n", p=P)
    yTv = a["yT"].rearrange("(k p) n -> p k n", p=P)

    with contextlib.ExitStack() as es0:
        E0 = es0.enter_context
        pp = E0(nc.sbuf_tensor("pp_sb", [P, npar], F32))
        ones = E0(nc.sbuf_tensor("ones", [P, P], BF16))
        epsc = E0(nc.sbuf_tensor("epsc", [P, 1], F32))
        S.dma("sp", "pp", pp[:], a["pp"][:, :], writes=["pp"])
        S.op("dve", lambda: nc.vector.memset(ones[:], 1.0), writes=["ones"])
        S.op("dve", lambda: nc.vector.memset(epsc[:], EPS), writes=["epsc"])
        d1_tiles = split_even(NT, n1max)
        N1 = max(n for _, n in d1_tiles)
        with contextlib.ExitStack() as es:
            E = es.enter_context
            wg = E(nc.sbuf_tensor("wg", [P, KD, 3 * D], BF16))
            wbr = E(nc.sbuf_tensor("wbr", [P, KB, D], BF16))
            wo = E(nc.sbuf_tensor("wo", [P, KD, D], BF16))
            r_wg = load_w(S, nc, wg, a["w_gate"], KD, key="wg")
            r_wbr = load_w(S, nc, wbr, a["w_br"], KB, key="wbr")
            r_wo = load_w(S, nc, wo, a["w_out"], KD, key="wo")
            x_sb = E(nc.sbuf_tensor("x_sb", [P, KD, N1], F32))
            br_sb = E(nc.sbuf_tensor("br_sb", [P, KB, N1], BF16))
            xsq = E(nc.sbuf_tensor("xsq", [P, KD, N1], BF16))
            xh = E(nc.sbuf_tensor("xh", [P, KD, N1], BF16))
            rstd = E(nc.sbuf_tensor("rstd", [P, N1], F32))
            rstd2 = E(nc.sbuf_tensor("rstd2", [P, N1], F32))
            gl = [E(nc.sbuf_tensor(f"gl{i}", [P, N1], F32)) for i in range(3)]
            tm = [E(nc.sbuf_tensor(f"tm{i}", [P, N1], F32)) for i in range(3)]
            mf = [E(nc.sbuf_tensor(f"mf{i}", [P, N1], F32)) for i in range(2)]
            mbf = E(nc.sbuf_tensor("mbf", [P, KD, N1], BF16))
            z_sb = E(nc.sbuf_tensor("z_sb", [P, KD, N1], F32))
            ps_st = E(nc.psum_tensor("ps_st", [P, 512], F32))
            ps_g = [E(nc.psum_tensor(f"ps_g{i}", [P, 512], F32)) for i in range(3)]
            ps_p = [E(nc.psum_tensor(f"ps_p{i}", [P, 512], F32)) for i in range(3)]
            ps_z = E(nc.psum_tensor("ps_z", [P, 512], F32))
            it = 0
            for ti, (c0, N) in enumerate(d1_tiles):
                S.dma("sp", "x", x_sb[:, :, :N], xTv[:, :, c0:c0 + N], writes=["x_sb"])
                S.dma("sp", "br", br_sb[:, :, :N], brTv[:, :, c0:c0 + N], writes=["br_sb"])
                S.op("act", lambda: nc.scalar.activation(out=xsq[:, :, :N], in_=x_sb[:, :, :N], func=AF.Square), reads=["x_sb"], writes=["xsq"])
                for k in range(KD):
                    S.op("act", lambda: nc.scalar.activation(out=xh[:, k, :N], in_=x_sb[:, k, :N], func=AF.Identity, scale=pp[:, o_gpre + k:o_gpre + k + 1]),
                         reads=["x_sb", "pp"], writes=[("xh", k)])
                for k in range(KD):
                    S.op("pe", lambda: nc.tensor.matmul(ps_st[:, :N], lhsT=ones[:], rhs=xsq[:, k, :N], start=(k == 0), stop=(k == KD - 1)),
                         reads=["xsq", "ones"], writes=["ps_st"], sig=(k == KD - 1))
                rstd_from_psum(S, nc, rstd[:, :N], ps_st[:, :N], D, epsc[:, 0:1], ["ps_st", "epsc"], "rstd")
                def d1_head(m, b, i3):
                    pg, pq, g_t = ps_g[i3], ps_p[i3], gl[i3]
                    for k in range(KD):
                        S.op("pe", lambda: nc.tensor.matmul(pg[:, :N], lhsT=wg[:, k, b * D + m * P:b * D + (m + 1) * P], rhs=xh[:, k, :N], start=(k == 0), stop=(k == KD - 1)),
                             reads=[("xh", k)] + r_wg, writes=[("pg", i3)], sig=(k == KD - 1))
                    for k in range(KBB):
                        S.op("pe", lambda: nc.tensor.matmul(pq[:, :N], lhsT=wbr[:, b * KBB + k, m * P:(m + 1) * P], rhs=br_sb[:, b * KBB + k, :N], start=(k == 0), stop=(k == KBB - 1)),
                             reads=["br_sb"] + r_wbr, writes=[("pq", i3)], sig=(k == KBB - 1))
                    S.op("dve", lambda: nc.vector.tensor_tensor(out=g_t[:, :N], in0=pg[:, :N], in1=rstd[:, :N], op=ALU.mult),
                         reads=[("pg", i3), "rstd"], writes=[("gl", i3)])
                    S.op("act", lambda: nc.scalar.activation(out=g_t[:, :N], in_=g_t[:, :N], func=AF.Sigmoid), reads=[("gl", i3)], writes=[("gl", i3)])

                def d1_tail(m, b, i3):
                    pq, g_t, t_t = ps_p[i3], gl[i3], tm[i3]
                    mfm = mf[m % 2]
                    if b == 0:
                        S.op("dve", lambda: nc.vector.tensor_tensor(out=mfm[:, :N], in0=g_t[:, :N], in1=pq[:, :N], op=ALU.mult),
                             reads=[("gl", i3), ("pq", i3)], writes=[("mf", m % 2)])
                    else:
                        S.op("dve", lambda: nc.vector.tensor_tensor(out=t_t[:, :N], in0=g_t[:, :N], in1=pq[:, :N], op=ALU.mult),
                             reads=[("gl", i3), ("pq", i3)], writes=[("tm", i3)])
                        dst = mfm[:, :N] if b == 1 else mbf[:, m, :N]
                        wr = [("mf", m % 2)] if b == 1 else [("mbf", m)]
                        S.op("pool", lambda: nc.gpsimd.tensor_tensor(out=dst, in0=mfm[:, :N], in1=t_t[:, :N], op=ALU.add),
                             reads=[("mf", m % 2), ("tm", i3)], writes=wr)

                groups = [(m, b) for m in range(KD) for b in range(3)]
                prev = None
                for (m, b) in groups:
                    i3 = it % 3
                    it += 1
                    d1_head(m, b, i3)
                    if prev is not None:
                        d1_tail(*prev)
                    prev = (m, b, i3)
                d1_tail(*prev)
                for m in range(KD):
                    for k in range(KD):
                        S.op("pe", lambda: nc.tensor.matmul(ps_z[:, :N], lhsT=wo[:, k, m * P:(m + 1) * P], rhs=mbf[:, k, :N], start=(k == 0), stop=(k == KD - 1)),
                             reads=[("mbf", k)] + r_wo, writes=["ps_z"], sig=(k == KD - 1))
                    S.op("act", lambda: nc.scalar.copy(out=z_sb[:, m, :N], in_=ps_z[:, :N]), reads=["ps_z"], writes=[("z", m)])
                    S.op("pool", lambda: nc.gpsimd.tensor_tensor(out=xsq[:, m, :N], in0=z_sb[:, m, :N], in1=z_sb[:, m, :N], op=ALU.mult), reads=[("z", m)], writes=["xsq"])
                for k in range(KD):
                    S.op("pe", lambda: nc.tensor.matmul(ps_st[:, :N], lhsT=ones[:], rhs=xsq[:, k, :N], start=(k == 0), stop=(k == KD - 1)),
                         reads=["xsq", "ones"], writes=["ps_st"], sig=(k == KD - 1))
                rstd_from_psum(S, nc, rstd2[:, :N], ps_st[:, :N], D, epsc[:, 0:1], ["ps_st", "epsc"], "rstd2")
                for m in range(KD):
                    S.op("dve", lambda: nc.vector.scalar_tensor_tensor(out=z_sb[:, m, :N], in0=z_sb[:, m, :N], scalar=pp[:, o_gpost + m:o_gpost + m + 1], in1=rstd2[:, :N], op0=ALU.mult, op1=ALU.mult),
                         reads=[("z", m), "rstd2", "pp"], writes=[("z", m)])
                    S.op("pool", lambda: nc.gpsimd.tensor_tensor(out=z_sb[:, m, :N], in0=z_sb[:, m, :N], in1=x_sb[:, m, :N], op=ALU.add),
                         reads=[("z", m), "x_sb"], writes=[("z", m)])
                S.dma("sp", "xm_st", xmv[:, :, c0:c0 + N], z_sb[:, :, :N], reads=[("z", m) for m in range(KD)], writes=[("xmid", ti)])
        S.barrier()
        d2_tiles = split_even(NTOK, n2max)
        N2 = max(n for _, n in d2_tiles)
        with contextlib.ExitStack() as es:
            E = es.enter_context
            wup = E(nc.sbuf_tensor("wup", [P, KD, 2 * DFF], BF16))
            wdn = E(nc.sbuf_tensor("wdn", [P, KF, D], BF16))
            r_wup = load_w(S, nc, wup, a["w_up"], KD, key="wup")
            r_wdn = load_w(S, nc, wdn, a["w_down"], KF, key="wdn")
            xm = E(nc.sbuf_tensor("xm", [P, KD, N2 + 2], F32))
            xsq = E(nc.sbuf_tensor("xsq2", [P, KD, N2 + 2], BF16))
            hn = E(nc.sbuf_tensor("hn", [P, KD, N2 + 2], BF16))
            rstd = E(nc.sbuf_tensor("rstd_b", [P, N2 + 2], F32))
            rstd3 = E(nc.sbuf_tensor("rstd3", [P, N2 + 2], F32))
            a_sb = E(nc.sbuf_tensor("a_sb", [P, KF, N2], BF16))
            cvg = [E(nc.sbuf_tensor(f"cvg{i}", [P, N2], F32)) for i in range(3)]
            cvu = [E(nc.sbuf_tensor(f"cvu{i}", [P, N2], F32)) for i in range(3)]
            y_sb = E(nc.sbuf_tensor("y_sb", [P, KD, N2], F32))
            ps_st = E(nc.psum_tensor("ps_st2", [P, 512], F32))
            ps_ug = [E(nc.psum_tensor(f"ps_ug{i}", [P, 512], F32)) for i in range(3)]
            ps_uu = [E(nc.psum_tensor(f"ps_uu{i}", [P, 512], F32)) for i in range(3)]
            ps_y = [E(nc.psum_tensor(f"ps_y{i}", [P, 512], F32)) for i in range(1)]
            all_xmid = [("xmid", ti) for ti in range(len(d1_tiles))]
            for ti, (o0, N) in enumerate(d2_tiles):
                M = N + 2
                S.dma("sp", "xm", xm[:, :, :M], xmv[:, :, o0:o0 + M], reads=all_xmid, writes=["xm"])
                S.op("act", lambda: nc.scalar.activation(out=xsq[:, :, :M], in_=xm[:, :, :M], func=AF.Square), reads=["xm"], writes=["xsq2"])
                for k in range(KD):
                    S.op("pe", lambda: nc.tensor.matmul(ps_st[:, :M], lhsT=ones[:], rhs=xsq[:, k, :M], start=(k == 0), stop=(k == KD - 1)),
                         reads=["xsq2", "ones"], writes=["ps_st2"], sig=(k == KD - 1))
                rstd_from_psum(S, nc, rstd[:, :M], ps_st[:, :M], D, epsc[:, 0:1], ["ps_st2", "epsc"], "rstd_b")
                for k in range(KD):
                    S.op("dve", lambda: nc.vector.scalar_tensor_tensor(out=hn[:, k, :M], in0=xm[:, k, :M], scalar=pp[:, o_gfpre + k:o_gfpre + k + 1], in1=rstd[:, :M], op0=ALU.mult, op1=ALU.mult),
                         reads=["xm", "rstd_b", "pp"], writes=[("hn", k)])
                def d2_head(c):
                    i2 = c % 3
                    for half, pst, cv, col, nm in ((0, ps_ug[i2], cvg[i2], c, "ug"), (1, ps_uu[i2], cvu[i2], KF + c, "uu")):
                        for k in range(KD):
                            S.op("pe", lambda: nc.tensor.matmul(pst[:, :M], lhsT=wup[:, k, col * P:(col + 1) * P], rhs=hn[:, k, :M], start=(k == 0), stop=(k == KD - 1)),
                                 reads=[("hn", k)] + r_wup, writes=[(nm, i2)], sig=(k == KD - 1))
                        cw = lambda j: pp[:, o_cw + j * 2 * KF + col:o_cw + j * 2 * KF + col + 1]
                        S.op("act", lambda: nc.scalar.activation(out=cv[:, :N], in_=pst[:, 2:N + 2], func=AF.Identity, scale=cw(2), bias=pp[:, o_cb + col:o_cb + col + 1]),
                             reads=[(nm, i2), "pp"], writes=[("cv" + nm, i2)])
                        S.op("dve", lambda: nc.vector.scalar_tensor_tensor(out=cv[:, :N], in0=pst[:, 1:N + 1], scalar=cw(1), in1=cv[:, :N], op0=ALU.mult, op1=ALU.add),
                             reads=[(nm, i2), "pp", ("cv" + nm, i2)], writes=[("cv" + nm, i2)])
                        S.op("dve", lambda: nc.vector.scalar_tensor_tensor(out=cv[:, :N], in0=pst[:, 0:N], scalar=cw(0), in1=cv[:, :N], op0=ALU.mult, op1=ALU.add),
                             reads=[(nm, i2), "pp", ("cv" + nm, i2)], writes=[("cv" + nm, i2)])

                def d2_tail(c):
                    i2 = c % 3
                    S.op("pool", lambda: nc.gpsimd.tensor_tensor(out=cvu[i2][:, :N], in0=cvu[i2][:, :N], in1=cvg[i2][:, :N], op=ALU.mult),
                         reads=[("cvug", i2), ("cvuu", i2)], writes=[("cvuu", i2)])
                    S.op("act", lambda: nc.scalar.activation(out=cvg[i2][:, :N], in_=cvg[i2][:, :N], func=AF.Sigmoid), reads=[("cvug", i2)], writes=[("cvug", i2)])
                    S.op("pool", lambda: nc.gpsimd.tensor_tensor(out=a_sb[:, c, :N], in0=cvu[i2][:, :N], in1=cvg[i2][:, :N], op=ALU.mult),
                         reads=[("cvug", i2), ("cvuu", i2)], writes=[("a", c)])

                for c in range(KF + 1):
                    if c < KF:
                        d2_head(c)
                    if c >= 1:
                        d2_tail(c - 1)
                for m in range(KD):
                    py = ps_y[0]
                    for c in range(KF):
                        S.op("pe", lambda: nc.tensor.matmul(py[:, :N], lhsT=wdn[:, c, m * P:(m + 1) * P], rhs=a_sb[:, c, :N], start=(c == 0), stop=(c == KF - 1)),
                             reads=[("a", c)] + r_wdn, writes=[("py", 0)], sig=(c == KF - 1))
                    S.op("act", lambda: nc.scalar.copy(out=y_sb[:, m, :N], in_=py[:, :N]), reads=[("py", 0)], writes=[("y", m)])
                    S.op("pool", lambda: nc.gpsimd.tensor_tensor(out=xsq[:, m, :N], in0=y_sb[:, m, :N], in1=y_sb[:, m, :N], op=ALU.mult), reads=[("y", m)], writes=["xsq2"])
                for k in range(KD):
                    S.op("pe", lambda: nc.tensor.matmul(ps_st[:, :N], lhsT=ones[:], rhs=xsq[:, k, :N], start=(k == 0), stop=(k == KD - 1)),
                         reads=["xsq2", "ones"], writes=["ps_st2"], sig=(k == KD - 1))
                rstd_from_psum(S, nc, rstd3[:, :N], ps_st[:, :N], D, epsc[:, 0:1], ["ps_st2", "epsc"], "rstd3")
                for m in range(KD):
                    S.op("dve", lambda: nc.vector.scalar_tensor_tensor(out=y_sb[:, m, :N], in0=y_sb[:, m, :N], scalar=pp[:, o_gfpost + m:o_gfpost + m + 1], in1=rstd3[:, :N], op0=ALU.mult, op1=ALU.mult),
                         reads=[("y", m), "rstd3", "pp"], writes=[("y", m)])
                    S.op("pool", lambda: nc.gpsimd.tensor_tensor(out=y_sb[:, m, :N], in0=y_sb[:, m, :N], in1=xm[:, m, 2:N + 2], op=ALU.add),
                         reads=[("y", m), "xm"], writes=[("y", m)])
                S.dma("sp", "y_st", yTv[:, :, o0:o0 + N], y_sb[:, :, :N], reads=[("y", m) for m in range(KD)], writes=[("yT", ti)])
            S.finish("sp", [("yT", ti) for ti in range(len(d2_tiles))])


def pack_params_dense(g_pre, g_post, g_fpre, g_fpost, conv_w, conv_b):
    def col(v):
        return np.ascontiguousarray(v.reshape(-1, P).T)
    cw = np.concatenate([col(conv_w[j]) for j in range(3)], axis=1)
    return np.ascontiguousarray(np.concatenate([col(g_pre), col(g_post), col(g_fpre), col(g_fpost), cw, col(conv_b)], axis=1).astype(np.float32))


def build_dense(dm):
    nc = bass.Bass("TRN2", target_bir_lowering=False)
    D, DFF, BW, NTOK = dm["D"], dm["DFF"], dm["BW"], dm["NTOK"]
    KD, KF = D // P, DFF // P
    npar = 4 * KD + 8 * KF
    a = {
        "xT": nc.dram_tensor("xT", [D, NTOK + 2], F32, kind="ExternalInput").ap(),
        "brT": nc.dram_tensor("brT", [BW, NTOK + 2], BF16, kind="ExternalInput").ap(),
        "w_gate": nc.dram_tensor("w_gate", [D, 3 * D], F32, kind="ExternalInput").ap(),
        "w_br": nc.dram_tensor("w_br", [BW, D], F32, kind="ExternalInput").ap(),
        "w_out": nc.dram_tensor("w_out", [D, D], F32, kind="ExternalInput").ap(),
        "w_up": nc.dram_tensor("w_up", [D, 2 * DFF], F32, kind="ExternalInput").ap(),
        "w_down": nc.dram_tensor("w_down", [DFF, D], F32, kind="ExternalInput").ap(),
        "pp": nc.dram_tensor("pp", [P, npar], F32, kind="ExternalInput").ap(),
        "xmid": nc.dram_tensor("xmid", [D, NTOK + 2], F32).ap(),
        "yT": nc.dram_tensor("yT", [D, NTOK], F32, kind="ExternalOutput").ap(),
    }
    with contextlib.ExitStack() as es:
        S = Sched(nc, es)
        emit_dense(nc, S, dm, a)
        print("dense instrs", S.ninstr, "sems", S.nsem)
    return nc

import contextlib
import numpy as np

P = 128
EPS = 1e-6
BIG = 30000.0
NFM = 912
NTM = 320
FM_GROUPS = [("gq", 0, 64), ("gk", 64, 64), ("gr", 128, 128), ("lr", 256, 16), ("mq", 272, 128), ("mk", 400, 128),
             ("sb", 528, 128), ("sc", 656, 128), ("sx", 784, 128)]
C_U, C_SL, C_TRI, C_ID, C_FILL, C_A0, C_PP = 0, 128, 256, 384, 512, 640, 768
NCONST = 768


def emit_mixer(nc, S, dm, a):
    D, T = dm["D"], dm["T"]
    KD = D // P
    NTILE = T // 512
    NKT = T // P
    o_gpre = C_PP
    o_wlr = o_gpre + KD
    o_gn = o_wlr + 64
    o_scw = o_gn + 1
    o_al = o_scw + 3
    ncc = o_al + 4
    xTv = a["xT"].rearrange("(k p) n -> p k n", p=P)
    brv = a["brT"].rearrange("(k p) n -> p k n", p=P)

    with contextlib.ExitStack() as es:
        E = es.enter_context
        cc = E(nc.sbuf_tensor("cc_sb", [P, ncc], F32))
        S.dma("sp", "cc", cc[:], a["cc"][:, :], writes=["cc"])
        shiftc = E(nc.sbuf_tensor("shiftc_sb", [P, 2 * NKT], F32))
        S.dma("sp", "shiftc", shiftc[:], a["shiftc"][:, :], writes=["shiftc"])
        causneg = E(nc.sbuf_tensor("causneg_sb", [P, 4, 512], BF16))
        S.dma("sp", "causneg", causneg[:], a["causneg"][:, :, :], writes=["causneg"])
        ident_bf = E(nc.sbuf_tensor("ident_bf", [P, P], BF16))
        ones_bf = E(nc.sbuf_tensor("ones_bf", [P, P], BF16))
        ones_f = E(nc.sbuf_tensor("ones_f", [P, P], F32))
        epsc = E(nc.sbuf_tensor("epsc_a", [P, 1], F32))
        S.op("dve", lambda: nc.vector.memset(ones_bf[:], 1.0), writes=["ones_bf"])
        S.op("dve", lambda: nc.vector.memset(ones_f[:], 1.0), writes=["ones_f"])
        S.op("dve", lambda: nc.vector.memset(epsc[:], EPS), writes=["epsc"])
        S.op("dve", lambda: nc.vector.tensor_copy(out=ident_bf[:], in_=cc[:, C_ID:C_ID + P]), reads=["cc"], writes=["ident_bf"])
        U = cc[:, C_U:C_U + P]
        SL = cc[:, C_SL:C_SL + P]
        TRI = cc[:, C_TRI:C_TRI + P]
        IDF = cc[:, C_ID:C_ID + P]
        wf = E(nc.sbuf_tensor("wf", [P, KD, NFM], BF16))
        wt = E(nc.sbuf_tensor("wt", [P, KD, NTM], BF16))
        wfv = a["w_f"].rearrange("(k p) n -> p k n", p=P)
        wtv = a["w_t"].rearrange("(k p) n -> p k n", p=P)
        for k0 in range(0, KD, 4):
            S.dma("pool", ("wf", k0), wf[:, k0:min(KD, k0 + 4), :], wfv[:, k0:min(KD, k0 + 4), :], writes=["wf"])
        S.dma("pool", "wt", wt[:], wtv[:, :, :], writes=["wt"])
        kaug = [E(nc.sbuf_tensor(f"kaug{h}", [P, T], BF16)) for h in range(2)]
        S.dma("sp", "kaug0", kaug[0][64:128, :], a["eblk"][:, :], writes=["kaug0e"])
        S.dma("sp", "kaug1", kaug[1][0:64, :], a["eblk"][:, :], writes=["kaug1e"])
        vaug0 = E(nc.sbuf_tensor("vaug0", [P, NKT, 65], BF16))
        vaug1 = E(nc.sbuf_tensor("vaug1", [P, NKT, 128], BF16))
        S.op("pool", lambda: nc.gpsimd.memset(vaug0[:, :, 64:65], 1.0), writes=["vaug0c"])
        S.op("pool", lambda: nc.gpsimd.memset(vaug1[:, :, 0:64], 0.0), writes=["vaug1c"])
        S.op("pool", lambda: nc.gpsimd.memset(vaug1[:, :, 0:1], 1.0), reads=["vaug1c"], writes=["vaug1c"])
        kmean = E(nc.sbuf_tensor("kmean", [P, 64], F32))
        S.op("dve", lambda: nc.vector.memset(kmean[:], 0.0), writes=["kmean"])
        x_st = [E(nc.sbuf_tensor(f"x_st{i}", [P, 512], F32)) for i in range(2)]
        xh = E(nc.sbuf_tensor("xh_a", [P, KD, 512], BF16))
        xsq = E(nc.sbuf_tensor("xsq_a", [P, KD, 512], BF16))
        rstd = E(nc.sbuf_tensor("rstd_a", [P, 512], F32))
        rstd_tok = E(nc.sbuf_tensor("rstd_tok", [P, 4], F32))
        fo = {nm: E(nc.sbuf_tensor("fo_" + nm, [P, 512], F32)) for nm in ("gq", "gk", "gr", "lr", "mq", "mk", "sb", "sc", "sx")}
        S.op("dve", lambda: nc.vector.memset(fo["lr"][0:32, :], 1.0), writes=["fo_lr1"])
        zc = E(nc.sbuf_tensor("zc", [P, 514], F32))
        S.op("dve", lambda: nc.vector.memset(zc[:, 0:2], 0.0), writes=["zc_h"])
        cva = E(nc.sbuf_tensor("cva", [P, 512], F32))
        gk_tok = E(nc.sbuf_tensor("gk_tok", [P, 4, 64], F32))
        gv = E(nc.sbuf_tensor("gv", [P, 4, 128], BF16))
        qaug = [E(nc.sbuf_tensor(f"qaug{h}", [P, 512], BF16)) for h in range(2)]
        mstage = E(nc.sbuf_tensor("mstage", [P, 4, P], F32))
        S.op("dve", lambda: nc.vector.memset(mstage[:], 0.0), writes=["mstage"])
        gm = E(nc.sbuf_tensor("gm", [P, 64], F32))
        top8 = E(nc.sbuf_tensor("top8", [P, 8], F32))
        thr = E(nc.sbuf_tensor("thr", [P, 1], F32))
        tsel = E(nc.sbuf_tensor("tsel", [P, 64], F32))
        pT = [E(nc.sbuf_tensor(f"pT{i}", [P, 512], BF16)) for i in range(3)]
        rs = E(nc.sbuf_tensor("rs", [P, 512], F32))
        br_sb = E(nc.sbuf_tensor("br_out", [P, 3, 512], BF16))
        la = E(nc.sbuf_tensor("la", [P, 4, 64], F32))
        er = E(nc.sbuf_tensor("er", [P, 4, 64], F32))
        qt_bf = E(nc.sbuf_tensor("qt_bf", [64, 512], BF16))
        kt_bf = E(nc.sbuf_tensor("kt_bf", [64, 512], BF16))
        kh_bf = E(nc.sbuf_tensor("kh_bf", [P, 4, 64], BF16))
        att_bf = E(nc.sbuf_tensor("att_bf", [P, P], BF16))
        Sst = E(nc.sbuf_tensor("Sst", [64, P], F32))
        Sbf = E(nc.sbuf_tensor("Sbf", [64, P], BF16))
        S.op("dve", lambda: nc.vector.memset(Sst[:], 0.0), writes=["Sst"])
        S.op("dve", lambda: nc.vector.memset(Sbf[:], 0.0), writes=["Sbf"])
        osq = E(nc.sbuf_tensor("osq", [P, 512], BF16))
        rstd_o = rs
        ot = cva
        o_sb = cva
        B = [E(nc.psum_tensor(f"bank{i}", [P, 512], F32)) for i in range(8)]

        def prologue(I):
            t0 = I * 512
            for k in range(KD):
                xs = x_st[k % 2]
                S.dma("sp", ("x", k % 2), xs[:], xTv[:, k, t0:t0 + 512], writes=[("x_st", k % 2)])
                S.op("act", lambda: nc.scalar.activation(out=xsq[:, k, :], in_=xs[:], func=AF.Square), reads=[("x_st", k % 2)], writes=[("xsq", k)])
                S.op("dve", lambda: nc.vector.tensor_scalar(out=xh[:, k, :], in0=xs[:], scalar1=cc[:, o_gpre + k:o_gpre + k + 1], scalar2=None, op0=ALU.mult),
                     reads=[("x_st", k % 2), "cc"], writes=[("xh", k)])

        def prologue_b():
            for k in range(KD):
                S.op("pe", lambda: nc.tensor.matmul(B[3][:, :], lhsT=ones_bf[:], rhs=xsq[:, k, :], start=(k == 0), stop=(k == KD - 1)),
                     reads=[("xsq", k), "ones_bf"], writes=["B3"], sig=(k == KD - 1))
            S.op("act", lambda: nc.scalar.activation(out=rstd[:], in_=B[3][:, :], func=AF.Ln, scale=1.0 / D, bias=epsc[:, 0:1]), reads=["B3", "epsc"], writes=["rstd"])
            S.op("act", lambda: nc.scalar.activation(out=rstd[:], in_=rstd[:], func=AF.Exp, scale=-0.5), reads=["rstd"], writes=["rstd"])
            for c in range(4):
                S.op("pe", lambda: nc.tensor.matmul(B[3][:, c:c + 1], lhsT=rstd[0:1, c * P:(c + 1) * P], rhs=ones_f[0:1, 0:1], start=True, stop=True),
                     reads=["rstd", "ones_f"], writes=["B3"])
            S.op("dve", lambda: nc.vector.tensor_copy(out=rstd_tok[:], in_=B[3][:, 0:4]), reads=["B3"], writes=["rstd_tok"])

        def fm_gen(I_, b0):
            banks = (0, 1) if b0 == 1 else (4, 5)
            for gi, (nm, c0, ncol) in enumerate(FM_GROUPS):
                bi_ = banks[gi % 2]
                pb = B[bi_]
                pn = "B%d" % bi_
                for k in range(KD):
                    S.op("pe", lambda: nc.tensor.matmul(pb[0:ncol, :], lhsT=wf[:, k, c0:c0 + ncol], rhs=xh[:, k, :], start=(k == 0), stop=(k == KD - 1)),
                         reads=[("xh", k), "wf"], writes=[pn], sig=(k == KD - 1))
                    yield
                dst = fo[nm]
                if nm in ("mq", "gq"):
                    S.op("dve", lambda: nc.vector.scalar_tensor_tensor(out=dst[0:ncol, :], in0=pb[0:ncol, :], scalar=0.125, in1=rstd[0:ncol, :], op0=ALU.mult, op1=ALU.mult),
                         reads=[pn, "rstd"], writes=["fo_" + nm])
                else:
                    extra = ["fo_lr1"] if nm == "lr" else []
                    S.op("dve", lambda: nc.vector.tensor_tensor(out=dst[0:ncol, :], in0=pb[0:ncol, :], in1=rstd[0:ncol, :], op=ALU.mult),
                         reads=[pn, "rstd"] + extra, writes=["fo_" + nm])
                yield

        prologue(0)
        prologue_b()
        for I in range(NTILE):
            t0 = I * 512
            nkt = 4 * I + 4

            def gating_gen(h):
                hs = slice(64 * h, 64 * h + 64)
                ms = slice(64 * (1 - h), 64 * (1 - h) + 64)
                for qs in range(4):
                    qt = 4 * I + qs
                    bi = qt // 2
                    S.op("pe", lambda: nc.tensor.matmul(B[3][:, 0:64], lhsT=fo["mq"][hs, qs * P:(qs + 1) * P], rhs=kmean[hs, :], start=True, stop=True),
                         reads=["fo_mq", "kmean"], writes=["B3"])
                    S.op("dve", lambda: nc.vector.tensor_tensor(out=gm[:], in0=B[3][:, 0:64], in1=cc[:, C_FILL + 63 - bi:C_FILL + 127 - bi], op=ALU.add), reads=["B3", "cc"], writes=["gm"])
                    yield
                    S.op("dve", lambda: nc.vector.max(out=top8[:], in_=gm[:]), reads=["gm"], writes=["top8"])
                    yield
                    S.op("dve", lambda: nc.vector.tensor_scalar(out=thr[:], in0=top8[:, 3:4], scalar1=-1e29, scalar2=None, op0=ALU.max), reads=["top8"], writes=["thr"])
                    yield
                    S.op("dve", lambda: nc.vector.tensor_scalar(out=tsel[:], in0=gm[:], scalar1=thr[:, 0:1], scalar2=BIG, op0=ALU.is_ge, op1=ALU.mult), reads=["gm", "thr"], writes=["tsel"])
                    yield
                    S.op("dve", lambda: nc.vector.scalar_tensor_tensor(out=mstage[:, qs, ms], in0=tsel[:], scalar=shiftc[:, h * NKT + qt:h * NKT + qt + 1], in1=cc[:, C_A0 + 64 * h:C_A0 + 64 * h + 64], op0=ALU.add, op1=ALU.add),
                         reads=["tsel", "shiftc", "cc"], writes=[("mstage", qs)])
                    yield
                for qs in range(4):
                    S.op("pe", lambda: nc.tensor.transpose(B[5][:, qs * P:(qs + 1) * P], mstage[:, qs, :], IDF), reads=[("mstage", qs), "cc"], writes=["B5"])
                yield
                S.op("act", lambda: nc.scalar.copy(out=qaug[h][ms, :], in_=B[5][ms, :]), reads=["B5"], writes=[f"qaug{h}m"])
                yield

            if I == 0:
                for _ in fm_gen(0, 1):
                    pass
            S.op("act", lambda: nc.scalar.copy(out=qaug[0][0:64, :], in_=fo["mq"][0:64, :]), reads=["fo_mq"], writes=["qaug0q"])
            S.op("act", lambda: nc.scalar.copy(out=qaug[1][64:128, :], in_=fo["mq"][64:128, :]), reads=["fo_mq"], writes=["qaug1q"])
            S.op("act", lambda: nc.scalar.copy(out=kaug[0][0:64, t0:t0 + 512], in_=fo["mk"][0:64, :]), reads=["fo_mk"], writes=["kaug0k"])
            S.op("act", lambda: nc.scalar.copy(out=kaug[1][64:128, t0:t0 + 512], in_=fo["mk"][64:128, :]), reads=["fo_mk"], writes=["kaug1k"])
            S.op("dve", lambda: nc.vector.tensor_reduce(out=kmean[:, 2 * I:2 * I + 2], in_=fo["mk"][:, :].rearrange("p (b n) -> p b n", n=256), axis=AX.X, op=ALU.add),
                 reads=["fo_mk"], writes=["kmean"])
            for c in range(4):
                for k in range(KD):
                    S.op("pe", lambda: nc.tensor.matmul(B[2][:, 0:NTM], lhsT=xh[:, k, c * P:(c + 1) * P], rhs=wt[:, k, :], start=(k == 0), stop=(k == KD - 1)),
                         reads=[("xh", k), "wt"], writes=["B2"], sig=(k == KD - 1))
                sc_ = rstd_tok[:, c:c + 1]
                S.op("act", lambda: nc.scalar.activation(out=gk_tok[:, c, :], in_=B[2][:, 0:64], func=AF.Identity, scale=sc_), reads=["B2", "rstd_tok"], writes=[("gk_tok", c)])
                S.op("act", lambda: nc.scalar.activation(out=gv[:, c, :], in_=B[2][:, 64:192], func=AF.Identity, scale=sc_), reads=["B2", "rstd_tok"], writes=[("gv", c)])
                S.op("act", lambda: nc.scalar.activation(out=vaug0[:, 4 * I + c, 0:64], in_=B[2][:, 192:256], func=AF.Identity, scale=sc_), reads=["B2", "rstd_tok"], writes=["vaug0"])
                S.op("act", lambda: nc.scalar.activation(out=vaug1[:, 4 * I + c, 64:128], in_=B[2][:, 256:320], func=AF.Identity, scale=sc_), reads=["B2", "rstd_tok"], writes=["vaug1"])
            S.op("pool", lambda: nc.gpsimd.tensor_tensor(out=zc[:, 2:514], in0=fo["sc"][:, :], in1=fo["sx"][:, :], op=ALU.mult), reads=["fo_sc", "fo_sx"], writes=["zc"])
            scw = lambda j: cc[:, o_scw + j:o_scw + j + 1]
            S.op("dve", lambda: nc.vector.tensor_scalar(out=cva[:], in0=zc[:, 2:514], scalar1=scw(2), scalar2=None, op0=ALU.mult), reads=["zc", "cc"], writes=["cva"])
            S.op("dve", lambda: nc.vector.scalar_tensor_tensor(out=cva[:], in0=zc[:, 1:513], scalar=scw(1), in1=cva[:], op0=ALU.mult, op1=ALU.add), reads=["zc", "zc_h", "cc", "cva"], writes=["cva"])
            S.op("dve", lambda: nc.vector.scalar_tensor_tensor(out=cva[:], in0=zc[:, 0:512], scalar=scw(0), in1=cva[:], op0=ALU.mult, op1=ALU.add), reads=["zc", "zc_h", "cc", "cva"], writes=["cva"])
            S.op("pool", lambda: nc.gpsimd.tensor_tensor(out=br_sb[:, 2, :], in0=cva[:], in1=fo["sb"][:, :], op=ALU.mult), reads=["cva", "fo_sb"], writes=["br_c"])
            S.op("pool", lambda: nc.gpsimd.tensor_copy(out=zc[:, 0:2], in_=zc[:, 512:514]), reads=["zc", "zc_h"], writes=["zc_h"])
            S.op("act", lambda: nc.scalar.activation(out=fo["gr"][:, :], in_=fo["gr"][:, :], func=AF.Silu), reads=["fo_gr"], writes=["fo_gr"])
            eb = rs[0:64, :]
            enb = cva[0:64, :]
            for c in range(4):
                S.op("pe", lambda: nc.tensor.matmul(B[0][:, c * 64:(c + 1) * 64], lhsT=fo["lr"][0:32, c * P:(c + 1) * P], rhs=cc[0:32, o_wlr:o_wlr + 64], start=True, stop=True),
                     reads=["fo_lr", "fo_lr1", "cc"], writes=["B0"], sig=(c == 3))
            S.op("act", lambda: nc.scalar.activation(out=la[:].rearrange("p c d -> p (c d)"), in_=B[0][:, 0:256], func=AF.Exp, scale=-1.0), reads=["B0"], writes=["la"])
            S.op("act", lambda: nc.scalar.activation(out=la[:].rearrange("p c d -> p (c d)"), in_=la[:].rearrange("p c d -> p (c d)"), func=AF.Ln, bias=1.0), reads=["la"], writes=["la"])
            for c in range(4):
                S.op("pe", lambda: nc.tensor.matmul(B[1][0:64, c * P:(c + 1) * P], lhsT=la[:, c, :], rhs=U, start=True, stop=True), reads=["la", "cc"], writes=["B1"], sig=(c == 3))
            for c in range(4):
                S.op("pe", lambda: nc.tensor.matmul(B[2][:, c * 64:(c + 1) * 64], lhsT=SL, rhs=la[:, c, :], start=True, stop=True), reads=["la", "cc"], writes=["B2"], sig=(c == 3))
            S.op("act", lambda: nc.scalar.activation(out=eb, in_=B[1][0:64, :], func=AF.Exp), reads=["B1", "rs"], writes=["rs"])
            S.op("act", lambda: nc.scalar.activation(out=enb, in_=B[1][0:64, :], func=AF.Exp, scale=-1.0), reads=["B1", "cva"], writes=["cva"])
            S.op("act", lambda: nc.scalar.activation(out=er[:].rearrange("p c d -> p (c d)"), in_=B[2][:, 0:256], func=AF.Exp), reads=["B2"], writes=["er"])
            S.op("dve", lambda: nc.vector.tensor_tensor(out=qt_bf[:], in0=fo["gq"][0:64, :], in1=eb, op=ALU.mult), reads=["fo_gq", "rs"], writes=["qt_bf"])
            S.op("dve", lambda: nc.vector.tensor_tensor(out=kt_bf[:], in0=fo["gk"][0:64, :], in1=enb, op=ALU.mult), reads=["fo_gk", "cva"], writes=["kt_bf"])
            S.op("pool", lambda: nc.gpsimd.tensor_tensor(out=kh_bf[:].rearrange("p c d -> p (c d)"), in0=gk_tok[:].rearrange("p c d -> p (c d)"), in1=er[:].rearrange("p c d -> p (c d)"), op=ALU.mult), reads=[("gk_tok", c) for c in range(4)] + ["er"], writes=["kh_bf"])
            gg0 = gating_gen(0)
            for c in range(4):
                cs = slice(c * P, (c + 1) * P)
                S.op("pe", lambda: nc.tensor.matmul(B[4][:, 0:128], lhsT=kt_bf[:, cs], rhs=qt_bf[:, cs], start=True, stop=True), reads=["kt_bf", "qt_bf"], writes=["B4"])
                S.op("dve", lambda: nc.vector.tensor_tensor(out=att_bf[:], in0=B[4][:, 0:128], in1=TRI, op=ALU.mult), reads=["B4", "cc"], writes=["att_bf"])
                S.op("pe", lambda: nc.tensor.matmul(B[4][0:64, 128:256], lhsT=kh_bf[:, c, :], rhs=gv[:, c, :], start=True, stop=True), reads=["kh_bf", ("gv", c)], writes=["B4"])
                S.op("pe", lambda: nc.tensor.matmul(B[5][:, cs], lhsT=gv[:, c, :], rhs=att_bf[:], start=True, stop=False), reads=[("gv", c), "att_bf"], writes=["B5"], sig=False)
                S.op("pe", lambda: nc.tensor.matmul(B[5][:, cs], lhsT=Sbf[:], rhs=qt_bf[:, cs], start=False, stop=True), reads=["Sbf", "qt_bf"], writes=["B5"])
                S.op("dve", lambda: nc.vector.scalar_tensor_tensor(out=Sst[:], in0=Sst[:], scalar=eb[:, c * P + 127:c * P + 128], in1=B[4][0:64, 128:256], op0=ALU.mult, op1=ALU.add),
                     reads=["Sst", "rs", "B4"], writes=["Sst"])
                S.op("pool", lambda: nc.gpsimd.tensor_copy(out=Sbf[:], in_=Sst[:]), reads=["Sst"], writes=["Sbf"])
                for _ in range(5):
                    next(gg0, None)
            S.op("act", lambda: nc.scalar.activation(out=osq[:], in_=B[5][:, :], func=AF.Square), reads=["B5"], writes=["osq"])
            S.op("pe", lambda: nc.tensor.matmul(B[3][:, :], lhsT=ones_bf[:], rhs=osq[:], start=True, stop=True), reads=["osq", "ones_bf"], writes=["B3"])
            S.op("act", lambda: nc.scalar.activation(out=rstd_o[:], in_=B[3][:, :], func=AF.Ln, scale=1.0 / P, bias=epsc[:, 0:1]), reads=["B3", "epsc"], writes=["rs"])
            S.op("act", lambda: nc.scalar.activation(out=rstd_o[:], in_=rstd_o[:], func=AF.Exp, scale=-0.5), reads=["rs"], writes=["rs"])
            S.op("dve", lambda: nc.vector.tensor_tensor(out=ot[:], in0=B[5][:, :], in1=rstd_o[:], op=ALU.mult), reads=["B5", "rs"], writes=["cva"])
            S.op("dve", lambda: nc.vector.scalar_tensor_tensor(out=br_sb[:, 0, :], in0=ot[:], scalar=cc[:, o_gn:o_gn + 1], in1=fo["gr"][:, :], op0=ALU.mult, op1=ALU.mult),
                 reads=["cva", "cc", "fo_gr"], writes=["br_a"])
            def attn_loop(h, filler):
                po = B[6 + h]
                nfill = 1 if h == 0 else max(1, -(-84 // nkt))

                def emit_s(kt):
                    ib = kt % 3
                    ps = B[ib]
                    pn = "B%d" % ib
                    diag = kt >= 4 * I
                    S.op("pe", lambda: nc.tensor.matmul(ps[:, :], lhsT=kaug[h][:, kt * P:(kt + 1) * P], rhs=qaug[h][:, :], start=True, stop=not diag),
                         reads=[f"kaug{h}k", f"kaug{h}e", f"qaug{h}q", f"qaug{h}m"], writes=[pn], sig=not diag)
                    if diag:
                        S.op("pe", lambda: nc.tensor.matmul(ps[:, :], lhsT=ident_bf[:], rhs=causneg[:, kt - 4 * I, :], start=False, stop=True),
                             reads=["ident_bf", "causneg"], writes=[pn])

                def emit_pv(kt):
                    ib = kt % 3
                    ps = B[ib]
                    pn = "B%d" % ib
                    S.op("act", lambda: nc.scalar.activation(out=pT[ib][:], in_=ps[:, :], func=AF.Exp, bias=cc[:, o_al + 2 * h + (kt % 2):o_al + 2 * h + (kt % 2) + 1]),
                         reads=[pn, "cc"], writes=[("pT", ib)])
                    if h == 0:
                        S.op("pe", lambda: nc.tensor.matmul(po[0:65, :], lhsT=vaug0[:, kt, :], rhs=pT[ib][:], start=(kt == 0), stop=(kt == nkt - 1)),
                             reads=[("pT", ib), "vaug0", "vaug0c"], writes=["B6"], sig=(kt == nkt - 1))
                    else:
                        S.op("pe", lambda: nc.tensor.matmul(po[:, :], lhsT=vaug1[:, kt, :], rhs=pT[ib][:], start=(kt == 0), stop=(kt == nkt - 1)),
                             reads=[("pT", ib), "vaug1", "vaug1c"], writes=["B7"], sig=(kt == nkt - 1))

                emit_s(0)
                if nkt > 1:
                    emit_s(1)
                for kt in range(nkt):
                    if kt + 2 < nkt:
                        emit_s(kt + 2)
                    emit_pv(kt)
                    if filler is not None:
                        for _ in range(nfill):
                            next(filler, None)

            def normalize(h):
                hs = slice(64 * h, 64 * h + 64)
                po = B[6 + h]
                srow = 64 if h == 0 else 0
                pnm = "B%d" % (6 + h)
                S.op("dve", lambda: nc.vector.reciprocal(out=rs[srow:srow + 1, :], in_=po[srow:srow + 1, :]), reads=[pnm], writes=["rs"])
                S.op("pe", lambda: nc.tensor.matmul(B[3][:, :], lhsT=ones_f[srow:srow + 1, :], rhs=rs[srow:srow + 1, :], start=True, stop=True), reads=["rs", "ones_f"], writes=["B3"])
                S.op("act", lambda: nc.scalar.copy(out=o_sb[hs, :], in_=po[hs, :]), reads=[pnm], writes=["cva"])
                S.op("dve", lambda: nc.vector.tensor_tensor(out=br_sb[hs, 1, :], in0=o_sb[hs, :], in1=B[3][hs, :], op=ALU.mult), reads=["cva", "B3"], writes=[("br_b", h)])

            for _ in gg0:
                pass
            if I + 1 < NTILE:
                prologue(I + 1)
            g1 = gating_gen(1)
            attn_loop(0, g1)
            for _ in g1:
                pass
            normalize(0)
            if I + 1 < NTILE:
                prologue_b()
            f1g = fm_gen(I + 1, 4) if I + 1 < NTILE else None
            attn_loop(1, f1g)
            if f1g is not None:
                for _ in f1g:
                    pass
            normalize(1)
            S.dma("sp", "br_st", brv[:, :, t0:t0 + 512], br_sb[:], reads=["br_a", ("br_b", 0), ("br_b", 1), "br_c"], writes=[("brT", I)])
        S.finish("sp", [("brT", I) for I in range(NTILE)])


def mixer_consts(D, T, slopes2, g_pre, wlr2_g, blr_g, gnorm, scw_g):
    KD = D // P
    NKT = T // P
    ncc = C_PP + KD + 64 + 1 + 3 + 4
    cc = np.zeros((P, ncc), np.float32)
    j = np.arange(P)[:, None]
    i = np.arange(P)[None, :]
    cc[:, C_U:C_U + P] = (j <= i) * (-1.0 / 16.0)
    cc[:, C_SL:C_SL + P] = (j > i) * (-1.0 / 16.0)
    cc[:, C_TRI:C_TRI + P] = (j <= i) * 1.0
    cc[:, C_ID:C_ID + P] = np.eye(P)
    c = np.arange(128)
    cc[:, C_FILL:C_FILL + 128] = np.where(c < 63, 0.0, np.where(c == 63, 1e30, -1e30))[None, :]
    il = np.arange(P)[:, None]
    b = np.arange(64)[None, :]
    for h in range(2):
        cc[:, C_A0 + 64 * h:C_A0 + 64 * h + 64] = -slopes2[h] * (il - 256.0 * b)
    o = C_PP
    cc[:, o:o + KD] = g_pre.reshape(KD, P).T
    o += KD
    cc[0:16, o:o + 64] = wlr2_g
    cc[16, o:o + 64] = blr_g
    o += 64
    cc[:, o] = gnorm
    o += 1
    cc[:, o:o + 3] = scw_g.T
    o += 3
    for h in range(2):
        for par in range(2):
            cc[:, o + 2 * h + par] = slopes2[h] * (128.0 * par + np.arange(P))
    shiftc = np.zeros((P, 2 * NKT), np.float32)
    for h in range(2):
        shiftc[:, h * NKT:(h + 1) * NKT] = (-BIG - slopes2[h] * 128.0 * np.arange(NKT))[None, :]
    import ml_dtypes
    jl = np.arange(P)[:, None, None]
    kk = np.arange(4)[None, :, None]
    ii = np.arange(512)[None, None, :]
    causneg = np.where(128 * kk + jl <= ii, 0.0, -BIG).astype(ml_dtypes.bfloat16)
    eblk = np.zeros((64, T), np.float32)
    for bb in range(min(64, T // 256)):
        eblk[bb, 256 * bb:256 * bb + 256] = 1.0
    return cc, shiftc, causneg, eblk.astype(ml_dtypes.bfloat16)


def build_mixer(dm):
    nc = bass.Bass("TRN2", target_bir_lowering=False)
    D, T = dm["D"], dm["T"]
    KD, NKT = D // P, T // P
    ncc = C_PP + KD + 64 + 1 + 3 + 4
    a = {
        "xT": nc.dram_tensor("xT", [D, T], F32, kind="ExternalInput").ap(),
        "w_f": nc.dram_tensor("w_f", [D, NFM], F32, kind="ExternalInput").ap(),
        "w_t": nc.dram_tensor("w_t", [D, NTM], F32, kind="ExternalInput").ap(),
        "cc": nc.dram_tensor("cc", [P, ncc], F32, kind="ExternalInput").ap(),
        "shiftc": nc.dram_tensor("shiftc", [P, 2 * NKT], F32, kind="ExternalInput").ap(),
        "causneg": nc.dram_tensor("causneg", [P, 4, 512], BF16, kind="ExternalInput").ap(),
        "eblk": nc.dram_tensor("eblk", [64, T], BF16, kind="ExternalInput").ap(),
        "brT": nc.dram_tensor("brT", [3 * P, T], BF16, kind="ExternalOutput").ap(),
    }
    with contextlib.ExitStack() as es:
        S = Sched(nc, es)
        emit_mixer(nc, S, dm, a)
        print("mixer instrs", S.ninstr, "sems", S.nsem)
    return nc


from concourse.bass_utils import run_bass_kernel_spmd
import ml_dtypes

D_MODEL, SEQ, BATCH, DEPTH, D_FF = 1024, 16384, 2, 2, 2816
ALIBI = 2.0 ** (-8.0 * (np.arange(8) + 1.0) / 8)
_NC_CACHE = {}


def _get(name, fn):
    if name not in _NC_CACHE:
        _NC_CACHE[name] = fn()
    return _NC_CACHE[name]


def _mixer_inputs(xT_b, l, g, w_in, gla_w_lr2, gla_b_lr, gla_norm, sc_conv_w, g_mix_pre):
    W = w_in[l]
    sl = lambda o, n: W[:, o + n * g: o + n * g + n]
    w_f = np.ascontiguousarray(np.concatenate([sl(0, 64), sl(256, 64), sl(1024, 128), W[:, 1536:1552], sl(1552, 128), sl(2064, 128),
                                               sl(3088, 128), sl(3600, 128), sl(4112, 128)], axis=1))
    w_t = np.ascontiguousarray(np.concatenate([sl(256, 64), sl(512, 128), sl(2576, 128)], axis=1))
    cc, shiftc, causneg, eblk = mixer_consts(D_MODEL, SEQ, ALIBI[2 * g:2 * g + 2], g_mix_pre[l], gla_w_lr2[l][:, 64 * g:64 * g + 64],
                                             gla_b_lr[l][64 * g:64 * g + 64], gla_norm[l], sc_conv_w[l][:, 128 * g:128 * g + 128])
    return {"xT": xT_b, "w_f": w_f, "w_t": w_t, "cc": cc, "shiftc": shiftc, "causneg": causneg, "eblk": eblk}


def kernel(x, g_mix_pre, w_in, gla_w_lr2, gla_b_lr, gla_norm, sc_conv_w, w_br_gla, w_br_moba, w_br_sc, w_out, g_mix_post,
           g_ffn_pre, ffn_w_up, ffn_conv_w, ffn_conv_b, ffn_w_down, g_ffn_post):
    f32 = lambda v: np.ascontiguousarray(np.asarray(v, dtype=np.float32))
    (x, g_mix_pre, w_in, gla_w_lr2, gla_b_lr, gla_norm, sc_conv_w, w_br_gla, w_br_moba, w_br_sc, w_out, g_mix_post,
     g_ffn_pre, ffn_w_up, ffn_conv_w, ffn_conv_b, ffn_w_down, g_ffn_post) = map(f32, (
        x, g_mix_pre, w_in, gla_w_lr2, gla_b_lr, gla_norm, sc_conv_w, w_br_gla, w_br_moba, w_br_sc, w_out, g_mix_post,
        g_ffn_pre, ffn_w_up, ffn_conv_w, ffn_conv_b, ffn_w_down, g_ffn_post))
    NTOK = SEQ // 4
    nc_a = _get("mixer", lambda: build_mixer(dict(D=D_MODEL, T=SEQ)))
    nc_b = _get("dense", lambda: build_dense(dict(D=D_MODEL, DFF=D_FF, BW=1536, NTOK=NTOK)))
    xT = [np.ascontiguousarray(x[b].T) for b in range(BATCH)]
    for l in range(DEPTH):
        in_maps = [_mixer_inputs(xT[c // 4], l, c % 4, w_in, gla_w_lr2, gla_b_lr, gla_norm, sc_conv_w, g_mix_pre) for c in range(8)]
        res = run_bass_kernel_spmd(nc_a, in_maps, core_ids=list(range(8)))
        brT = []
        for b in range(BATCH):
            parts = [np.asarray(res.results[4 * b + g]["brT"]) for g in range(4)]
            brT.append(np.concatenate([p[0:128] for p in parts] + [p[128:256] for p in parts] + [p[256:384] for p in parts], axis=0))
        w_gate = np.ascontiguousarray(w_in[l][:, 4624:7696])
        w_br = np.ascontiguousarray(np.concatenate([w_br_gla[l], w_br_moba[l], w_br_sc[l]], axis=0))
        pp = pack_params_dense(g_mix_pre[l], g_mix_post[l], g_ffn_pre[l], g_ffn_post[l], ffn_conv_w[l], ffn_conv_b[l])
        in_maps = []
        for c in range(8):
            b, j = c // 4, c % 4
            t0 = j * NTOK
            xs = np.zeros((D_MODEL, NTOK + 2), np.float32)
            bs = np.zeros((1536, NTOK + 2), ml_dtypes.bfloat16)
            lo = max(t0 - 2, 0)
            xs[:, 2 - (t0 - lo):] = xT[b][:, lo:t0 + NTOK]
            bs[:, 2 - (t0 - lo):] = brT[b][:, lo:t0 + NTOK]
            in_maps.append({"xT": xs, "brT": bs, "w_gate": w_gate, "w_br": w_br, "w_out": w_out[l], "w_up": ffn_w_up[l],
                            "w_down": ffn_w_down[l], "pp": pp})
        res = run_bass_kernel_spmd(nc_b, in_maps, core_ids=list(range(8)))
        xT = [np.ascontiguousarray(np.concatenate([np.asarray(res.results[4 * b + j]["yT"]) for j in range(4)], axis=1)) for b in range(BATCH)]
    return np.ascontiguousarray(np.stack([xT[b].T for b in range(BATCH)], axis=0).astype(np.float32))
```

```python
import concourse.bass as bass
import concourse.mybir as mybir

F32 = mybir.dt.float32
BF16 = mybir.dt.bfloat16
AF = mybir.ActivationFunctionType
ALU = mybir.AluOpType
AX = mybir.AxisListType

SEM_EPOCH = 40000


class Sched:
    def __init__(self, nc, es, same_engine_sync=True):
        self.nc = nc
        self.es = es
        self.same = same_engine_sync
        self.eng = {"pe": nc.tensor, "act": nc.scalar, "dve": nc.vector, "pool": nc.gpsimd, "sp": nc.sync}
        self.sem = {}
        self.cnt = {}
        self.nsem = 0
        for e in self.eng:
            self._new_sem(e)
        self.waited = {e: {} for e in self.eng}
        self.lastw = {}
        self.readers = {}
        self.dma_sems = {}
        self.ninstr = 0
        self.pending = {}

    def _new_sem(self, e):
        self.nsem += 1
        self.sem[e] = self.es.enter_context(self.nc.semaphore(f"s_{e}_{self.nsem}"))
        self.cnt[e] = 0

    def _wait(self, e, tok):
        if tok is None:
            return
        sem, val, src = tok
        if src == e and (not self.same or e == "pe"):
            return
        w = self.waited[e]
        k = id(sem)
        if w.get(k, 0) >= val:
            return
        self.eng[e].wait_ge(sem, val)
        w[k] = val

    def _deps(self, e, reads, writes):
        for r in reads:
            self._wait(e, self.lastw.get(r))
        for r in writes:
            self._wait(e, self.lastw.get(r))
            for t in list(self.readers.get(r, {}).values()):
                self._wait(e, t)

    def _commit(self, tok, reads, writes):
        for r in reads:
            self.readers.setdefault(r, {})[(tok[2], id(tok[0]))] = tok
        for r in writes:
            self.lastw[r] = tok
            self.readers[r] = {}

    def op(self, e, fn, reads=(), writes=(), sig=True):
        self._deps(e, reads, writes)
        if self.cnt[e] >= SEM_EPOCH and not self.pending.get(e):
            self._new_sem(e)
        ins = fn()
        if sig:
            self.cnt[e] += 1
            ins.then_inc(self.sem[e], 1)
            tok = (self.sem[e], self.cnt[e], e)
            self.pending[e] = False
        else:
            tok = (self.sem[e], self.cnt[e] + 1, e)
            self.pending[e] = True
        self._commit(tok, reads, writes)
        self.ninstr += 1
        return tok

    def dma(self, q, key, out, in_, reads=(), writes=()):
        self._deps(q, reads, writes)
        st = self.dma_sems.get(key)
        if st is None or st[1] >= SEM_EPOCH:
            self.nsem += 1
            st = [self.es.enter_context(self.nc.semaphore(f"d_{self.nsem}")), 0]
            self.dma_sems[key] = st
        ins = self.eng[q].dma_start(out=out, in_=in_)
        st[1] += 16
        ins.then_inc(st[0], 16)
        tok = (st[0], st[1], "dma")
        self._commit(tok, reads, writes)
        self.ninstr += 1
        return tok

    def finish(self, e, resources):
        for r in resources:
            self._wait(e, self.lastw.get(r))

    def barrier(self):
        toks = [(self.sem[e], self.cnt[e], e) for e in self.eng if self.cnt[e] > 0]
        toks += [(st[0], st[1], "dma") for st in self.dma_sems.values()]
        for e in self.eng:
            for t in toks:
                self._wait(e, t)

import contextlib
import numpy as np

P = 128
EPS = 1e-6


def split_even(n, maxn):
    k = -(-n // maxn)
    base, rem = divmod(n, k)
    out, s = [], 0
    for i in range(k):
        sz = base + (1 if i < rem else 0)
        out.append((s, sz))
        s += sz
    return out


def load_w(S, nc, wt, wd, nk, qname="pool", key="w"):
    v = wd.rearrange("(k p) n -> p k n", p=P)
    n = wd.shape[1]
    step = max(1, 4096 // n)
    for k0 in range(0, nk, step):
        k1 = min(nk, k0 + step)
        S.dma(qname, (key, k0 % 4), wt[:, k0:k1, :], v[:, k0:k1, :], writes=[(key, k0)])
    return [(key, k0) for k0 in range(0, nk, step)]


def rstd_from_psum(S, nc, out, ps, D, eps_col, reads, wres):
    S.op("act", lambda: nc.scalar.activation(out=out, in_=ps, func=AF.Ln, scale=1.0 / D, bias=eps_col), reads=reads, writes=[wres])
    S.op("act", lambda: nc.scalar.activation(out=out, in_=out, func=AF.Exp, scale=-0.5), reads=[wres], writes=[wres])


def emit_dense(nc, S, dm, a, n1max=456, n2max=376):
    D, DFF, BW, NTOK = dm["D"], dm["DFF"], dm["BW"], dm["NTOK"]
    KD, KB, KF = D // P, BW // P, DFF // P
    KBB = KB // 3
    NT = NTOK + 2
    o_gpre, o_gpost, o_gfpre, o_gfpost = 0, KD, 2 * KD, 3 * KD
    o_cw = 4 * KD
    o_cb = o_cw + 3 * 2 * KF
    npar = o_cb + 2 * KF
    xTv = a["xT"].rearrange("(k p) n -> p k n", p=P)
    brTv = a["brT"].rearrange("(k p) n -> p k n", p=P)
    xmv = a["xmid"].rearrange("(k p) n -> p k n", p=P)
    yTv = a["yT"].rearrange("(k p) n -> p k n", p=P)

    with contextlib.ExitStack() as es0:
        E0 = es0.enter_context
        pp = E0(nc.sbuf_tensor("pp_sb", [P, npar], F32))
        ones = E0(nc.sbuf_tensor("ones", [P, P], BF16))
        epsc = E0(nc.sbuf_tensor("epsc", [P, 1], F32))
        S.dma("sp", "pp", pp[:], a["pp"][:, :], writes=["pp"])
        S.op("dve", lambda: nc.vector.memset(ones[:], 1.0), writes=["ones"])
        S.op("dve", lambda: nc.vector.memset(epsc[:], EPS), writes=["epsc"])
        d1_tiles = split_even(NT, n1max)
        N1 = max(n for _, n in d1_tiles)
        with contextlib.ExitStack() as es:
            E = es.enter_context
            wg = E(nc.sbuf_tensor("wg", [P, KD, 3 * D], BF16))
            wbr = E(nc.sbuf_tensor("wbr", [P, KB, D], BF16))
            wo = E(nc.sbuf_tensor("wo", [P, KD, D], BF16))
            r_wg = load_w(S, nc, wg, a["w_gate"], KD, key="wg")
            r_wbr = load_w(S, nc, wbr, a["w_br"], KB, key="wbr")
            r_wo = load_w(S, nc, wo, a["w_out"], KD, key="wo")
            x_sb = E(nc.sbuf_tensor("x_sb", [P, KD, N1], F32))
            br_sb = E(nc.sbuf_tensor("br_sb", [P, KB, N1], BF16))
            xsq = E(nc.sbuf_tensor("xsq", [P, KD, N1], BF16))
            xh = E(nc.sbuf_tensor("xh", [P, KD, N1], BF16))
            rstd = E(nc.sbuf_tensor("rstd", [P, N1], F32))
            rstd2 = E(nc.sbuf_tensor("rstd2", [P, N1], F32))
            gl = [E(nc.sbuf_tensor(f"gl{i}", [P, N1], F32)) for i in range(3)]
            tm = [E(nc.sbuf_tensor(f"tm{i}", [P, N1], F32)) for i in range(3)]
            mf = [E(nc.sbuf_tensor(f"mf{i}", [P, N1], F32)) for i in range(2)]
            mbf = E(nc.sbuf_tensor("mbf", [P, KD, N1], BF16))
            z_sb = E(nc.sbuf_tensor("z_sb", [P, KD, N1], F32))
            ps_st = E(nc.psum_tensor("ps_st", [P, 512], F32))
            ps_g = [E(nc.psum_tensor(f"ps_g{i}", [P, 512], F32)) for i in range(3)]
            ps_p = [E(nc.psum_tensor(f"ps_p{i}", [P, 512], F32)) for i in range(3)]
            ps_z = E(nc.psum_tensor("ps_z", [P, 512], F32))
            it = 0
            x_sb2 = E(nc.sbuf_tensor("x_sb2", [P, KD, N1], F32))
            x_sbs = [x_sb, x_sb2]

            def d1_pro(ti):
                c0, N = d1_tiles[ti]
                par = ti % 2
                xs = x_sbs[par]
                S.dma("sp", ("x", par), xs[:, :, :N], xTv[:, :, c0:c0 + N], writes=[("x_sb", par)])
                S.dma("sp", "br", br_sb[:, :, :N], brTv[:, :, c0:c0 + N], writes=["br_sb"])
                S.op("act", lambda: nc.scalar.activation(out=xsq[:, :, :N], in_=xs[:, :, :N], func=AF.Square), reads=[("x_sb", par)], writes=[("zsq", q) for q in range(KD)])
                for k in range(KD):
                    S.op("act", lambda: nc.scalar.activation(out=xh[:, k, :N], in_=xs[:, k, :N], func=AF.Identity, scale=pp[:, o_gpre + k:o_gpre + k + 1]),
                         reads=[("x_sb", par), "pp"], writes=[("xh", k)])
                for k in range(KD):
                    S.op("pe", lambda: nc.tensor.matmul(ps_st[:, :N], lhsT=ones[:], rhs=xsq[:, k, :N], start=(k == 0), stop=(k == KD - 1)),
                         reads=[("zsq", k), "ones"], writes=["ps_st"], sig=(k == KD - 1))
                rstd_from_psum(S, nc, rstd[:, :N], ps_st[:, :N], D, epsc[:, 0:1], ["ps_st", "epsc"], "rstd")

            d1_pro(0)
            for ti, (c0, N) in enumerate(d1_tiles):
                xs = x_sbs[ti % 2]
                def d1_head(m, b, i3):
                    pg, pq, g_t = ps_g[i3], ps_p[i3], gl[i3]
                    for k in range(KD):
                        S.op("pe", lambda: nc.tensor.matmul(pg[:, :N], lhsT=wg[:, k, b * D + m * P:b * D + (m + 1) * P], rhs=xh[:, k, :N], start=(k == 0), stop=(k == KD - 1)),
                             reads=[("xh", k)] + r_wg, writes=[("pg", i3)], sig=(k == KD - 1))
                    for k in range(KBB):
                        S.op("pe", lambda: nc.tensor.matmul(pq[:, :N], lhsT=wbr[:, b * KBB + k, m * P:(m + 1) * P], rhs=br_sb[:, b * KBB + k, :N], start=(k == 0), stop=(k == KBB - 1)),
                             reads=["br_sb"] + r_wbr, writes=[("pq", i3)], sig=(k == KBB - 1))
                    S.op("dve", lambda: nc.vector.tensor_tensor(out=g_t[:, :N], in0=pg[:, :N], in1=rstd[:, :N], op=ALU.mult),
                         reads=[("pg", i3), "rstd"], writes=[("gl", i3)])
                    S.op("act", lambda: nc.scalar.activation(out=g_t[:, :N], in_=g_t[:, :N], func=AF.Sigmoid), reads=[("gl", i3)], writes=[("gl", i3)])

                def d1_tail(m, b, i3):
                    pq, g_t, t_t = ps_p[i3], gl[i3], tm[i3]
                    mfm = mf[m % 2]
                    if b == 0:
                        S.op("dve", lambda: nc.vector.tensor_tensor(out=mfm[:, :N], in0=g_t[:, :N], in1=pq[:, :N], op=ALU.mult),
                             reads=[("gl", i3), ("pq", i3)], writes=[("mf", m % 2)])
                    else:
                        S.op("dve", lambda: nc.vector.tensor_tensor(out=t_t[:, :N], in0=g_t[:, :N], in1=pq[:, :N], op=ALU.mult),
                             reads=[("gl", i3), ("pq", i3)], writes=[("tm", i3)])
                        dst = mfm[:, :N] if b == 1 else mbf[:, m, :N]
                        wr = [("mf", m % 2)] if b == 1 else [("mbf", m)]
                        S.op("pool", lambda: nc.gpsimd.tensor_tensor(out=dst, in0=mfm[:, :N], in1=t_t[:, :N], op=ALU.add),
                             reads=[("mf", m % 2), ("tm", i3)], writes=wr)

                groups = [(m, b) for m in range(KD) for b in range(3)]
                prev = None
                for (m, b) in groups:
                    i3 = it % 3
                    it += 1
                    d1_head(m, b, i3)
                    if prev is not None:
                        d1_tail(*prev)
                    prev = (m, b, i3)
                d1_tail(*prev)
                if ti + 1 < len(d1_tiles):
                    d1_pro(ti + 1)
                for m in range(KD):
                    pz = ps_g[m % 3]
                    for k in range(KD):
                        S.op("pe", lambda: nc.tensor.matmul(pz[:, :N], lhsT=wo[:, k, m * P:(m + 1) * P], rhs=mbf[:, k, :N], start=(k == 0), stop=(k == KD - 1)),
                             reads=[("mbf", k)] + r_wo, writes=[("pg", m % 3)], sig=(k == KD - 1))
                    S.op("act", lambda: nc.scalar.copy(out=z_sb[:, m, :N], in_=pz[:, :N]), reads=[("pg", m % 3)], writes=[("z", m)])
                    S.op("dve", lambda: nc.vector.tensor_tensor(out=xsq[:, m, :N], in0=z_sb[:, m, :N], in1=z_sb[:, m, :N], op=ALU.mult), reads=[("z", m)], writes=[("zsq", m)])
                for k in range(KD):
                    S.op("pe", lambda: nc.tensor.matmul(ps_st[:, :N], lhsT=ones[:], rhs=xsq[:, k, :N], start=(k == 0), stop=(k == KD - 1)),
                         reads=[("zsq", k), "ones"], writes=["ps_st"], sig=(k == KD - 1))
                rstd_from_psum(S, nc, rstd2[:, :N], ps_st[:, :N], D, epsc[:, 0:1], ["ps_st", "epsc"], "rstd2")
                for m in range(KD):
                    S.op("dve", lambda: nc.vector.scalar_tensor_tensor(out=z_sb[:, m, :N], in0=z_sb[:, m, :N], scalar=pp[:, o_gpost + m:o_gpost + m + 1], in1=rstd2[:, :N], op0=ALU.mult, op1=ALU.mult),
                         reads=[("z", m), "rstd2", "pp"], writes=[("z", m)])
                    if m % 2 == 0:
                        S.op("pool", lambda: nc.gpsimd.tensor_tensor(out=z_sb[:, m, :N], in0=z_sb[:, m, :N], in1=xs[:, m, :N], op=ALU.add),
                             reads=[("z", m), ("x_sb", ti % 2)], writes=[("z", m)])
                    else:
                        S.op("dve", lambda: nc.vector.tensor_tensor(out=z_sb[:, m, :N], in0=z_sb[:, m, :N], in1=xs[:, m, :N], op=ALU.add),
                             reads=[("z", m), ("x_sb", ti % 2)], writes=[("z", m)])
                S.dma("sp", "xm_st", xmv[:, :, c0:c0 + N], z_sb[:, :, :N], reads=[("z", m) for m in range(KD)], writes=[("xmid", ti)])
        S.barrier()
        d2_tiles = split_even(NTOK, n2max)
        N2 = max(n for _, n in d2_tiles)
        with contextlib.ExitStack() as es:
            E = es.enter_context
            wup = E(nc.sbuf_tensor("wup", [P, KD, 2 * DFF], BF16))
            wdn = E(nc.sbuf_tensor("wdn", [P, KF, D], BF16))
            r_wup = load_w(S, nc, wup, a["w_up"], KD, key="wup")
            r_wdn = load_w(S, nc, wdn, a["w_down"], KF, key="wdn")
            xm = E(nc.sbuf_tensor("xm", [P, KD, N2 + 2], F32))
            xsq = E(nc.sbuf_tensor("xsq2", [P, KD, N2 + 2], BF16))
            hn = E(nc.sbuf_tensor("hn", [P, KD, N2 + 2], BF16))
            rstd = E(nc.sbuf_tensor("rstd_b", [P, N2 + 2], F32))
            rstd3 = E(nc.sbuf_tensor("rstd3", [P, N2 + 2], F32))
            a_sb = E(nc.sbuf_tensor("a_sb", [P, KF, N2], BF16))
            cvg = [E(nc.sbuf_tensor(f"cvg{i}", [P, N2], F32)) for i in range(3)]
            cvu = [E(nc.sbuf_tensor(f"cvu{i}", [P, N2], F32)) for i in range(3)]
            y_sb = E(nc.sbuf_tensor("y_sb", [P, KD, N2], F32))
            ps_st = E(nc.psum_tensor("ps_st2", [P, 512], F32))
            ps_ug = [E(nc.psum_tensor(f"ps_ug{i}", [P, 512], F32)) for i in range(3)]
            ps_uu = [E(nc.psum_tensor(f"ps_uu{i}", [P, 512], F32)) for i in range(3)]
            ps_y = [E(nc.psum_tensor(f"ps_y{i}", [P, 512], F32)) for i in range(1)]
            all_xmid = [("xmid", ti) for ti in range(len(d1_tiles))]
            xm2 = E(nc.sbuf_tensor("xm2", [P, KD, N2 + 2], F32))
            xms = [xm, xm2]

            def d2_pro(ti):
                o0, N = d2_tiles[ti]
                M = N + 2
                par = ti % 2
                xmc = xms[par]
                S.dma("sp", ("xm", par), xmc[:, :, :M], xmv[:, :, o0:o0 + M], reads=all_xmid, writes=[("xm", par)])
                S.op("act", lambda: nc.scalar.activation(out=xsq[:, :, :M], in_=xmc[:, :, :M], func=AF.Square), reads=[("xm", par)], writes=[("ysq", q) for q in range(KD)])
                for k in range(KD):
                    S.op("pe", lambda: nc.tensor.matmul(ps_st[:, :M], lhsT=ones[:], rhs=xsq[:, k, :M], start=(k == 0), stop=(k == KD - 1)),
                         reads=[("ysq", k), "ones"], writes=["ps_st2"], sig=(k == KD - 1))
                rstd_from_psum(S, nc, rstd[:, :M], ps_st[:, :M], D, epsc[:, 0:1], ["ps_st2", "epsc"], "rstd_b")
                for k in range(KD):
                    S.op("dve", lambda: nc.vector.scalar_tensor_tensor(out=hn[:, k, :M], in0=xmc[:, k, :M], scalar=pp[:, o_gfpre + k:o_gfpre + k + 1], in1=rstd[:, :M], op0=ALU.mult, op1=ALU.mult),
                         reads=[("xm", par), "rstd_b", "pp"], writes=[("hn", k)])

            d2_pro(0)
            for ti, (o0, N) in enumerate(d2_tiles):
                M = N + 2
                xmc = xms[ti % 2]
                def d2_head(c):
                    i2 = c % 3
                    for half, pst, cv, col, nm in ((0, ps_ug[i2], cvg[i2], c, "ug"), (1, ps_uu[i2], cvu[i2], KF + c, "uu")):
                        for k in range(KD):
                            S.op("pe", lambda: nc.tensor.matmul(pst[:, :M], lhsT=wup[:, k, col * P:(col + 1) * P], rhs=hn[:, k, :M], start=(k == 0), stop=(k == KD - 1)),
                                 reads=[("hn", k)] + r_wup, writes=[(nm, i2)], sig=(k == KD - 1))
                        cw = lambda j: pp[:, o_cw + j * 2 * KF + col:o_cw + j * 2 * KF + col + 1]
                        S.op("act", lambda: nc.scalar.activation(out=cv[:, :N], in_=pst[:, 2:N + 2], func=AF.Identity, scale=cw(2), bias=pp[:, o_cb + col:o_cb + col + 1]),
                             reads=[(nm, i2), "pp"], writes=[("cv" + nm, i2)])
                        S.op("dve", lambda: nc.vector.scalar_tensor_tensor(out=cv[:, :N], in0=pst[:, 1:N + 1], scalar=cw(1), in1=cv[:, :N], op0=ALU.mult, op1=ALU.add),
                             reads=[(nm, i2), "pp", ("cv" + nm, i2)], writes=[("cv" + nm, i2)])
                        S.op("dve", lambda: nc.vector.scalar_tensor_tensor(out=cv[:, :N], in0=pst[:, 0:N], scalar=cw(0), in1=cv[:, :N], op0=ALU.mult, op1=ALU.add),
                             reads=[(nm, i2), "pp", ("cv" + nm, i2)], writes=[("cv" + nm, i2)])

                def d2_tail(c):
                    i2 = c % 3
                    S.op("pool", lambda: nc.gpsimd.tensor_tensor(out=cvu[i2][:, :N], in0=cvu[i2][:, :N], in1=cvg[i2][:, :N], op=ALU.mult),
                         reads=[("cvug", i2), ("cvuu", i2)], writes=[("cvuu", i2)])
                    S.op("act", lambda: nc.scalar.activation(out=cvg[i2][:, :N], in_=cvg[i2][:, :N], func=AF.Sigmoid), reads=[("cvug", i2)], writes=[("cvug", i2)])
                    S.op("pool", lambda: nc.gpsimd.tensor_tensor(out=a_sb[:, c, :N], in0=cvu[i2][:, :N], in1=cvg[i2][:, :N], op=ALU.mult),
                         reads=[("cvug", i2), ("cvuu", i2)], writes=[("a", c)])

                for c in range(KF + 1):
                    if c < KF:
                        d2_head(c)
                    if c >= 1:
                        d2_tail(c - 1)
                if ti + 1 < len(d2_tiles):
                    d2_pro(ti + 1)
                for m in range(KD):
                    py = ps_ug[m % 3]
                    for c in range(KF):
                        S.op("pe", lambda: nc.tensor.matmul(py[:, :N], lhsT=wdn[:, c, m * P:(m + 1) * P], rhs=a_sb[:, c, :N], start=(c == 0), stop=(c == KF - 1)),
                             reads=[("a", c)] + r_wdn, writes=[("ug", m % 3)], sig=(c == KF - 1))
                    S.op("act", lambda: nc.scalar.copy(out=y_sb[:, m, :N], in_=py[:, :N]), reads=[("ug", m % 3)], writes=[("y", m)])
                    S.op("dve", lambda: nc.vector.tensor_tensor(out=xsq[:, m, :N], in0=y_sb[:, m, :N], in1=y_sb[:, m, :N], op=ALU.mult), reads=[("y", m)], writes=[("ysq", m)])
                for k in range(KD):
                    S.op("pe", lambda: nc.tensor.matmul(ps_st[:, :N], lhsT=ones[:], rhs=xsq[:, k, :N], start=(k == 0), stop=(k == KD - 1)),
                         reads=[("ysq", k), "ones"], writes=["ps_st2"], sig=(k == KD - 1))
                rstd_from_psum(S, nc, rstd3[:, :N], ps_st[:, :N], D, epsc[:, 0:1], ["ps_st2", "epsc"], "rstd3")
                for m in range(KD):
                    S.op("dve", lambda: nc.vector.scalar_tensor_tensor(out=y_sb[:, m, :N], in0=y_sb[:, m, :N], scalar=pp[:, o_gfpost + m:o_gfpost + m + 1], in1=rstd3[:, :N], op0=ALU.mult, op1=ALU.mult),
                         reads=[("y", m), "rstd3", "pp"], writes=[("y", m)])
                    if m % 2 == 0:
                        S.op("pool", lambda: nc.gpsimd.tensor_tensor(out=y_sb[:, m, :N], in0=y_sb[:, m, :N], in1=xmc[:, m, 2:N + 2], op=ALU.add),
                             reads=[("y", m), ("xm", ti % 2)], writes=[("y", m)])
                    else:
                        S.op("dve", lambda: nc.vector.tensor_tensor(out=y_sb[:, m, :N], in0=y_sb[:, m, :N], in1=xmc[:, m, 2:N + 2], op=ALU.add),
                             reads=[("y", m), ("xm", ti % 2)], writes=[("y", m)])
                S.dma("sp", "y_st", yTv[:, :, o0:o0 + N], y_sb[:, :, :N], reads=[("y", m) for m in range(KD)], writes=[("yT", ti)])
            S.finish("sp", [("yT", ti) for ti in range(len(d2_tiles))])


def pack_params_dense(g_pre, g_post, g_fpre, g_fpost, conv_w, conv_b):
    def col(v):
        return np.ascontiguousarray(v.reshape(-1, P).T)
    cw = np.concatenate([col(conv_w[j]) for j in range(3)], axis=1)
    return np.ascontiguousarray(np.concatenate([col(g_pre), col(g_post), col(g_fpre), col(g_fpost), cw, col(conv_b)], axis=1).astype(np.float32))


def build_dense(dm):
    nc = bass.Bass("TRN2", target_bir_lowering=False)
    D, DFF, BW, NTOK = dm["D"], dm["DFF"], dm["BW"], dm["NTOK"]
    KD, KF = D // P, DFF // P
    npar = 4 * KD + 8 * KF
    a = {
        "xT": nc.dram_tensor("xT", [D, NTOK + 2], F32, kind="ExternalInput").ap(),
        "brT": nc.dram_tensor("brT", [BW, NTOK + 2], BF16, kind="ExternalInput").ap(),
        "w_gate": nc.dram_tensor("w_gate", [D, 3 * D], F32, kind="ExternalInput").ap(),
        "w_br": nc.dram_tensor("w_br", [BW, D], F32, kind="ExternalInput").ap(),
        "w_out": nc.dram_tensor("w_out", [D, D], F32, kind="ExternalInput").ap(),
        "w_up": nc.dram_tensor("w_up", [D, 2 * DFF], F32, kind="ExternalInput").ap(),
        "w_down": nc.dram_tensor("w_down", [DFF, D], F32, kind="ExternalInput").ap(),
        "pp": nc.dram_tensor("pp", [P, npar], F32, kind="ExternalInput").ap(),
        "xmid": nc.dram_tensor("xmid", [D, NTOK + 2], F32).ap(),
        "yT": nc.dram_tensor("yT", [D, NTOK], F32, kind="ExternalOutput").ap(),
    }
    with contextlib.ExitStack() as es:
        S = Sched(nc, es)
        emit_dense(nc, S, dm, a)
        print("dense instrs", S.ninstr, "sems", S.nsem)
    return nc

import contextlib
import numpy as np

P = 128
EPS = 1e-6
BIG = 30000.0
NFM = 912
NTM = 320
FM_GROUPS = [("gq", 0, 64), ("gk", 64, 64), ("gr", 128, 128), ("lr", 256, 16), ("mq", 272, 128), ("mk", 400, 128),
             ("sb", 528, 128), ("sc", 656, 128), ("sx", 784, 128)]
C_U, C_SL, C_TRI, C_ID, C_FILL, C_A0, C_PP = 0, 128, 256, 384, 512, 640, 768
NCONST = 768


def emit_mixer(nc, S, dm, a):
    D, T = dm["D"], dm["T"]
    KD = D // P
    NTILE = T // 512
    NKT = T // P
    o_gpre = C_PP
    o_wlr = o_gpre + KD
    o_gn = o_wlr + 64
    o_scw = o_gn + 1
    o_al = o_scw + 3
    ncc = o_al + 4
    xTv = a["xT"].rearrange("(k p) n -> p k n", p=P)
    brv = a["brT"].rearrange("(k p) n -> p k n", p=P)

    with contextlib.ExitStack() as es:
        E = es.enter_context
        cc = E(nc.sbuf_tensor("cc_sb", [P, ncc], F32))
        S.dma("sp", "cc", cc[:], a["cc"][:, :], writes=["cc"])
        shiftc = E(nc.sbuf_tensor("shiftc_sb", [P, 2 * NKT], F32))
        S.dma("sp", "shiftc", shiftc[:], a["shiftc"][:, :], writes=["shiftc"])
        causneg = E(nc.sbuf_tensor("causneg_sb", [P, 4, 512], BF16))
        S.dma("sp", "causneg", causneg[:], a["causneg"][:, :, :], writes=["causneg"])
        ident_bf = E(nc.sbuf_tensor("ident_bf", [P, P], BF16))
        ones_bf = E(nc.sbuf_tensor("ones_bf", [P, P], BF16))
        ones_f = E(nc.sbuf_tensor("ones_f", [P, P], F32))
        epsc = E(nc.sbuf_tensor("epsc_a", [P, 1], F32))
        S.op("dve", lambda: nc.vector.memset(ones_bf[:], 1.0), writes=["ones_bf"])
        S.op("dve", lambda: nc.vector.memset(ones_f[:], 1.0), writes=["ones_f"])
        S.op("dve", lambda: nc.vector.memset(epsc[:], EPS), writes=["epsc"])
        S.op("dve", lambda: nc.vector.tensor_copy(out=ident_bf[:], in_=cc[:, C_ID:C_ID + P]), reads=["cc"], writes=["ident_bf"])
        U = cc[:, C_U:C_U + P]
        SL = cc[:, C_SL:C_SL + P]
        TRI = cc[:, C_TRI:C_TRI + P]
        IDF = cc[:, C_ID:C_ID + P]
        wf = E(nc.sbuf_tensor("wf", [P, KD, NFM], BF16))
        wt = E(nc.sbuf_tensor("wt", [P, KD, NTM], BF16))
        wfv = a["w_f"].rearrange("(k p) n -> p k n", p=P)
        wtv = a["w_t"].rearrange("(k p) n -> p k n", p=P)
        for k0 in range(0, KD, 4):
            S.dma("pool", ("wf", k0), wf[:, k0:min(KD, k0 + 4), :], wfv[:, k0:min(KD, k0 + 4), :], writes=["wf"])
        S.dma("pool", "wt", wt[:], wtv[:, :, :], writes=["wt"])
        kaug = [E(nc.sbuf_tensor(f"kaug{h}", [P, T], BF16)) for h in range(2)]
        S.dma("sp", "kaug0", kaug[0][64:128, :], a["eblk"][:, :], writes=["kaug0e"])
        S.dma("sp", "kaug1", kaug[1][0:64, :], a["eblk"][:, :], writes=["kaug1e"])
        vaug0 = E(nc.sbuf_tensor("vaug0", [P, NKT, 65], BF16))
        vaug1 = E(nc.sbuf_tensor("vaug1", [P, NKT, 128], BF16))
        S.op("pool", lambda: nc.gpsimd.memset(vaug0[:, :, 64:65], 1.0), writes=["vaug0c"])
        S.op("pool", lambda: nc.gpsimd.memset(vaug1[:, :, 0:64], 0.0), writes=["vaug1c"])
        S.op("pool", lambda: nc.gpsimd.memset(vaug1[:, :, 0:1], 1.0), reads=["vaug1c"], writes=["vaug1c"])
        kmean = E(nc.sbuf_tensor("kmean", [P, 64], F32))
        S.op("dve", lambda: nc.vector.memset(kmean[:], 0.0), writes=["kmean"])
        x_st = [E(nc.sbuf_tensor(f"x_st{i}", [P, 512], F32)) for i in range(2)]
        xh = E(nc.sbuf_tensor("xh_a", [P, KD, 512], BF16))
        xsq = E(nc.sbuf_tensor("xsq_a", [P, KD, 512], BF16))
        rstd = E(nc.sbuf_tensor("rstd_a", [P, 512], F32))
        rstd_tok = E(nc.sbuf_tensor("rstd_tok", [P, 4], F32))
        fo = {nm: E(nc.sbuf_tensor("fo_" + nm, [P, 512], F32)) for nm in ("gq", "gk", "gr", "lr", "mq", "mk", "sb", "sc", "sx")}
        S.op("dve", lambda: nc.vector.memset(fo["lr"][0:32, :], 1.0), writes=["fo_lr1"])
        zc = E(nc.sbuf_tensor("zc", [P, 514], F32))
        S.op("dve", lambda: nc.vector.memset(zc[:, 0:2], 0.0), writes=["zc_h"])
        cva = E(nc.sbuf_tensor("cva", [P, 512], F32))
        gk_tok = E(nc.sbuf_tensor("gk_tok", [P, 4, 64], F32))
        gv = E(nc.sbuf_tensor("gv", [P, 4, 128], BF16))
        qaug = [E(nc.sbuf_tensor(f"qaug{h}", [P, 512], BF16)) for h in range(2)]
        mstage = E(nc.sbuf_tensor("mstage", [P, 4, P], F32))
        S.op("dve", lambda: nc.vector.memset(mstage[:], 0.0), writes=["mstage"])
        gm = E(nc.sbuf_tensor("gm", [P, 64], F32))
        top8 = E(nc.sbuf_tensor("top8", [P, 8], F32))
        thr = E(nc.sbuf_tensor("thr", [P, 1], F32))
        tsel = E(nc.sbuf_tensor("tsel", [P, 64], F32))
        pT = [E(nc.sbuf_tensor(f"pT{i}", [P, 512], BF16)) for i in range(3)]
        rs = E(nc.sbuf_tensor("rs", [P, 512], F32))
        br_sb = E(nc.sbuf_tensor("br_out", [P, 3, 512], BF16))
        la = E(nc.sbuf_tensor("la", [P, 4, 64], F32))
        er = E(nc.sbuf_tensor("er", [P, 4, 64], F32))
        qt_bf = E(nc.sbuf_tensor("qt_bf", [64, 512], BF16))
        kt_bf = E(nc.sbuf_tensor("kt_bf", [64, 512], BF16))
        kh_bf = E(nc.sbuf_tensor("kh_bf", [P, 4, 64], BF16))
        att_bf = E(nc.sbuf_tensor("att_bf", [P, P], BF16))
        Sst = E(nc.sbuf_tensor("Sst", [64, P], F32))
        Sbf = E(nc.sbuf_tensor("Sbf", [64, P], BF16))
        S.op("dve", lambda: nc.vector.memset(Sst[:], 0.0), writes=["Sst"])
        S.op("dve", lambda: nc.vector.memset(Sbf[:], 0.0), writes=["Sbf"])
        osq = E(nc.sbuf_tensor("osq", [P, 512], BF16))
        rstd_o = rs
        ot = cva
        o_sb = cva
        B = [E(nc.psum_tensor(f"bank{i}", [P, 512], F32)) for i in range(8)]

        def prologue(I):
            t0 = I * 512
            for k in range(KD):
                xs = x_st[k % 2]
                S.dma("sp", ("x", k % 2), xs[:], xTv[:, k, t0:t0 + 512], writes=[("x_st", k % 2)])
                S.op("act", lambda: nc.scalar.activation(out=xsq[:, k, :], in_=xs[:], func=AF.Square), reads=[("x_st", k % 2)], writes=[("xsq", k)])
                S.op("dve", lambda: nc.vector.tensor_scalar(out=xh[:, k, :], in0=xs[:], scalar1=cc[:, o_gpre + k:o_gpre + k + 1], scalar2=None, op0=ALU.mult),
                     reads=[("x_st", k % 2), "cc"], writes=[("xh", k)])

        def prologue_b():
            for k in range(KD):
                S.op("pe", lambda: nc.tensor.matmul(B[3][:, :], lhsT=ones_bf[:], rhs=xsq[:, k, :], start=(k == 0), stop=(k == KD - 1)),
                     reads=[("xsq", k), "ones_bf"], writes=["B3"], sig=(k == KD - 1))
            S.op("act", lambda: nc.scalar.activation(out=rstd[:], in_=B[3][:, :], func=AF.Ln, scale=1.0 / D, bias=epsc[:, 0:1]), reads=["B3", "epsc"], writes=["rstd"])
            S.op("act", lambda: nc.scalar.activation(out=rstd[:], in_=rstd[:], func=AF.Exp, scale=-0.5), reads=["rstd"], writes=["rstd"])
            for c in range(4):
                S.op("pe", lambda: nc.tensor.matmul(B[3][:, c:c + 1], lhsT=rstd[0:1, c * P:(c + 1) * P], rhs=ones_f[0:1, 0:1], start=True, stop=True),
                     reads=["rstd", "ones_f"], writes=["B3"])
            S.op("dve", lambda: nc.vector.tensor_copy(out=rstd_tok[:], in_=B[3][:, 0:4]), reads=["B3"], writes=["rstd_tok"])

        def fm_gen(I_, b0):
            banks = (0, 1) if b0 == 1 else (4, 5)
            for gi, (nm, c0, ncol) in enumerate(FM_GROUPS):
                bi_ = banks[gi % 2]
                pb = B[bi_]
                pn = "B%d" % bi_
                for k in range(KD):
                    S.op("pe", lambda: nc.tensor.matmul(pb[0:ncol, :], lhsT=wf[:, k, c0:c0 + ncol], rhs=xh[:, k, :], start=(k == 0), stop=(k == KD - 1)),
                         reads=[("xh", k), "wf"], writes=[pn], sig=(k == KD - 1))
                    yield
                dst = fo[nm]
                if nm in ("mq", "gq"):
                    S.op("dve", lambda: nc.vector.scalar_tensor_tensor(out=dst[0:ncol, :], in0=pb[0:ncol, :], scalar=0.125, in1=rstd[0:ncol, :], op0=ALU.mult, op1=ALU.mult),
                         reads=[pn, "rstd"], writes=["fo_" + nm])
                else:
                    extra = ["fo_lr1"] if nm == "lr" else []
                    S.op("dve", lambda: nc.vector.tensor_tensor(out=dst[0:ncol, :], in0=pb[0:ncol, :], in1=rstd[0:ncol, :], op=ALU.mult),
                         reads=[pn, "rstd"] + extra, writes=["fo_" + nm])
                yield

        prologue(0)
        prologue_b()
        for I in range(NTILE):
            t0 = I * 512
            nkt = 4 * I + 4

            def gating_gen(h):
                hs = slice(64 * h, 64 * h + 64)
                ms = slice(64 * (1 - h), 64 * (1 - h) + 64)
                for qs in range(4):
                    qt = 4 * I + qs
                    bi = qt // 2
                    S.op("pe", lambda: nc.tensor.matmul(B[3][:, 0:64], lhsT=fo["mq"][hs, qs * P:(qs + 1) * P], rhs=kmean[hs, :], start=True, stop=True),
                         reads=["fo_mq", "kmean"], writes=["B3"])
                    S.op("dve", lambda: nc.vector.tensor_tensor(out=gm[:], in0=B[3][:, 0:64], in1=cc[:, C_FILL + 63 - bi:C_FILL + 127 - bi], op=ALU.add), reads=["B3", "cc"], writes=["gm"])
                    yield
                    S.op("dve", lambda: nc.vector.max(out=top8[:], in_=gm[:]), reads=["gm"], writes=["top8"])
                    yield
                    S.op("dve", lambda: nc.vector.tensor_scalar(out=thr[:], in0=top8[:, 3:4], scalar1=-1e29, scalar2=None, op0=ALU.max), reads=["top8"], writes=["thr"])
                    yield
                    S.op("dve", lambda: nc.vector.tensor_scalar(out=tsel[:], in0=gm[:], scalar1=thr[:, 0:1], scalar2=BIG, op0=ALU.is_ge, op1=ALU.mult), reads=["gm", "thr"], writes=["tsel"])
                    yield
                    S.op("dve", lambda: nc.vector.scalar_tensor_tensor(out=mstage[:, qs, ms], in0=tsel[:], scalar=shiftc[:, h * NKT + qt:h * NKT + qt + 1], in1=cc[:, C_A0 + 64 * h:C_A0 + 64 * h + 64], op0=ALU.add, op1=ALU.add),
                         reads=["tsel", "shiftc", "cc"], writes=[("mstage", qs)])
                    yield
                for qs in range(4):
                    S.op("pe", lambda: nc.tensor.transpose(B[5][:, qs * P:(qs + 1) * P], mstage[:, qs, :], IDF), reads=[("mstage", qs), "cc"], writes=["B5"])
                yield
                S.op("act", lambda: nc.scalar.copy(out=qaug[h][ms, :], in_=B[5][ms, :]), reads=["B5"], writes=[f"qaug{h}m"])
                yield

            if I == 0:
                for _ in fm_gen(0, 1):
                    pass
            S.op("act", lambda: nc.scalar.copy(out=qaug[0][0:64, :], in_=fo["mq"][0:64, :]), reads=["fo_mq"], writes=["qaug0q"])
            S.op("act", lambda: nc.scalar.copy(out=qaug[1][64:128, :], in_=fo["mq"][64:128, :]), reads=["fo_mq"], writes=["qaug1q"])
            S.op("act", lambda: nc.scalar.copy(out=kaug[0][0:64, t0:t0 + 512], in_=fo["mk"][0:64, :]), reads=["fo_mk"], writes=["kaug0k"])
            S.op("act", lambda: nc.scalar.copy(out=kaug[1][64:128, t0:t0 + 512], in_=fo["mk"][64:128, :]), reads=["fo_mk"], writes=["kaug1k"])
            S.op("dve", lambda: nc.vector.tensor_reduce(out=kmean[:, 2 * I:2 * I + 2], in_=fo["mk"][:, :].rearrange("p (b n) -> p b n", n=256), axis=AX.X, op=ALU.add),
                 reads=["fo_mk"], writes=["kmean"])
            for c in range(4):
                for k in range(KD):
                    S.op("pe", lambda: nc.tensor.matmul(B[2][:, 0:NTM], lhsT=xh[:, k, c * P:(c + 1) * P], rhs=wt[:, k, :], start=(k == 0), stop=(k == KD - 1)),
                         reads=[("xh", k), "wt"], writes=["B2"], sig=(k == KD - 1))
                sc_ = rstd_tok[:, c:c + 1]
                S.op("act", lambda: nc.scalar.activation(out=gk_tok[:, c, :], in_=B[2][:, 0:64], func=AF.Identity, scale=sc_), reads=["B2", "rstd_tok"], writes=[("gk_tok", c)])
                S.op("act", lambda: nc.scalar.activation(out=gv[:, c, :], in_=B[2][:, 64:192], func=AF.Identity, scale=sc_), reads=["B2", "rstd_tok"], writes=[("gv", c)])
                S.op("act", lambda: nc.scalar.activation(out=vaug0[:, 4 * I + c, 0:64], in_=B[2][:, 192:256], func=AF.Identity, scale=sc_), reads=["B2", "rstd_tok"], writes=["vaug0"])
                S.op("act", lambda: nc.scalar.activation(out=vaug1[:, 4 * I + c, 64:128], in_=B[2][:, 256:320], func=AF.Identity, scale=sc_), reads=["B2", "rstd_tok"], writes=["vaug1"])
            S.op("pool", lambda: nc.gpsimd.tensor_tensor(out=zc[:, 2:514], in0=fo["sc"][:, :], in1=fo["sx"][:, :], op=ALU.mult), reads=["fo_sc", "fo_sx"], writes=["zc"])
            scw = lambda j: cc[:, o_scw + j:o_scw + j + 1]
            S.op("dve", lambda: nc.vector.tensor_scalar(out=cva[:], in0=zc[:, 2:514], scalar1=scw(2), scalar2=None, op0=ALU.mult), reads=["zc", "cc"], writes=["cva"])
            S.op("dve", lambda: nc.vector.scalar_tensor_tensor(out=cva[:], in0=zc[:, 1:513], scalar=scw(1), in1=cva[:], op0=ALU.mult, op1=ALU.add), reads=["zc", "zc_h", "cc", "cva"], writes=["cva"])
            S.op("dve", lambda: nc.vector.scalar_tensor_tensor(out=cva[:], in0=zc[:, 0:512], scalar=scw(0), in1=cva[:], op0=ALU.mult, op1=ALU.add), reads=["zc", "zc_h", "cc", "cva"], writes=["cva"])
            S.op("pool", lambda: nc.gpsimd.tensor_tensor(out=br_sb[:, 2, :], in0=cva[:], in1=fo["sb"][:, :], op=ALU.mult), reads=["cva", "fo_sb"], writes=["br_c"])
            S.op("pool", lambda: nc.gpsimd.tensor_copy(out=zc[:, 0:2], in_=zc[:, 512:514]), reads=["zc", "zc_h"], writes=["zc_h"])
            S.op("act", lambda: nc.scalar.activation(out=fo["gr"][:, :], in_=fo["gr"][:, :], func=AF.Silu), reads=["fo_gr"], writes=["fo_gr"])
            eb = rs[0:64, :]
            enb = cva[0:64, :]
            for c in range(4):
                S.op("pe", lambda: nc.tensor.matmul(B[0][:, c * 64:(c + 1) * 64], lhsT=fo["lr"][0:32, c * P:(c + 1) * P], rhs=cc[0:32, o_wlr:o_wlr + 64], start=True, stop=True),
                     reads=["fo_lr", "fo_lr1", "cc"], writes=["B0"], sig=(c == 3))
            S.op("act", lambda: nc.scalar.activation(out=la[:].rearrange("p c d -> p (c d)"), in_=B[0][:, 0:256], func=AF.Exp, scale=-1.0), reads=["B0"], writes=["la"])
            S.op("act", lambda: nc.scalar.activation(out=la[:].rearrange("p c d -> p (c d)"), in_=la[:].rearrange("p c d -> p (c d)"), func=AF.Ln, bias=1.0), reads=["la"], writes=["la"])
            for c in range(4):
                S.op("pe", lambda: nc.tensor.matmul(B[1][0:64, c * P:(c + 1) * P], lhsT=la[:, c, :], rhs=U, start=True, stop=True), reads=["la", "cc"], writes=["B1"], sig=(c == 3))
            for c in range(4):
                S.op("pe", lambda: nc.tensor.matmul(B[2][:, c * 64:(c + 1) * 64], lhsT=SL, rhs=la[:, c, :], start=True, stop=True), reads=["la", "cc"], writes=["B2"], sig=(c == 3))
            S.op("act", lambda: nc.scalar.activation(out=eb, in_=B[1][0:64, :], func=AF.Exp), reads=["B1", "rs"], writes=["rs"])
            S.op("act", lambda: nc.scalar.activation(out=enb, in_=B[1][0:64, :], func=AF.Exp, scale=-1.0), reads=["B1", "cva"], writes=["cva"])
            S.op("act", lambda: nc.scalar.activation(out=er[:].rearrange("p c d -> p (c d)"), in_=B[2][:, 0:256], func=AF.Exp), reads=["B2"], writes=["er"])
            S.op("dve", lambda: nc.vector.tensor_tensor(out=qt_bf[:], in0=fo["gq"][0:64, :], in1=eb, op=ALU.mult), reads=["fo_gq", "rs"], writes=["qt_bf"])
            S.op("dve", lambda: nc.vector.tensor_tensor(out=kt_bf[:], in0=fo["gk"][0:64, :], in1=enb, op=ALU.mult), reads=["fo_gk", "cva"], writes=["kt_bf"])
            S.op("pool", lambda: nc.gpsimd.tensor_tensor(out=kh_bf[:].rearrange("p c d -> p (c d)"), in0=gk_tok[:].rearrange("p c d -> p (c d)"), in1=er[:].rearrange("p c d -> p (c d)"), op=ALU.mult), reads=[("gk_tok", c) for c in range(4)] + ["er"], writes=["kh_bf"])
            gg0 = gating_gen(0)
            for c in range(4):
                cs = slice(c * P, (c + 1) * P)
                S.op("pe", lambda: nc.tensor.matmul(B[4][:, 0:128], lhsT=kt_bf[:, cs], rhs=qt_bf[:, cs], start=True, stop=True), reads=["kt_bf", "qt_bf"], writes=["B4"])
                S.op("dve", lambda: nc.vector.tensor_tensor(out=att_bf[:], in0=B[4][:, 0:128], in1=TRI, op=ALU.mult), reads=["B4", "cc"], writes=["att_bf"])
                S.op("pe", lambda: nc.tensor.matmul(B[4][0:64, 128:256], lhsT=kh_bf[:, c, :], rhs=gv[:, c, :], start=True, stop=True), reads=["kh_bf", ("gv", c)], writes=["B4"])
                S.op("pe", lambda: nc.tensor.matmul(B[5][:, cs], lhsT=gv[:, c, :], rhs=att_bf[:], start=True, stop=False), reads=[("gv", c), "att_bf"], writes=["B5"], sig=False)
                S.op("pe", lambda: nc.tensor.matmul(B[5][:, cs], lhsT=Sbf[:], rhs=qt_bf[:, cs], start=False, stop=True), reads=["Sbf", "qt_bf"], writes=["B5"])
                S.op("dve", lambda: nc.vector.scalar_tensor_tensor(out=Sst[:], in0=Sst[:], scalar=eb[:, c * P + 127:c * P + 128], in1=B[4][0:64, 128:256], op0=ALU.mult, op1=ALU.add),
                     reads=["Sst", "rs", "B4"], writes=["Sst"])
                S.op("pool", lambda: nc.gpsimd.tensor_copy(out=Sbf[:], in_=Sst[:]), reads=["Sst"], writes=["Sbf"])
                for _ in range(5):
                    next(gg0, None)
            S.op("act", lambda: nc.scalar.activation(out=osq[:], in_=B[5][:, :], func=AF.Square), reads=["B5"], writes=["osq"])
            S.op("pe", lambda: nc.tensor.matmul(B[3][:, :], lhsT=ones_bf[:], rhs=osq[:], start=True, stop=True), reads=["osq", "ones_bf"], writes=["B3"])
            S.op("act", lambda: nc.scalar.activation(out=rstd_o[:], in_=B[3][:, :], func=AF.Ln, scale=1.0 / P, bias=epsc[:, 0:1]), reads=["B3", "epsc"], writes=["rs"])
            S.op("act", lambda: nc.scalar.activation(out=rstd_o[:], in_=rstd_o[:], func=AF.Exp, scale=-0.5), reads=["rs"], writes=["rs"])
            S.op("dve", lambda: nc.vector.tensor_tensor(out=ot[:], in0=B[5][:, :], in1=rstd_o[:], op=ALU.mult), reads=["B5", "rs"], writes=["cva"])
            S.op("dve", lambda: nc.vector.scalar_tensor_tensor(out=br_sb[:, 0, :], in0=ot[:], scalar=cc[:, o_gn:o_gn + 1], in1=fo["gr"][:, :], op0=ALU.mult, op1=ALU.mult),
                 reads=["cva", "cc", "fo_gr"], writes=["br_a"])
            def attn_loop(h, filler):
                po = B[6 + h]
                nfill = 1 if h == 0 else max(1, -(-84 // nkt))

                def emit_s(kt):
                    ib = kt % 3
                    ps = B[ib]
                    pn = "B%d" % ib
                    diag = kt >= 4 * I
                    S.op("pe", lambda: nc.tensor.matmul(ps[:, :], lhsT=kaug[h][:, kt * P:(kt + 1) * P], rhs=qaug[h][:, :], start=True, stop=not diag),
                         reads=[f"kaug{h}k", f"kaug{h}e", f"qaug{h}q", f"qaug{h}m"], writes=[pn], sig=not diag)
                    if diag:
                        S.op("pe", lambda: nc.tensor.matmul(ps[:, :], lhsT=ident_bf[:], rhs=causneg[:, kt - 4 * I, :], start=False, stop=True),
                             reads=["ident_bf", "causneg"], writes=[pn])

                def emit_pv(kt):
                    ib = kt % 3
                    ps = B[ib]
                    pn = "B%d" % ib
                    S.op("act", lambda: nc.scalar.activation(out=pT[ib][:], in_=ps[:, :], func=AF.Exp, bias=cc[:, o_al + 2 * h + (kt % 2):o_al + 2 * h + (kt % 2) + 1]),
                         reads=[pn, "cc"], writes=[("pT", ib)])
                    if h == 0:
                        S.op("pe", lambda: nc.tensor.matmul(po[0:65, :], lhsT=vaug0[:, kt, :], rhs=pT[ib][:], start=(kt == 0), stop=(kt == nkt - 1)),
                             reads=[("pT", ib), "vaug0", "vaug0c"], writes=["B6"], sig=(kt == nkt - 1))
                    else:
                        S.op("pe", lambda: nc.tensor.matmul(po[:, :], lhsT=vaug1[:, kt, :], rhs=pT[ib][:], start=(kt == 0), stop=(kt == nkt - 1)),
                             reads=[("pT", ib), "vaug1", "vaug1c"], writes=["B7"], sig=(kt == nkt - 1))

                emit_s(0)
                if nkt > 1:
                    emit_s(1)
                for kt in range(nkt):
                    if kt + 2 < nkt:
                        emit_s(kt + 2)
                    emit_pv(kt)
                    if filler is not None:
                        for _ in range(nfill):
                            next(filler, None)

            def normalize(h):
                hs = slice(64 * h, 64 * h + 64)
                po = B[6 + h]
                srow = 64 if h == 0 else 0
                pnm = "B%d" % (6 + h)
                S.op("dve", lambda: nc.vector.reciprocal(out=rs[srow:srow + 1, :], in_=po[srow:srow + 1, :]), reads=[pnm], writes=["rs"])
                S.op("pe", lambda: nc.tensor.matmul(B[3][:, :], lhsT=ones_f[srow:srow + 1, :], rhs=rs[srow:srow + 1, :], start=True, stop=True), reads=["rs", "ones_f"], writes=["B3"])
                S.op("act", lambda: nc.scalar.copy(out=o_sb[hs, :], in_=po[hs, :]), reads=[pnm], writes=["cva"])
                S.op("dve", lambda: nc.vector.tensor_tensor(out=br_sb[hs, 1, :], in0=o_sb[hs, :], in1=B[3][hs, :], op=ALU.mult), reads=["cva", "B3"], writes=[("br_b", h)])

            for _ in gg0:
                pass
            if I + 1 < NTILE:
                prologue(I + 1)
            g1 = gating_gen(1)
            attn_loop(0, g1)
            for _ in g1:
                pass
            normalize(0)
            if I + 1 < NTILE:
                prologue_b()
            f1g = fm_gen(I + 1, 4) if I + 1 < NTILE else None
            attn_loop(1, f1g)
            if f1g is not None:
                for _ in f1g:
                    pass
            normalize(1)
            S.dma("sp", "br_st", brv[:, :, t0:t0 + 512], br_sb[:], reads=["br_a", ("br_b", 0), ("br_b", 1), "br_c"], writes=[("brT", I)])
        S.finish("sp", [("brT", I) for I in range(NTILE)])


def mixer_consts(D, T, slopes2, g_pre, wlr2_g, blr_g, gnorm, scw_g):
    KD = D // P
    NKT = T // P
    ncc = C_PP + KD + 64 + 1 + 3 + 4
    cc = np.zeros((P, ncc), np.float32)
    j = np.arange(P)[:, None]
    i = np.arange(P)[None, :]
    cc[:, C_U:C_U + P] = (j <= i) * (-1.0 / 16.0)
    cc[:, C_SL:C_SL + P] = (j > i) * (-1.0 / 16.0)
    cc[:, C_TRI:C_TRI + P] = (j <= i) * 1.0
    cc[:, C_ID:C_ID + P] = np.eye(P)
    c = np.arange(128)
    cc[:, C_FILL:C_FILL + 128] = np.where(c < 63, 0.0, np.where(c == 63, 1e30, -1e30))[None, :]
    il = np.arange(P)[:, None]
    b = np.arange(64)[None, :]
    for h in range(2):
        cc[:, C_A0 + 64 * h:C_A0 + 64 * h + 64] = -slopes2[h] * (il - 256.0 * b)
    o = C_PP
    cc[:, o:o + KD] = g_pre.reshape(KD, P).T
    o += KD
    cc[0:16, o:o + 64] = wlr2_g
    cc[16, o:o + 64] = blr_g
    o += 64
    cc[:, o] = gnorm
    o += 1
    cc[:, o:o + 3] = scw_g.T
    o += 3
    for h in range(2):
        for par in range(2):
            cc[:, o + 2 * h + par] = slopes2[h] * (128.0 * par + np.arange(P))
    shiftc = np.zeros((P, 2 * NKT), np.float32)
    for h in range(2):
        shiftc[:, h * NKT:(h + 1) * NKT] = (-BIG - slopes2[h] * 128.0 * np.arange(NKT))[None, :]
    import ml_dtypes
    jl = np.arange(P)[:, None, None]
    kk = np.arange(4)[None, :, None]
    ii = np.arange(512)[None, None, :]
    causneg = np.where(128 * kk + jl <= ii, 0.0, -BIG).astype(ml_dtypes.bfloat16)
    eblk = np.zeros((64, T), np.float32)
    for bb in range(min(64, T // 256)):
        eblk[bb, 256 * bb:256 * bb + 256] = 1.0
    return cc, shiftc, causneg, eblk.astype(ml_dtypes.bfloat16)


def build_mixer(dm):
    nc = bass.Bass("TRN2", target_bir_lowering=False)
    D, T = dm["D"], dm["T"]
    KD, NKT = D // P, T // P
    ncc = C_PP + KD + 64 + 1 + 3 + 4
    a = {
        "xT": nc.dram_tensor("xT", [D, T], F32, kind="ExternalInput").ap(),
        "w_f": nc.dram_tensor("w_f", [D, NFM], F32, kind="ExternalInput").ap(),
        "w_t": nc.dram_tensor("w_t", [D, NTM], F32, kind="ExternalInput").ap(),
        "cc": nc.dram_tensor("cc", [P, ncc], F32, kind="ExternalInput").ap(),
        "shiftc": nc.dram_tensor("shiftc", [P, 2 * NKT], F32, kind="ExternalInput").ap(),
        "causneg": nc.dram_tensor("causneg", [P, 4, 512], BF16, kind="ExternalInput").ap(),
        "eblk": nc.dram_tensor("eblk", [64, T], BF16, kind="ExternalInput").ap(),
        "brT": nc.dram_tensor("brT", [3 * P, T], BF16, kind="ExternalOutput").ap(),
    }
    with contextlib.ExitStack() as es:
        S = Sched(nc, es)
        emit_mixer(nc, S, dm, a)
        print("mixer instrs", S.ninstr, "sems", S.nsem)
    return nc


from concourse.bass_utils import run_bass_kernel_spmd
import ml_dtypes

D_MODEL, SEQ, BATCH, DEPTH, D_FF = 1024, 16384, 2, 2, 2816
ALIBI = 2.0 ** (-8.0 * (np.arange(8) + 1.0) / 8)
_NC_CACHE = {}


def _get(name, fn):
    if name not in _NC_CACHE:
        _NC_CACHE[name] = fn()
    return _NC_CACHE[name]


def _mixer_inputs(xT_b, l, g, w_in, gla_w_lr2, gla_b_lr, gla_norm, sc_conv_w, g_mix_pre):
    W = w_in[l]
    sl = lambda o, n: W[:, o + n * g: o + n * g + n]
    w_f = np.ascontiguousarray(np.concatenate([sl(0, 64), sl(256, 64), sl(1024, 128), W[:, 1536:1552], sl(1552, 128), sl(2064, 128),
                                               sl(3088, 128), sl(3600, 128), sl(4112, 128)], axis=1))
    w_t = np.ascontiguousarray(np.concatenate([sl(256, 64), sl(512, 128), sl(2576, 128)], axis=1))
    cc, shiftc, causneg, eblk = mixer_consts(D_MODEL, SEQ, ALIBI[2 * g:2 * g + 2], g_mix_pre[l], gla_w_lr2[l][:, 64 * g:64 * g + 64],
                                             gla_b_lr[l][64 * g:64 * g + 64], gla_norm[l], sc_conv_w[l][:, 128 * g:128 * g + 128])
    return {"xT": xT_b, "w_f": w_f, "w_t": w_t, "cc": cc, "shiftc": shiftc, "causneg": causneg, "eblk": eblk}


def kernel(x, g_mix_pre, w_in, gla_w_lr2, gla_b_lr, gla_norm, sc_conv_w, w_br_gla, w_br_moba, w_br_sc, w_out, g_mix_post,
           g_ffn_pre, ffn_w_up, ffn_conv_w, ffn_conv_b, ffn_w_down, g_ffn_post):
    f32 = lambda v: np.ascontiguousarray(np.asarray(v, dtype=np.float32))
    (x, g_mix_pre, w_in, gla_w_lr2, gla_b_lr, gla_norm, sc_conv_w, w_br_gla, w_br_moba, w_br_sc, w_out, g_mix_post,
     g_ffn_pre, ffn_w_up, ffn_conv_w, ffn_conv_b, ffn_w_down, g_ffn_post) = map(f32, (
        x, g_mix_pre, w_in, gla_w_lr2, gla_b_lr, gla_norm, sc_conv_w, w_br_gla, w_br_moba, w_br_sc, w_out, g_mix_post,
        g_ffn_pre, ffn_w_up, ffn_conv_w, ffn_conv_b, ffn_w_down, g_ffn_post))
    NTOK = SEQ // 4
    nc_a = _get("mixer", lambda: build_mixer(dict(D=D_MODEL, T=SEQ)))
    nc_b = _get("dense", lambda: build_dense(dict(D=D_MODEL, DFF=D_FF, BW=1536, NTOK=NTOK)))
    xT = [np.ascontiguousarray(x[b].T) for b in range(BATCH)]
    for l in range(DEPTH):
        in_maps = [_mixer_inputs(xT[c // 4], l, c % 4, w_in, gla_w_lr2, gla_b_lr, gla_norm, sc_conv_w, g_mix_pre) for c in range(8)]
        res = run_bass_kernel_spmd(nc_a, in_maps, core_ids=list(range(8)))
        brT = []
        for b in range(BATCH):
            parts = [np.asarray(res.results[4 * b + g]["brT"]) for g in range(4)]
            brT.append(np.concatenate([p[0:128] for p in parts] + [p[128:256] for p in parts] + [p[256:384] for p in parts], axis=0))
        w_gate = np.ascontiguousarray(w_in[l][:, 4624:7696])
        w_br = np.ascontiguousarray(np.concatenate([w_br_gla[l], w_br_moba[l], w_br_sc[l]], axis=0))
        pp = pack_params_dense(g_mix_pre[l], g_mix_post[l], g_ffn_pre[l], g_ffn_post[l], ffn_conv_w[l], ffn_conv_b[l])
        in_maps = []
        for c in range(8):
            b, j = c // 4, c % 4
            t0 = j * NTOK
            xs = np.zeros((D_MODEL, NTOK + 2), np.float32)
            bs = np.zeros((1536, NTOK + 2), ml_dtypes.bfloat16)
            lo = max(t0 - 2, 0)
            xs[:, 2 - (t0 - lo):] = xT[b][:, lo:t0 + NTOK]
            bs[:, 2 - (t0 - lo):] = brT[b][:, lo:t0 + NTOK]
            in_maps.append({"xT": xs, "brT": bs, "w_gate": w_gate, "w_br": w_br, "w_out": w_out[l], "w_up": ffn_w_up[l],
                            "w_down": ffn_w_down[l], "pp": pp})
        res = run_bass_kernel_spmd(nc_b, in_maps, core_ids=list(range(8)))
        xT = [np.ascontiguousarray(np.concatenate([np.asarray(res.results[4 * b + j]["yT"]) for j in range(4)], axis=1)) for b in range(BATCH)]
    return np.ascontiguousarray(np.stack([xT[b].T for b in range(BATCH)], axis=0).astype(np.float32))
```

```python
import concourse.bass as bass
import concourse.mybir as mybir

F32 = mybir.dt.float32
BF16 = mybir.dt.bfloat16
AF = mybir.ActivationFunctionType
ALU = mybir.AluOpType
AX = mybir.AxisListType

SEM_EPOCH = 40000


class Sched:
    def __init__(self, nc, es, same_engine_sync=True):
        self.nc = nc
        self.es = es
        self.same = same_engine_sync
        self.eng = {"pe": nc.tensor, "act": nc.scalar, "dve": nc.vector, "pool": nc.gpsimd, "sp": nc.sync}
        self.sem = {}
        self.cnt = {}
        self.nsem = 0
        for e in self.eng:
            self._new_sem(e)
        self.waited = {e: {} for e in self.eng}
        self.lastw = {}
        self.readers = {}
        self.dma_sems = {}
        self.ninstr = 0
        self.pending = {}

    def _new_sem(self, e):
        self.nsem += 1
        self.sem[e] = self.es.enter_context(self.nc.semaphore(f"s_{e}_{self.nsem}"))
        self.cnt[e] = 0

    def _wait(self, e, tok):
        if tok is None:
            return
        sem, val, src = tok
        if src == e and (not self.same or e == "pe"):
            return
        w = self.waited[e]
        k = id(sem)
        if w.get(k, 0) >= val:
            return
        self.eng[e].wait_ge(sem, val)
        w[k] = val

    def _deps(self, e, reads, writes):
        for r in reads:
            self._wait(e, self.lastw.get(r))
        for r in writes:
            self._wait(e, self.lastw.get(r))
            for t in list(self.readers.get(r, {}).values()):
                self._wait(e, t)

    def _commit(self, tok, reads, writes):
        for r in reads:
            self.readers.setdefault(r, {})[(tok[2], id(tok[0]))] = tok
        for r in writes:
            self.lastw[r] = tok
            self.readers[r] = {}

    def op(self, e, fn, reads=(), writes=(), sig=True):
        self._deps(e, reads, writes)
        if self.cnt[e] >= SEM_EPOCH and not self.pending.get(e):
            self._new_sem(e)
        ins = fn()
        if sig:
            self.cnt[e] += 1
            ins.then_inc(self.sem[e], 1)
            tok = (self.sem[e], self.cnt[e], e)
            self.pending[e] = False
        else:
            tok = (self.sem[e], self.cnt[e] + 1, e)
            self.pending[e] = True
        self._commit(tok, reads, writes)
        self.ninstr += 1
        return tok

    def dma(self, q, key, out, in_, reads=(), writes=()):
        self._deps(q, reads, writes)
        st = self.dma_sems.get(key)
        if st is None or st[1] >= SEM_EPOCH:
            self.nsem += 1
            st = [self.es.enter_context(self.nc.semaphore(f"d_{self.nsem}")), 0]
            self.dma_sems[key] = st
        ins = self.eng[q].dma_start(out=out, in_=in_)
        st[1] += 16
        ins.then_inc(st[0], 16)
        tok = (st[0], st[1], "dma")
        self._commit(tok, reads, writes)
        self.ninstr += 1
        return tok

    def finish(self, e, resources):
        for r in resources:
            self._wait(e, self.lastw.get(r))

    def barrier(self):
        toks = [(self.sem[e], self.cnt[e], e) for e in self.eng if self.cnt[e] > 0]
        toks += [(st[0], st[1], "dma") for st in self.dma_sems.values()]
        for e in self.eng:
            for t in toks:
                self._wait(e, t)

import contextlib
import numpy as np

P = 128
EPS = 1e-6


def split_even(n, maxn):
    k = -(-n // maxn)
    base, rem = divmod(n, k)
    out, s = [], 0
    for i in range(k):
        sz = base + (1 if i < rem else 0)
        out.append((s, sz))
        s += sz
    return out


def load_w(S, nc, wt, wd, nk, qname="pool", key="w"):
    v = wd.rearrange("(k p) n -> p k n", p=P)
    n = wd.shape[1]
    step = max(1, 4096 // n)
    for k0 in range(0, nk, step):
        k1 = min(nk, k0 + step)
        S.dma(qname, (key, k0 % 4), wt[:, k0:k1, :], v[:, k0:k1, :], writes=[(key, k0)])
    return [(key, k0) for k0 in range(0, nk, step)]


def rstd_from_psum(S, nc, out, ps, D, eps_col, reads, wres):
    S.op("act", lambda: nc.scalar.activation(out=out, in_=ps, func=AF.Ln, scale=1.0 / D, bias=eps_col), reads=reads, writes=[wres])
    S.op("act", lambda: nc.scalar.activation(out=out, in_=out, func=AF.Exp, scale=-0.5), reads=[wres], writes=[wres])


def emit_dense(nc, S, dm, a, n1max=456, n2max=376):
    D, DFF, BW, NTOK = dm["D"], dm["DFF"], dm["BW"], dm["NTOK"]
    KD, KB, KF = D // P, BW // P, DFF // P
    KBB = KB // 3
    NT = NTOK + 2
    o_gpre, o_gpost, o_gfpre, o_gfpost = 0, KD, 2 * KD, 3 * KD
    o_cw = 4 * KD
    o_cb = o_cw + 3 * 2 * KF
    npar = o_cb + 2 * KF
    xTv = a["xT"].rearrange("(k p) n -> p k n", p=P)
    brTv = a["brT"].rearrange("(k p) n -> p k n", p=P)
    xmv = a["xmid"].rearrange("(k p) n -> p k n", p=P)
    yTv = a["yT"].rearrange("(k p) n -> p k n", p=P)

    with contextlib.ExitStack() as es0:
        E0 = es0.enter_context
        pp = E0(nc.sbuf_tensor("pp_sb", [P, npar], F32))
        ones = E0(nc.sbuf_tensor("ones", [P, P], BF16))
        epsc = E0(nc.sbuf_tensor("epsc", [P, 1], F32))
        S.dma("sp", "pp", pp[:], a["pp"][:, :], writes=["pp"])
        S.op("dve", lambda: nc.vector.memset(ones[:], 1.0), writes=["ones"])
        S.op("dve", lambda: nc.vector.memset(epsc[:], EPS), writes=["epsc"])
        d1_tiles = split_even(NT, n1max)
        N1 = max(n for _, n in d1_tiles)
        with contextlib.ExitStack() as es:
            E = es.enter_context
            wg = E(nc.sbuf_tensor("wg", [P, KD, 3 * D], BF16))
            wbr = E(nc.sbuf_tensor("wbr", [P, KB, D], BF16))
            wo = E(nc.sbuf_tensor("wo", [P, KD, D], BF16))
            r_wg = load_w(S, nc, wg, a["w_gate"], KD, key="wg")
            r_wbr = load_w(S, nc, wbr, a["w_br"], KB, key="wbr")
            r_wo = load_w(S, nc, wo, a["w_out"], KD, key="wo")
            x_sb = E(nc.sbuf_tensor("x_sb", [P, KD, N1], F32))
            br_sb = E(nc.sbuf_tensor("br_sb", [P, KB, N1], BF16))
            xsq = E(nc.sbuf_tensor("xsq", [P, KD, N1], BF16))
            xh = E(nc.sbuf_tensor("xh", [P, KD, N1], BF16))
            rstd = E(nc.sbuf_tensor("rstd", [P, N1], F32))
            rstd2 = E(nc.sbuf_tensor("rstd2", [P, N1], F32))
            gl = [E(nc.sbuf_tensor(f"gl{i}", [P, N1], F32)) for i in range(3)]
            tm = [E(nc.sbuf_tensor(f"tm{i}", [P, N1], F32)) for i in range(3)]
            mf = [E(nc.sbuf_tensor(f"mf{i}", [P, N1], F32)) for i in range(2)]
            mbf = E(nc.sbuf_tensor("mbf", [P, KD, N1], BF16))
            z_sb = E(nc.sbuf_tensor("z_sb", [P, KD, N1], F32))
            ps_st = E(nc.psum_tensor("ps_st", [P, 512], F32))
            ps_g = [E(nc.psum_tensor(f"ps_g{i}", [P, 512], F32)) for i in range(3)]
            ps_p = [E(nc.psum_tensor(f"ps_p{i}", [P, 512], F32)) for i in range(3)]
            ps_z = E(nc.psum_tensor("ps_z", [P, 512], F32))
            it = 0
            x_sb2 = E(nc.sbuf_tensor("x_sb2", [P, KD, N1], F32))
            x_sbs = [x_sb, x_sb2]

            def d1_pro(ti):
                c0, N = d1_tiles[ti]
                par = ti % 2
                xs = x_sbs[par]
                S.dma("sp", ("x", par), xs[:, :, :N], xTv[:, :, c0:c0 + N], writes=[("x_sb", par)])
                S.dma("sp", "br", br_sb[:, :, :N], brTv[:, :, c0:c0 + N], writes=["br_sb"])
                S.op("act", lambda: nc.scalar.activation(out=xsq[:, :, :N], in_=xs[:, :, :N], func=AF.Square), reads=[("x_sb", par)], writes=[("zsq", q) for q in range(KD)])
                for k in range(KD):
                    S.op("act", lambda: nc.scalar.activation(out=xh[:, k, :N], in_=xs[:, k, :N], func=AF.Identity, scale=pp[:, o_gpre + k:o_gpre + k + 1]),
                         reads=[("x_sb", par), "pp"], writes=[("xh", k)])
                for k in range(KD):
                    S.op("pe", lambda: nc.tensor.matmul(ps_st[:, :N], lhsT=ones[:], rhs=xsq[:, k, :N], start=(k == 0), stop=(k == KD - 1)),
                         reads=[("zsq", k), "ones"], writes=["ps_st"], sig=(k == KD - 1))
                rstd_from_psum(S, nc, rstd[:, :N], ps_st[:, :N], D, epsc[:, 0:1], ["ps_st", "epsc"], "rstd")

            d1_pro(0)
            for ti, (c0, N) in enumerate(d1_tiles):
                xs = x_sbs[ti % 2]
                def d1_head(m, b, i3):
                    pg, pq, g_t = ps_g[i3], ps_p[i3], gl[i3]
                    for k in range(KD):
                        S.op("pe", lambda: nc.tensor.matmul(pg[:, :N], lhsT=wg[:, k, b * D + m * P:b * D + (m + 1) * P], rhs=xh[:, k, :N], start=(k == 0), stop=(k == KD - 1)),
                             reads=[("xh", k)] + r_wg, writes=[("pg", i3)], sig=(k == KD - 1))
                    for k in range(KBB):
                        S.op("pe", lambda: nc.tensor.matmul(pq[:, :N], lhsT=wbr[:, b * KBB + k, m * P:(m + 1) * P], rhs=br_sb[:, b * KBB + k, :N], start=(k == 0), stop=(k == KBB - 1)),
                             reads=["br_sb"] + r_wbr, writes=[("pq", i3)], sig=(k == KBB - 1))
                    S.op("dve", lambda: nc.vector.tensor_tensor(out=g_t[:, :N], in0=pg[:, :N], in1=rstd[:, :N], op=ALU.mult),
                         reads=[("pg", i3), "rstd"], writes=[("gl", i3)])
                    S.op("act", lambda: nc.scalar.activation(out=g_t[:, :N], in_=g_t[:, :N], func=AF.Sigmoid), reads=[("gl", i3)], writes=[("gl", i3)])

                def d1_tail(m, b, i3):
                    pq, g_t, t_t = ps_p[i3], gl[i3], tm[i3]
                    mfm = mf[m % 2]
                    if b == 0:
                        S.op("dve", lambda: nc.vector.tensor_tensor(out=mfm[:, :N], in0=g_t[:, :N], in1=pq[:, :N], op=ALU.mult),
                             reads=[("gl", i3), ("pq", i3)], writes=[("mf", m % 2)])
                    else:
                        S.op("dve", lambda: nc.vector.tensor_tensor(out=t_t[:, :N], in0=g_t[:, :N], in1=pq[:, :N], op=ALU.mult),
                             reads=[("gl", i3), ("pq", i3)], writes=[("tm", i3)])
                        dst = mfm[:, :N] if b == 1 else mbf[:, m, :N]
                        wr = [("mf", m % 2)] if b == 1 else [("mbf", m)]
                        S.op("pool", lambda: nc.gpsimd.tensor_tensor(out=dst, in0=mfm[:, :N], in1=t_t[:, :N], op=ALU.add),
                             reads=[("mf", m % 2), ("tm", i3)], writes=wr)

                groups = [(m, b) for m in range(KD) for b in range(3)]
                prev = None
                for (m, b) in groups:
                    i3 = it % 3
                    it += 1
                    d1_head(m, b, i3)
                    if prev is not None:
                        d1_tail(*prev)
                    prev = (m, b, i3)
                d1_tail(*prev)
                if ti + 1 < len(d1_tiles):
                    d1_pro(ti + 1)
                for m in range(KD):
                    pz = ps_g[m % 3]
                    for k in range(KD):
                        S.op("pe", lambda: nc.tensor.matmul(pz[:, :N], lhsT=wo[:, k, m * P:(m + 1) * P], rhs=mbf[:, k, :N], start=(k == 0), stop=(k == KD - 1)),
                             reads=[("mbf", k)] + r_wo, writes=[("pg", m % 3)], sig=(k == KD - 1))
                    S.op("act", lambda: nc.scalar.copy(out=z_sb[:, m, :N], in_=pz[:, :N]), reads=[("pg", m % 3)], writes=[("z", m)])
                    S.op("dve", lambda: nc.vector.tensor_tensor(out=xsq[:, m, :N], in0=z_sb[:, m, :N], in1=z_sb[:, m, :N], op=ALU.mult), reads=[("z", m)], writes=[("zsq", m)])
                for k in range(KD):
                    S.op("pe", lambda: nc.tensor.matmul(ps_st[:, :N], lhsT=ones[:], rhs=xsq[:, k, :N], start=(k == 0), stop=(k == KD - 1)),
                         reads=[("zsq", k), "ones"], writes=["ps_st"], sig=(k == KD - 1))
                rstd_from_psum(S, nc, rstd2[:, :N], ps_st[:, :N], D, epsc[:, 0:1], ["ps_st", "epsc"], "rstd2")
                for m in range(KD):
                    S.op("dve", lambda: nc.vector.scalar_tensor_tensor(out=z_sb[:, m, :N], in0=z_sb[:, m, :N], scalar=pp[:, o_gpost + m:o_gpost + m + 1], in1=rstd2[:, :N], op0=ALU.mult, op1=ALU.mult),
                         reads=[("z", m), "rstd2", "pp"], writes=[("z", m)])
                    if m % 2 == 0:
                        S.op("pool", lambda: nc.gpsimd.tensor_tensor(out=z_sb[:, m, :N], in0=z_sb[:, m, :N], in1=xs[:, m, :N], op=ALU.add),
                             reads=[("z", m), ("x_sb", ti % 2)], writes=[("z", m)])
                    else:
                        S.op("dve", lambda: nc.vector.tensor_tensor(out=z_sb[:, m, :N], in0=z_sb[:, m, :N], in1=xs[:, m, :N], op=ALU.add),
                             reads=[("z", m), ("x_sb", ti % 2)], writes=[("z", m)])
                S.dma("sp", "xm_st", xmv[:, :, c0:c0 + N], z_sb[:, :, :N], reads=[("z", m) for m in range(KD)], writes=[("xmid", ti)])
        S.barrier()
        d2_tiles = split_even(NTOK, n2max)
        N2 = max(n for _, n in d2_tiles)
        with contextlib.ExitStack() as es:
            E = es.enter_context
            wup = E(nc.sbuf_tensor("wup", [P, KD, 2 * DFF], BF16))
            wdn = E(nc.sbuf_tensor("wdn", [P, KF, D], BF16))
            r_wup = load_w(S, nc, wup, a["w_up"], KD, key="wup")
            r_wdn = load_w(S, nc, wdn, a["w_down"], KF, key="wdn")
            xm = E(nc.sbuf_tensor("xm", [P, KD, N2 + 2], F32))
            xsq = E(nc.sbuf_tensor("xsq2", [P, KD, N2 + 2], BF16))
            hn = E(nc.sbuf_tensor("hn", [P, KD, N2 + 2], BF16))
            rstd = E(nc.sbuf_tensor("rstd_b", [P, N2 + 2], F32))
            rstd3 = E(nc.sbuf_tensor("rstd3", [P, N2 + 2], F32))
            a_sb = E(nc.sbuf_tensor("a_sb", [P, KF, N2], BF16))
            cvg = [E(nc.sbuf_tensor(f"cvg{i}", [P, N2], F32)) for i in range(3)]
            cvu = [E(nc.sbuf_tensor(f"cvu{i}", [P, N2], F32)) for i in range(3)]
            y_sb = E(nc.sbuf_tensor("y_sb", [P, KD, N2], F32))
            ps_st = E(nc.psum_tensor("ps_st2", [P, 512], F32))
            ps_ug = [E(nc.psum_tensor(f"ps_ug{i}", [P, 512], F32)) for i in range(3)]
            ps_uu = [E(nc.psum_tensor(f"ps_uu{i}", [P, 512], F32)) for i in range(3)]
            ps_y = [E(nc.psum_tensor(f"ps_y{i}", [P, 512], F32)) for i in range(1)]
            all_xmid = [("xmid", ti) for ti in range(len(d1_tiles))]
            xm2 = E(nc.sbuf_tensor("xm2", [P, KD, N2 + 2], F32))
            xms = [xm, xm2]

            def d2_pro(ti):
                o0, N = d2_tiles[ti]
                M = N + 2
                par = ti % 2
                xmc = xms[par]
                S.dma("sp", ("xm", par), xmc[:, :, :M], xmv[:, :, o0:o0 + M], reads=all_xmid, writes=[("xm", par)])
                S.op("act", lambda: nc.scalar.activation(out=xsq[:, :, :M], in_=xmc[:, :, :M], func=AF.Square), reads=[("xm", par)], writes=[("ysq", q) for q in range(KD)])
                for k in range(KD):
                    S.op("pe", lambda: nc.tensor.matmul(ps_st[:, :M], lhsT=ones[:], rhs=xsq[:, k, :M], start=(k == 0), stop=(k == KD - 1)),
                         reads=[("ysq", k), "ones"], writes=["ps_st2"], sig=(k == KD - 1))
                rstd_from_psum(S, nc, rstd[:, :M], ps_st[:, :M], D, epsc[:, 0:1], ["ps_st2", "epsc"], "rstd_b")
                for k in range(KD):
                    S.op("dve", lambda: nc.vector.scalar_tensor_tensor(out=hn[:, k, :M], in0=xmc[:, k, :M], scalar=pp[:, o_gfpre + k:o_gfpre + k + 1], in1=rstd[:, :M], op0=ALU.mult, op1=ALU.mult),
                         reads=[("xm", par), "rstd_b", "pp"], writes=[("hn", k)])

            d2_pro(0)
            for ti, (o0, N) in enumerate(d2_tiles):
                M = N + 2
                xmc = xms[ti % 2]
                def d2_head(c):
                    i2 = c % 3
                    for half, pst, cv, col, nm in ((0, ps_ug[i2], cvg[i2], c, "ug"), (1, ps_uu[i2], cvu[i2], KF + c, "uu")):
                        for k in range(KD):
                            S.op("pe", lambda: nc.tensor.matmul(pst[:, :M], lhsT=wup[:, k, col * P:(col + 1) * P], rhs=hn[:, k, :M], start=(k == 0), stop=(k == KD - 1)),
                                 reads=[("hn", k)] + r_wup, writes=[(nm, i2)], sig=(k == KD - 1))
                        cw = lambda j: pp[:, o_cw + j * 2 * KF + col:o_cw + j * 2 * KF + col + 1]
                        S.op("act", lambda: nc.scalar.activation(out=cv[:, :N], in_=pst[:, 2:N + 2], func=AF.Identity, scale=cw(2), bias=pp[:, o_cb + col:o_cb + col + 1]),
                             reads=[(nm, i2), "pp"], writes=[("cv" + nm, i2)])
                        S.op("dve", lambda: nc.vector.scalar_tensor_tensor(out=cv[:, :N], in0=pst[:, 1:N + 1], scalar=cw(1), in1=cv[:, :N], op0=ALU.mult, op1=ALU.add),
                             reads=[(nm, i2), "pp", ("cv" + nm, i2)], writes=[("cv" + nm, i2)])
                        S.op("dve", lambda: nc.vector.scalar_tensor_tensor(out=cv[:, :N], in0=pst[:, 0:N], scalar=cw(0), in1=cv[:, :N], op0=ALU.mult, op1=ALU.add),
                             reads=[(nm, i2), "pp", ("cv" + nm, i2)], writes=[("cv" + nm, i2)])

                def d2_tail(c):
                    i2 = c % 3
                    S.op("pool", lambda: nc.gpsimd.tensor_tensor(out=cvu[i2][:, :N], in0=cvu[i2][:, :N], in1=cvg[i2][:, :N], op=ALU.mult),
                         reads=[("cvug", i2), ("cvuu", i2)], writes=[("cvuu", i2)])
                    S.op("act", lambda: nc.scalar.activation(out=cvg[i2][:, :N], in_=cvg[i2][:, :N], func=AF.Sigmoid), reads=[("cvug", i2)], writes=[("cvug", i2)])
                    S.op("pool", lambda: nc.gpsimd.tensor_tensor(out=a_sb[:, c, :N], in0=cvu[i2][:, :N], in1=cvg[i2][:, :N], op=ALU.mult),
                         reads=[("cvug", i2), ("cvuu", i2)], writes=[("a", c)])

                for c in range(KF + 1):
                    if c < KF:
                        d2_head(c)
                    if c >= 1:
                        d2_tail(c - 1)
                if ti + 1 < len(d2_tiles):
                    d2_pro(ti + 1)
                for m in range(KD):
                    py = ps_ug[m % 3]
                    for c in range(KF):
                        S.op("pe", lambda: nc.tensor.matmul(py[:, :N], lhsT=wdn[:, c, m * P:(m + 1) * P], rhs=a_sb[:, c, :N], start=(c == 0), stop=(c == KF - 1)),
                             reads=[("a", c)] + r_wdn, writes=[("ug", m % 3)], sig=(c == KF - 1))
                    S.op("act", lambda: nc.scalar.copy(out=y_sb[:, m, :N], in_=py[:, :N]), reads=[("ug", m % 3)], writes=[("y", m)])
                    S.op("dve", lambda: nc.vector.tensor_tensor(out=xsq[:, m, :N], in0=y_sb[:, m, :N], in1=y_sb[:, m, :N], op=ALU.mult), reads=[("y", m)], writes=[("ysq", m)])
                for k in range(KD):
                    S.op("pe", lambda: nc.tensor.matmul(ps_st[:, :N], lhsT=ones[:], rhs=xsq[:, k, :N], start=(k == 0), stop=(k == KD - 1)),
                         reads=[("ysq", k), "ones"], writes=["ps_st2"], sig=(k == KD - 1))
                rstd_from_psum(S, nc, rstd3[:, :N], ps_st[:, :N], D, epsc[:, 0:1], ["ps_st2", "epsc"], "rstd3")
                for m in range(KD):
                    S.op("dve", lambda: nc.vector.scalar_tensor_tensor(out=y_sb[:, m, :N], in0=y_sb[:, m, :N], scalar=pp[:, o_gfpost + m:o_gfpost + m + 1], in1=rstd3[:, :N], op0=ALU.mult, op1=ALU.mult),
                         reads=[("y", m), "rstd3", "pp"], writes=[("y", m)])
                    if m % 2 == 0:
                        S.op("pool", lambda: nc.gpsimd.tensor_tensor(out=y_sb[:, m, :N], in0=y_sb[:, m, :N], in1=xmc[:, m, 2:N + 2], op=ALU.add),
                             reads=[("y", m), ("xm", ti % 2)], writes=[("y", m)])
                    else:
                        S.op("dve", lambda: nc.vector.tensor_tensor(out=y_sb[:, m, :N], in0=y_sb[:, m, :N], in1=xmc[:, m, 2:N + 2], op=ALU.add),
                             reads=[("y", m), ("xm", ti % 2)], writes=[("y", m)])
                S.dma("sp", "y_st", yTv[:, :, o0:o0 + N], y_sb[:, :, :N], reads=[("y", m) for m in range(KD)], writes=[("yT", ti)])
            S.finish("sp", [("yT", ti) for ti in range(len(d2_tiles))])


def pack_params_dense(g_pre, g_post, g_fpre, g_fpost, conv_w, conv_b):
    def col(v):
        return np.ascontiguousarray(v.reshape(-1, P).T)
    cw = np.concatenate([col(conv_w[j]) for j in range(3)], axis=1)
    return np.ascontiguousarray(np.concatenate([col(g_pre), col(g_post), col(g_fpre), col(g_fpost), cw, col(conv_b)], axis=1).astype(np.float32))


def build_dense(dm):
    nc = bass.Bass("TRN2", target_bir_lowering=False)
    D, DFF, BW, NTOK = dm["D"], dm["DFF"], dm["BW"], dm["NTOK"]
    KD, KF = D // P, DFF // P
    npar = 4 * KD + 8 * KF
    a = {
        "xT": nc.dram_tensor("xT", [D, NTOK + 2], F32, kind="ExternalInput").ap(),
        "brT": nc.dram_tensor("brT", [BW, NTOK + 2], BF16, kind="ExternalInput").ap(),
        "w_gate": nc.dram_tensor("w_gate", [D, 3 * D], F32, kind="ExternalInput").ap(),
        "w_br": nc.dram_tensor("w_br", [BW, D], F32, kind="ExternalInput").ap(),
        "w_out": nc.dram_tensor("w_out", [D, D], F32, kind="ExternalInput").ap(),
        "w_up": nc.dram_tensor("w_up", [D, 2 * DFF], F32, kind="ExternalInput").ap(),
        "w_down": nc.dram_tensor("w_down", [DFF, D], F32, kind="ExternalInput").ap(),
        "pp": nc.dram_tensor("pp", [P, npar], F32, kind="ExternalInput").ap(),
        "xmid": nc.dram_tensor("xmid", [D, NTOK + 2], F32).ap(),
        "yT": nc.dram_tensor("yT", [D, NTOK], F32, kind="ExternalOutput").ap(),
    }
    with contextlib.ExitStack() as es:
        S = Sched(nc, es)
        emit_dense(nc, S, dm, a)
        print("dense instrs", S.ninstr, "sems", S.nsem)
    return nc

import contextlib
import itertools
import numpy as np

P = 128
EPS = 1e-6
BIG = 30000.0
NFM = 912
NTM = 320
FM_GROUPS = [("gq", 0, 64), ("gk", 64, 64), ("gr", 128, 128), ("lr", 256, 16), ("mq", 272, 128), ("mk", 400, 128),
             ("sb", 528, 128), ("sc", 656, 128), ("sx", 784, 128)]
C_U, C_SL, C_TRI, C_ID, C_FILL, C_A0, C_PP = 0, 128, 256, 384, 512, 640, 768
NCONST = 768


def emit_mixer(nc, S, dm, a):
    D, T = dm["D"], dm["T"]
    KD = D // P
    NTILE = T // 512
    NKT = T // P
    o_gpre = C_PP
    o_wlr = o_gpre + KD
    o_gn = o_wlr + 64
    o_scw = o_gn + 1
    o_al = o_scw + 3
    ncc = o_al + 4
    xTv = a["xT"].rearrange("(k p) n -> p k n", p=P)
    brv = a["brT"].rearrange("(k p) n -> p k n", p=P)

    with contextlib.ExitStack() as es:
        E = es.enter_context
        cc = E(nc.sbuf_tensor("cc_sb", [P, ncc], F32))
        S.dma("sp", "cc", cc[:], a["cc"][:, :], writes=["cc"])
        shiftc = E(nc.sbuf_tensor("shiftc_sb", [P, 2 * NKT], F32))
        S.dma("sp", "shiftc", shiftc[:], a["shiftc"][:, :], writes=["shiftc"])
        causneg = E(nc.sbuf_tensor("causneg_sb", [P, 4, 512], BF16))
        S.dma("sp", "causneg", causneg[:], a["causneg"][:, :, :], writes=["causneg"])
        ident_bf = E(nc.sbuf_tensor("ident_bf", [P, P], BF16))
        ones_bf = E(nc.sbuf_tensor("ones_bf", [P, P], BF16))
        ones_f = E(nc.sbuf_tensor("ones_f", [P, P], F32))
        epsc = E(nc.sbuf_tensor("epsc_a", [P, 1], F32))
        S.op("dve", lambda: nc.vector.memset(ones_bf[:], 1.0), writes=["ones_bf"])
        S.op("dve", lambda: nc.vector.memset(ones_f[:], 1.0), writes=["ones_f"])
        S.op("dve", lambda: nc.vector.memset(epsc[:], EPS), writes=["epsc"])
        S.op("dve", lambda: nc.vector.tensor_copy(out=ident_bf[:], in_=cc[:, C_ID:C_ID + P]), reads=["cc"], writes=["ident_bf"])
        U = cc[:, C_U:C_U + P]
        SL = cc[:, C_SL:C_SL + P]
        TRI = cc[:, C_TRI:C_TRI + P]
        IDF = cc[:, C_ID:C_ID + P]
        wf = E(nc.sbuf_tensor("wf", [P, KD, NFM], BF16))
        wt = E(nc.sbuf_tensor("wt", [P, KD, NTM], BF16))
        wfv = a["w_f"].rearrange("(k p) n -> p k n", p=P)
        wtv = a["w_t"].rearrange("(k p) n -> p k n", p=P)
        for k0 in range(0, KD, 4):
            S.dma("pool", ("wf", k0), wf[:, k0:min(KD, k0 + 4), :], wfv[:, k0:min(KD, k0 + 4), :], writes=["wf"])
        S.dma("pool", "wt", wt[:], wtv[:, :, :], writes=["wt"])
        kaug = [E(nc.sbuf_tensor(f"kaug{h}", [P, T], BF16)) for h in range(2)]
        S.dma("sp", "kaug0", kaug[0][64:128, :], a["eblk"][:, :], writes=["kaug0e"])
        S.dma("sp", "kaug1", kaug[1][0:64, :], a["eblk"][:, :], writes=["kaug1e"])
        vaug0 = E(nc.sbuf_tensor("vaug0", [P, NKT, 65], BF16))
        vaug1 = E(nc.sbuf_tensor("vaug1", [P, NKT, 128], BF16))
        S.op("pool", lambda: nc.gpsimd.memset(vaug0[:, :, 64:65], 1.0), writes=["vaug0c"])
        S.op("pool", lambda: nc.gpsimd.memset(vaug1[:, :, 0:64], 0.0), writes=["vaug1c"])
        S.op("pool", lambda: nc.gpsimd.memset(vaug1[:, :, 0:1], 1.0), reads=["vaug1c"], writes=["vaug1c"])
        kmean = E(nc.sbuf_tensor("kmean", [P, 64], F32))
        S.op("dve", lambda: nc.vector.memset(kmean[:], 0.0), writes=["kmean"])
        x_st = [E(nc.sbuf_tensor(f"x_st{i}", [P, 512], F32)) for i in range(2)]
        xh = E(nc.sbuf_tensor("xh_a", [P, KD, 512], BF16))
        xsq = E(nc.sbuf_tensor("xsq_a", [P, KD, 512], BF16))
        rstd = E(nc.sbuf_tensor("rstd_a", [P, 512], F32))
        rstd_tok = E(nc.sbuf_tensor("rstd_tok", [P, 4], F32))
        fo = {nm: E(nc.sbuf_tensor("fo_" + nm, [P, 512], F32)) for nm in ("gq", "gk", "gr", "lr", "mq", "mk", "sb", "sc", "sx")}
        S.op("dve", lambda: nc.vector.memset(fo["lr"][0:32, :], 1.0), writes=["fo_lr1"])
        zc = E(nc.sbuf_tensor("zc", [P, 514], F32))
        S.op("dve", lambda: nc.vector.memset(zc[:, 0:2], 0.0), writes=["zc_h"])
        cva = E(nc.sbuf_tensor("cva", [P, 512], F32))
        gk_tok = E(nc.sbuf_tensor("gk_tok", [P, 4, 64], F32))
        gv = E(nc.sbuf_tensor("gv", [P, 4, 128], BF16))
        qaug = [E(nc.sbuf_tensor(f"qaug{h}", [P, 512], BF16)) for h in range(2)]
        mstage = E(nc.sbuf_tensor("mstage", [P, 4, P], F32))
        S.op("dve", lambda: nc.vector.memset(mstage[:], 0.0), writes=["mstage"])
        gm = E(nc.sbuf_tensor("gm", [P, 64], F32))
        top8 = E(nc.sbuf_tensor("top8", [P, 8], F32))
        thr = E(nc.sbuf_tensor("thr", [P, 1], F32))
        tsel = E(nc.sbuf_tensor("tsel", [P, 64], F32))
        pT = [E(nc.sbuf_tensor(f"pT{i}", [P, 512], BF16)) for i in range(3)]
        rs = E(nc.sbuf_tensor("rs", [P, 512], F32))
        br_sb = E(nc.sbuf_tensor("br_out", [P, 3, 512], BF16))
        la = E(nc.sbuf_tensor("la", [P, 4, 64], F32))
        er = E(nc.sbuf_tensor("er", [P, 4, 64], F32))
        qt_bf = E(nc.sbuf_tensor("qt_bf", [64, 512], BF16))
        kt_bf = E(nc.sbuf_tensor("kt_bf", [64, 512], BF16))
        kh_bf = E(nc.sbuf_tensor("kh_bf", [P, 4, 64], BF16))
        att_bf = E(nc.sbuf_tensor("att_bf", [P, P], BF16))
        Sst = E(nc.sbuf_tensor("Sst", [64, P], F32))
        Sbf = E(nc.sbuf_tensor("Sbf", [64, P], BF16))
        S.op("dve", lambda: nc.vector.memset(Sst[:], 0.0), writes=["Sst"])
        S.op("dve", lambda: nc.vector.memset(Sbf[:], 0.0), writes=["Sbf"])
        osq = E(nc.sbuf_tensor("osq", [P, 512], BF16))
        rstd_o = rs
        ot = cva
        o_sb = cva
        B = [E(nc.psum_tensor(f"bank{i}", [P, 512], F32)) for i in range(8)]

        def prologue(I):
            t0 = I * 512
            for k in range(KD):
                xs = x_st[k % 2]
                S.dma("sp", ("x", k % 2), xs[:], xTv[:, k, t0:t0 + 512], writes=[("x_st", k % 2)])
                S.op("act", lambda: nc.scalar.activation(out=xsq[:, k, :], in_=xs[:], func=AF.Square), reads=[("x_st", k % 2)], writes=[("xsq", k)])
                S.op("dve", lambda: nc.vector.tensor_scalar(out=xh[:, k, :], in0=xs[:], scalar1=cc[:, o_gpre + k:o_gpre + k + 1], scalar2=None, op0=ALU.mult),
                     reads=[("x_st", k % 2), "cc"], writes=[("xh", k)])

        def prologue_b():
            for k in range(KD):
                S.op("pe", lambda: nc.tensor.matmul(B[3][:, :], lhsT=ones_bf[:], rhs=xsq[:, k, :], start=(k == 0), stop=(k == KD - 1)),
                     reads=[("xsq", k), "ones_bf"], writes=["B3"], sig=(k == KD - 1))
            S.op("act", lambda: nc.scalar.activation(out=rstd[:], in_=B[3][:, :], func=AF.Ln, scale=1.0 / D, bias=epsc[:, 0:1]), reads=["B3", "epsc"], writes=["rstd"])
            S.op("act", lambda: nc.scalar.activation(out=rstd[:], in_=rstd[:], func=AF.Exp, scale=-0.5), reads=["rstd"], writes=["rstd"])
            for c in range(4):
                S.op("pe", lambda: nc.tensor.matmul(B[3][:, c:c + 1], lhsT=rstd[0:1, c * P:(c + 1) * P], rhs=ones_f[0:1, 0:1], start=True, stop=True),
                     reads=["rstd", "ones_f"], writes=["B3"])
            S.op("dve", lambda: nc.vector.tensor_copy(out=rstd_tok[:], in_=B[3][:, 0:4]), reads=["B3"], writes=["rstd_tok"])

        def fm_gen(I_, b0):
            banks = (0, 1) if b0 == 1 else (4, 5)
            for gi, (nm, c0, ncol) in enumerate(FM_GROUPS):
                bi_ = banks[gi % 2]
                pb = B[bi_]
                pn = "B%d" % bi_
                for k in range(KD):
                    S.op("pe", lambda: nc.tensor.matmul(pb[0:ncol, :], lhsT=wf[:, k, c0:c0 + ncol], rhs=xh[:, k, :], start=(k == 0), stop=(k == KD - 1)),
                         reads=[("xh", k), "wf"], writes=[pn], sig=(k == KD - 1))
                    yield
                dst = fo[nm]
                if nm in ("mq", "gq"):
                    S.op("dve", lambda: nc.vector.scalar_tensor_tensor(out=dst[0:ncol, :], in0=pb[0:ncol, :], scalar=0.125, in1=rstd[0:ncol, :], op0=ALU.mult, op1=ALU.mult),
                         reads=[pn, "rstd"], writes=["fo_" + nm])
                else:
                    extra = ["fo_lr1"] if nm == "lr" else []
                    S.op("dve", lambda: nc.vector.tensor_tensor(out=dst[0:ncol, :], in0=pb[0:ncol, :], in1=rstd[0:ncol, :], op=ALU.mult),
                         reads=[pn, "rstd"] + extra, writes=["fo_" + nm])
                yield

        def tm_gen(I_):
            for c in range(4):
                bi_ = 4 + (c % 2)
                pb = B[bi_]
                pn = "B%d" % bi_
                for k in range(KD):
                    S.op("pe", lambda: nc.tensor.matmul(pb[:, 0:NTM], lhsT=xh[:, k, c * P:(c + 1) * P], rhs=wt[:, k, :], start=(k == 0), stop=(k == KD - 1)),
                         reads=[("xh", k), "wt"], writes=[pn], sig=(k == KD - 1))
                    yield
                sc_ = rstd_tok[:, c:c + 1]
                S.op("act", lambda: nc.scalar.activation(out=gk_tok[:, c, :], in_=pb[:, 0:64], func=AF.Identity, scale=sc_), reads=[pn, "rstd_tok"], writes=[("gk_tok", c)])
                S.op("act", lambda: nc.scalar.activation(out=gv[:, c, :], in_=pb[:, 64:192], func=AF.Identity, scale=sc_), reads=[pn, "rstd_tok"], writes=[("gv", c)])
                yield
                S.op("act", lambda: nc.scalar.activation(out=vaug0[:, 4 * I_ + c, 0:64], in_=pb[:, 192:256], func=AF.Identity, scale=sc_), reads=[pn, "rstd_tok"], writes=[("vaug", 0, I_)])
                S.op("act", lambda: nc.scalar.activation(out=vaug1[:, 4 * I_ + c, 64:128], in_=pb[:, 256:320], func=AF.Identity, scale=sc_), reads=[pn, "rstd_tok"], writes=[("vaug", 1, I_)])
                yield

        prologue(0)
        prologue_b()
        for I in range(NTILE):
            t0 = I * 512
            nkt = 4 * I + 4

            def gating_gen(h):
                hs = slice(64 * h, 64 * h + 64)
                ms = slice(64 * (1 - h), 64 * (1 - h) + 64)
                for qs in range(4):
                    qt = 4 * I + qs
                    bi = qt // 2
                    S.op("pe", lambda: nc.tensor.matmul(B[3][:, 0:64], lhsT=fo["mq"][hs, qs * P:(qs + 1) * P], rhs=kmean[hs, :], start=True, stop=True),
                         reads=["fo_mq", "kmean"], writes=["B3"])
                    S.op("dve", lambda: nc.vector.tensor_tensor(out=gm[:], in0=B[3][:, 0:64], in1=cc[:, C_FILL + 63 - bi:C_FILL + 127 - bi], op=ALU.add), reads=["B3", "cc"], writes=["gm"])
                    yield
                    S.op("dve", lambda: nc.vector.max(out=top8[:], in_=gm[:]), reads=["gm"], writes=["top8"])
                    yield
                    S.op("dve", lambda: nc.vector.tensor_scalar(out=thr[:], in0=top8[:, 3:4], scalar1=-1e29, scalar2=None, op0=ALU.max), reads=["top8"], writes=["thr"])
                    yield
                    S.op("dve", lambda: nc.vector.tensor_scalar(out=tsel[:], in0=gm[:], scalar1=thr[:, 0:1], scalar2=BIG, op0=ALU.is_ge, op1=ALU.mult), reads=["gm", "thr"], writes=["tsel"])
                    yield
                    S.op("dve", lambda: nc.vector.scalar_tensor_tensor(out=mstage[:, qs, ms], in0=tsel[:], scalar=shiftc[:, h * NKT + qt:h * NKT + qt + 1], in1=cc[:, C_A0 + 64 * h:C_A0 + 64 * h + 64], op0=ALU.add, op1=ALU.add),
                         reads=["tsel", "shiftc", "cc"], writes=[("mstage", qs)])
                    yield
                for qs in range(4):
                    S.op("pe", lambda: nc.tensor.transpose(B[5][:, qs * P:(qs + 1) * P], mstage[:, qs, :], IDF), reads=[("mstage", qs), "cc"], writes=["B5"])
                yield
                S.op("act", lambda: nc.scalar.copy(out=qaug[h][ms, :], in_=B[5][ms, :]), reads=["B5"], writes=[f"qaug{h}m"])
                yield

            if I == 0:
                for _ in fm_gen(0, 1):
                    pass
            S.op("act", lambda: nc.scalar.copy(out=qaug[0][0:64, :], in_=fo["mq"][0:64, :]), reads=["fo_mq"], writes=["qaug0q"])
            S.op("act", lambda: nc.scalar.copy(out=qaug[1][64:128, :], in_=fo["mq"][64:128, :]), reads=["fo_mq"], writes=["qaug1q"])
            S.op("act", lambda: nc.scalar.copy(out=kaug[0][0:64, t0:t0 + 512], in_=fo["mk"][0:64, :]), reads=["fo_mk"], writes=["kaug0k"])
            S.op("act", lambda: nc.scalar.copy(out=kaug[1][64:128, t0:t0 + 512], in_=fo["mk"][64:128, :]), reads=["fo_mk"], writes=["kaug1k"])
            S.op("dve", lambda: nc.vector.tensor_reduce(out=kmean[:, 2 * I:2 * I + 2], in_=fo["mk"][:, :].rearrange("p (b n) -> p b n", n=256), axis=AX.X, op=ALU.add),
                 reads=["fo_mk"], writes=["kmean"])
            if I == 0:
                for _ in tm_gen(0):
                    pass
            S.op("pool", lambda: nc.gpsimd.tensor_tensor(out=zc[:, 2:514], in0=fo["sc"][:, :], in1=fo["sx"][:, :], op=ALU.mult), reads=["fo_sc", "fo_sx"], writes=["zc"])
            scw = lambda j: cc[:, o_scw + j:o_scw + j + 1]
            S.op("dve", lambda: nc.vector.tensor_scalar(out=cva[:], in0=zc[:, 2:514], scalar1=scw(2), scalar2=None, op0=ALU.mult), reads=["zc", "cc"], writes=["cva"])
            S.op("dve", lambda: nc.vector.scalar_tensor_tensor(out=cva[:], in0=zc[:, 1:513], scalar=scw(1), in1=cva[:], op0=ALU.mult, op1=ALU.add), reads=["zc", "zc_h", "cc", "cva"], writes=["cva"])
            S.op("dve", lambda: nc.vector.scalar_tensor_tensor(out=cva[:], in0=zc[:, 0:512], scalar=scw(0), in1=cva[:], op0=ALU.mult, op1=ALU.add), reads=["zc", "zc_h", "cc", "cva"], writes=["cva"])
            S.op("pool", lambda: nc.gpsimd.tensor_tensor(out=br_sb[:, 2, :], in0=cva[:], in1=fo["sb"][:, :], op=ALU.mult), reads=["cva", "fo_sb"], writes=["br_c"])
            S.op("pool", lambda: nc.gpsimd.tensor_copy(out=zc[:, 0:2], in_=zc[:, 512:514]), reads=["zc", "zc_h"], writes=["zc_h"])
            S.op("act", lambda: nc.scalar.activation(out=fo["gr"][:, :], in_=fo["gr"][:, :], func=AF.Silu), reads=["fo_gr"], writes=["fo_gr"])
            eb = rs[0:64, :]
            enb = cva[0:64, :]
            for c in range(4):
                S.op("pe", lambda: nc.tensor.matmul(B[0][:, c * 64:(c + 1) * 64], lhsT=fo["lr"][0:32, c * P:(c + 1) * P], rhs=cc[0:32, o_wlr:o_wlr + 64], start=True, stop=True),
                     reads=["fo_lr", "fo_lr1", "cc"], writes=["B0"], sig=(c == 3))
            S.op("act", lambda: nc.scalar.activation(out=la[:].rearrange("p c d -> p (c d)"), in_=B[0][:, 0:256], func=AF.Exp, scale=-1.0), reads=["B0"], writes=["la"])
            S.op("act", lambda: nc.scalar.activation(out=la[:].rearrange("p c d -> p (c d)"), in_=la[:].rearrange("p c d -> p (c d)"), func=AF.Ln, bias=1.0), reads=["la"], writes=["la"])
            for c in range(4):
                S.op("pe", lambda: nc.tensor.matmul(B[1][0:64, c * P:(c + 1) * P], lhsT=la[:, c, :], rhs=U, start=True, stop=True), reads=["la", "cc"], writes=["B1"], sig=(c == 3))
            for c in range(4):
                S.op("pe", lambda: nc.tensor.matmul(B[2][:, c * 64:(c + 1) * 64], lhsT=SL, rhs=la[:, c, :], start=True, stop=True), reads=["la", "cc"], writes=["B2"], sig=(c == 3))
            S.op("act", lambda: nc.scalar.activation(out=eb, in_=B[1][0:64, :], func=AF.Exp), reads=["B1", "rs"], writes=["rs"])
            S.op("act", lambda: nc.scalar.activation(out=enb, in_=B[1][0:64, :], func=AF.Exp, scale=-1.0), reads=["B1", "cva"], writes=["cva"])
            S.op("act", lambda: nc.scalar.activation(out=er[:].rearrange("p c d -> p (c d)"), in_=B[2][:, 0:256], func=AF.Exp), reads=["B2"], writes=["er"])
            S.op("dve", lambda: nc.vector.tensor_tensor(out=qt_bf[:], in0=fo["gq"][0:64, :], in1=eb, op=ALU.mult), reads=["fo_gq", "rs"], writes=["qt_bf"])
            S.op("dve", lambda: nc.vector.tensor_tensor(out=kt_bf[:], in0=fo["gk"][0:64, :], in1=enb, op=ALU.mult), reads=["fo_gk", "cva"], writes=["kt_bf"])
            S.op("pool", lambda: nc.gpsimd.tensor_tensor(out=kh_bf[:].rearrange("p c d -> p (c d)"), in0=gk_tok[:].rearrange("p c d -> p (c d)"), in1=er[:].rearrange("p c d -> p (c d)"), op=ALU.mult), reads=[("gk_tok", c) for c in range(4)] + ["er"], writes=["kh_bf"])
            gg0 = gating_gen(0)
            for c in range(4):
                cs = slice(c * P, (c + 1) * P)
                S.op("pe", lambda: nc.tensor.matmul(B[4][:, 0:128], lhsT=kt_bf[:, cs], rhs=qt_bf[:, cs], start=True, stop=True), reads=["kt_bf", "qt_bf"], writes=["B4"])
                S.op("dve", lambda: nc.vector.tensor_tensor(out=att_bf[:], in0=B[4][:, 0:128], in1=TRI, op=ALU.mult), reads=["B4", "cc"], writes=["att_bf"])
                S.op("pe", lambda: nc.tensor.matmul(B[4][0:64, 128:256], lhsT=kh_bf[:, c, :], rhs=gv[:, c, :], start=True, stop=True), reads=["kh_bf", ("gv", c)], writes=["B4"])
                S.op("pe", lambda: nc.tensor.matmul(B[5][:, cs], lhsT=gv[:, c, :], rhs=att_bf[:], start=True, stop=False), reads=[("gv", c), "att_bf"], writes=["B5"], sig=False)
                S.op("pe", lambda: nc.tensor.matmul(B[5][:, cs], lhsT=Sbf[:], rhs=qt_bf[:, cs], start=False, stop=True), reads=["Sbf", "qt_bf"], writes=["B5"])
                S.op("dve", lambda: nc.vector.scalar_tensor_tensor(out=Sst[:], in0=Sst[:], scalar=eb[:, c * P + 127:c * P + 128], in1=B[4][0:64, 128:256], op0=ALU.mult, op1=ALU.add),
                     reads=["Sst", "rs", "B4"], writes=["Sst"])
                S.op("pool", lambda: nc.gpsimd.tensor_copy(out=Sbf[:], in_=Sst[:]), reads=["Sst"], writes=["Sbf"])
                for _ in range(5):
                    next(gg0, None)
            S.op("act", lambda: nc.scalar.activation(out=osq[:], in_=B[5][:, :], func=AF.Square), reads=["B5"], writes=["osq"])
            S.op("pe", lambda: nc.tensor.matmul(B[3][:, :], lhsT=ones_bf[:], rhs=osq[:], start=True, stop=True), reads=["osq", "ones_bf"], writes=["B3"])
            S.op("act", lambda: nc.scalar.activation(out=rstd_o[:], in_=B[3][:, :], func=AF.Ln, scale=1.0 / P, bias=epsc[:, 0:1]), reads=["B3", "epsc"], writes=["rs"])
            S.op("act", lambda: nc.scalar.activation(out=rstd_o[:], in_=rstd_o[:], func=AF.Exp, scale=-0.5), reads=["rs"], writes=["rs"])
            S.op("dve", lambda: nc.vector.tensor_tensor(out=ot[:], in0=B[5][:, :], in1=rstd_o[:], op=ALU.mult), reads=["B5", "rs"], writes=["cva"])
            S.op("dve", lambda: nc.vector.scalar_tensor_tensor(out=br_sb[:, 0, :], in0=ot[:], scalar=cc[:, o_gn:o_gn + 1], in1=fo["gr"][:, :], op0=ALU.mult, op1=ALU.mult),
                 reads=["cva", "cc", "fo_gr"], writes=["br_a"])
            def attn_loop(h, filler):
                po = B[6 + h]
                nfill = 1 if h == 0 else max(1, -(-128 // nkt))

                def emit_s(kt):
                    ib = kt % 3
                    ps = B[ib]
                    pn = "B%d" % ib
                    diag = kt >= 4 * I
                    S.op("pe", lambda: nc.tensor.matmul(ps[:, :], lhsT=kaug[h][:, kt * P:(kt + 1) * P], rhs=qaug[h][:, :], start=True, stop=not diag),
                         reads=[f"kaug{h}k", f"kaug{h}e", f"qaug{h}q", f"qaug{h}m"], writes=[pn], sig=not diag)
                    if diag:
                        S.op("pe", lambda: nc.tensor.matmul(ps[:, :], lhsT=ident_bf[:], rhs=causneg[:, kt - 4 * I, :], start=False, stop=True),
                             reads=["ident_bf", "causneg"], writes=[pn])

                def emit_pv(kt):
                    ib = kt % 3
                    ps = B[ib]
                    pn = "B%d" % ib
                    S.op("act", lambda: nc.scalar.activation(out=pT[ib][:], in_=ps[:, :], func=AF.Exp, bias=cc[:, o_al + 2 * h + (kt % 2):o_al + 2 * h + (kt % 2) + 1]),
                         reads=[pn, "cc"], writes=[("pT", ib)])
                    if h == 0:
                        S.op("pe", lambda: nc.tensor.matmul(po[0:65, :], lhsT=vaug0[:, kt, :], rhs=pT[ib][:], start=(kt == 0), stop=(kt == nkt - 1)),
                             reads=[("pT", ib), ("vaug", 0, kt // 4), "vaug0c"], writes=["B6"], sig=(kt == nkt - 1))
                    else:
                        S.op("pe", lambda: nc.tensor.matmul(po[:, :], lhsT=vaug1[:, kt, :], rhs=pT[ib][:], start=(kt == 0), stop=(kt == nkt - 1)),
                             reads=[("pT", ib), ("vaug", 1, kt // 4), "vaug1c"], writes=["B7"], sig=(kt == nkt - 1))

                emit_s(0)
                if nkt > 1:
                    emit_s(1)
                for kt in range(nkt):
                    if kt + 2 < nkt:
                        emit_s(kt + 2)
                    emit_pv(kt)
                    if filler is not None:
                        for _ in range(nfill):
                            next(filler, None)

            def normalize(h):
                hs = slice(64 * h, 64 * h + 64)
                po = B[6 + h]
                srow = 64 if h == 0 else 0
                pnm = "B%d" % (6 + h)
                S.op("dve", lambda: nc.vector.reciprocal(out=rs[srow:srow + 1, :], in_=po[srow:srow + 1, :]), reads=[pnm], writes=["rs"])
                S.op("pe", lambda: nc.tensor.matmul(B[3][:, :], lhsT=ones_f[srow:srow + 1, :], rhs=rs[srow:srow + 1, :], start=True, stop=True), reads=["rs", "ones_f"], writes=["B3"])
                S.op("act", lambda: nc.scalar.copy(out=o_sb[hs, :], in_=po[hs, :]), reads=[pnm], writes=["cva"])
                S.op("dve", lambda: nc.vector.tensor_tensor(out=br_sb[hs, 1, :], in0=o_sb[hs, :], in1=B[3][hs, :], op=ALU.mult), reads=["cva", "B3"], writes=[("br_b", h)])

            for _ in gg0:
                pass
            if I + 1 < NTILE:
                prologue(I + 1)
            g1 = gating_gen(1)
            attn_loop(0, g1)
            for _ in g1:
                pass
            normalize(0)
            if I + 1 < NTILE:
                prologue_b()
            f1g = itertools.chain(fm_gen(I + 1, 4), tm_gen(I + 1)) if I + 1 < NTILE else None
            attn_loop(1, f1g)
            if f1g is not None:
                for _ in f1g:
                    pass
            normalize(1)
            S.dma("sp", "br_st", brv[:, :, t0:t0 + 512], br_sb[:], reads=["br_a", ("br_b", 0), ("br_b", 1), "br_c"], writes=[("brT", I)])
        S.finish("sp", [("brT", I) for I in range(NTILE)])


def mixer_consts(D, T, slopes2, g_pre, wlr2_g, blr_g, gnorm, scw_g):
    KD = D // P
    NKT = T // P
    ncc = C_PP + KD + 64 + 1 + 3 + 4
    cc = np.zeros((P, ncc), np.float32)
    j = np.arange(P)[:, None]
    i = np.arange(P)[None, :]
    cc[:, C_U:C_U + P] = (j <= i) * (-1.0 / 16.0)
    cc[:, C_SL:C_SL + P] = (j > i) * (-1.0 / 16.0)
    cc[:, C_TRI:C_TRI + P] = (j <= i) * 1.0
    cc[:, C_ID:C_ID + P] = np.eye(P)
    c = np.arange(128)
    cc[:, C_FILL:C_FILL + 128] = np.where(c < 63, 0.0, np.where(c == 63, 1e30, -1e30))[None, :]
    il = np.arange(P)[:, None]
    b = np.arange(64)[None, :]
    for h in range(2):
        cc[:, C_A0 + 64 * h:C_A0 + 64 * h + 64] = -slopes2[h] * (il - 256.0 * b)
    o = C_PP
    cc[:, o:o + KD] = g_pre.reshape(KD, P).T
    o += KD
    cc[0:16, o:o + 64] = wlr2_g
    cc[16, o:o + 64] = blr_g
    o += 64
    cc[:, o] = gnorm
    o += 1
    cc[:, o:o + 3] = scw_g.T
    o += 3
    for h in range(2):
        for par in range(2):
            cc[:, o + 2 * h + par] = slopes2[h] * (128.0 * par + np.arange(P))
    shiftc = np.zeros((P, 2 * NKT), np.float32)
    for h in range(2):
        shiftc[:, h * NKT:(h + 1) * NKT] = (-BIG - slopes2[h] * 128.0 * np.arange(NKT))[None, :]
    import ml_dtypes
    jl = np.arange(P)[:, None, None]
    kk = np.arange(4)[None, :, None]
    ii = np.arange(512)[None, None, :]
    causneg = np.where(128 * kk + jl <= ii, 0.0, -BIG).astype(ml_dtypes.bfloat16)
    eblk = np.zeros((64, T), np.float32)
    for bb in range(min(64, T // 256)):
        eblk[bb, 256 * bb:256 * bb + 256] = 1.0
    return cc, shiftc, causneg, eblk.astype(ml_dtypes.bfloat16)


def build_mixer(dm):
    nc = bass.Bass("TRN2", target_bir_lowering=False)
    D, T = dm["D"], dm["T"]
    KD, NKT = D // P, T // P
    ncc = C_PP + KD + 64 + 1 + 3 + 4
    a = {
        "xT": nc.dram_tensor("xT", [D, T], F32, kind="ExternalInput").ap(),
        "w_f": nc.dram_tensor("w_f", [D, NFM], F32, kind="ExternalInput").ap(),
        "w_t": nc.dram_tensor("w_t", [D, NTM], F32, kind="ExternalInput").ap(),
        "cc": nc.dram_tensor("cc", [P, ncc], F32, kind="ExternalInput").ap(),
        "shiftc": nc.dram_tensor("shiftc", [P, 2 * NKT], F32, kind="ExternalInput").ap(),
        "causneg": nc.dram_tensor("causneg", [P, 4, 512], BF16, kind="ExternalInput").ap(),
        "eblk": nc.dram_tensor("eblk", [64, T], BF16, kind="ExternalInput").ap(),
        "brT": nc.dram_tensor("brT", [3 * P, T], BF16, kind="ExternalOutput").ap(),
    }
    with contextlib.ExitStack() as es:
        S = Sched(nc, es)
        emit_mixer(nc, S, dm, a)
        print("mixer instrs", S.ninstr, "sems", S.nsem)
    return nc


from concourse.bass_utils import run_bass_kernel_spmd
import ml_dtypes

D_MODEL, SEQ, BATCH, DEPTH, D_FF = 1024, 16384, 2, 2, 2816
ALIBI = 2.0 ** (-8.0 * (np.arange(8) + 1.0) / 8)
_NC_CACHE = {}


def _get(name, fn):
    if name not in _NC_CACHE:
        _NC_CACHE[name] = fn()
    return _NC_CACHE[name]


def _mixer_inputs(xT_b, l, g, w_in, gla_w_lr2, gla_b_lr, gla_norm, sc_conv_w, g_mix_pre):
    W = w_in[l]
    sl = lambda o, n: W[:, o + n * g: o + n * g + n]
    w_f = np.ascontiguousarray(np.concatenate([sl(0, 64), sl(256, 64), sl(1024, 128), W[:, 1536:1552], sl(1552, 128), sl(2064, 128),
                                               sl(3088, 128), sl(3600, 128), sl(4112, 128)], axis=1))
    w_t = np.ascontiguousarray(np.concatenate([sl(256, 64), sl(512, 128), sl(2576, 128)], axis=1))
    cc, shiftc, causneg, eblk = mixer_consts(D_MODEL, SEQ, ALIBI[2 * g:2 * g + 2], g_mix_pre[l], gla_w_lr2[l][:, 64 * g:64 * g + 64],
                                             gla_b_lr[l][64 * g:64 * g + 64], gla_norm[l], sc_conv_w[l][:, 128 * g:128 * g + 128])
    return {"xT": xT_b, "w_f": w_f, "w_t": w_t, "cc": cc, "shiftc": shiftc, "causneg": causneg, "eblk": eblk}


def kernel(x, g_mix_pre, w_in, gla_w_lr2, gla_b_lr, gla_norm, sc_conv_w, w_br_gla, w_br_moba, w_br_sc, w_out, g_mix_post,
           g_ffn_pre, ffn_w_up, ffn_conv_w, ffn_conv_b, ffn_w_down, g_ffn_post):
    f32 = lambda v: np.ascontiguousarray(np.asarray(v, dtype=np.float32))
    (x, g_mix_pre, w_in, gla_w_lr2, gla_b_lr, gla_norm, sc_conv_w, w_br_gla, w_br_moba, w_br_sc, w_out, g_mix_post,
     g_ffn_pre, ffn_w_up, ffn_conv_w, ffn_conv_b, ffn_w_down, g_ffn_post) = map(f32, (
        x, g_mix_pre, w_in, gla_w_lr2, gla_b_lr, gla_norm, sc_conv_w, w_br_gla, w_br_moba, w_br_sc, w_out, g_mix_post,
        g_ffn_pre, ffn_w_up, ffn_conv_w, ffn_conv_b, ffn_w_down, g_ffn_post))
    NTOK = SEQ // 4
    nc_a = _get("mixer", lambda: build_mixer(dict(D=D_MODEL, T=SEQ)))
    nc_b = _get("dense", lambda: build_dense(dict(D=D_MODEL, DFF=D_FF, BW=1536, NTOK=NTOK)))
    xT = [np.ascontiguousarray(x[b].T) for b in range(BATCH)]
    for l in range(DEPTH):
        in_maps = [_mixer_inputs(xT[c // 4], l, c % 4, w_in, gla_w_lr2, gla_b_lr, gla_norm, sc_conv_w, g_mix_pre) for c in range(8)]
        res = run_bass_kernel_spmd(nc_a, in_maps, core_ids=list(range(8)))
        brT = []
        for b in range(BATCH):
            parts = [np.asarray(res.results[4 * b + g]["brT"]) for g in range(4)]
            brT.append(np.concatenate([p[0:128] for p in parts] + [p[128:256] for p in parts] + [p[256:384] for p in parts], axis=0))
        w_gate = np.ascontiguousarray(w_in[l][:, 4624:7696])
        w_br = np.ascontiguousarray(np.concatenate([w_br_gla[l], w_br_moba[l], w_br_sc[l]], axis=0))
        pp = pack_params_dense(g_mix_pre[l], g_mix_post[l], g_ffn_pre[l], g_ffn_post[l], ffn_conv_w[l], ffn_conv_b[l])
        in_maps = []
        for c in range(8):
            b, j = c // 4, c % 4
            t0 = j * NTOK
            xs = np.zeros((D_MODEL, NTOK + 2), np.float32)
            bs = np.zeros((1536, NTOK + 2), ml_dtypes.bfloat16)
            lo = max(t0 - 2, 0)
            xs[:, 2 - (t0 - lo):] = xT[b][:, lo:t0 + NTOK]
            bs[:, 2 - (t0 - lo):] = brT[b][:, lo:t0 + NTOK]
            in_maps.append({"xT": xs, "brT": bs, "w_gate": w_gate, "w_br": w_br, "w_out": w_out[l], "w_up": ffn_w_up[l],
                            "w_down": ffn_w_down[l], "pp": pp})
        res = run_bass_kernel_spmd(nc_b, in_maps, core_ids=list(range(8)))
        xT = [np.ascontiguousarray(np.concatenate([np.asarray(res.results[4 * b + j]["yT"]) for j in range(4)], axis=1)) for b in range(BATCH)]
    return np.ascontiguousarray(np.stack([xT[b].T for b in range(BATCH)], axis=0).astype(np.float32))
```
